# Optimizing a Trainium2 kernel written in Bass

```python
import math
import jax, jax.numpy as jnp
from jax import lax
import numpy as np

D_MODEL = 1024
BATCH = 4
SEQ = 4096
DEPTH = 1

CTX_LEN = 256
GRID_W = 64

MLA_HEADS = 8
QK_NOPE = 64
QK_ROPE = 32
V_DIM = 64
Q_LORA = 256
KV_LORA = 128
MLA_W = MLA_HEADS * V_DIM
ROPE_BASE = 10000.0
ROPE_FREQS_PER_AXIS = QK_ROPE // 4
Q_BLOCK = 128

POOL_WINDOWS = (2, 4, 8, 16)
POOL_GROUPS = 4
POOL_CH = 128
POOL_W = POOL_GROUPS * POOL_CH

IN_W = Q_LORA + KV_LORA + QK_ROPE + POOL_W
MIX_W = MLA_W + POOL_W

N_EXPERTS = 32
TOP_K = 4
D_EXPERT = 1024
SWIGLU_LIMIT = 7.0
SWIGLU_ALPHA = 1.702
MOE_BLOCK = 256

N_MOD = 6
EPS = 1e-6

kernel_name = "hybrid_mla_pool_moe_dit_block"


def _rmsnorm(x, g):
    xf = x.astype(jnp.float32)
    y = xf * lax.rsqrt(jnp.mean(xf * xf, axis=-1, keepdims=True) + EPS)
    return (y * g.astype(jnp.float32)).astype(x.dtype)


def _modulate(x, g, shift, scale):
    return _rmsnorm(x, g) * (1 + scale) + shift


def _axial_rope_angles(rows):
    row = jnp.repeat(jnp.arange(rows, dtype=jnp.float32), GRID_W)
    col = jnp.tile(jnp.arange(GRID_W, dtype=jnp.float32), rows)
    freqs = ROPE_BASE ** (-jnp.arange(ROPE_FREQS_PER_AXIS, dtype=jnp.float32) / ROPE_FREQS_PER_AXIS)
    ang = jnp.concatenate([row[:, None] * freqs, col[:, None] * freqs], axis=-1)
    return jnp.cos(ang), jnp.sin(ang)


def _apply_rope(x, cos, sin):
    half = QK_ROPE // 2
    xf = x.astype(jnp.float32)
    x1, x2 = xf[..., :half], xf[..., half:]
    return jnp.concatenate([x1 * cos - x2 * sin, x2 * cos + x1 * sin], axis=-1).astype(x.dtype)


def _queries(cq, q_norm_g, w_uq):
    B, L, _ = cq.shape
    q = (_rmsnorm(cq, q_norm_g) @ w_uq).reshape(B, L, MLA_HEADS, QK_NOPE + QK_ROPE)
    return q[..., :QK_NOPE], q[..., QK_NOPE:]


def _keys_values(ckv, k_rope, kv_norm_g, w_ukv):
    B, L, _ = ckv.shape
    kv = (_rmsnorm(ckv, kv_norm_g) @ w_ukv).reshape(B, L, MLA_HEADS, QK_NOPE + V_DIM)
    return kv[..., :QK_NOPE], k_rope, kv[..., QK_NOPE:]


def _attend(q_nope, q_rope, k_nope, k_rope, v):
    scale = 1.0 / math.sqrt(QK_NOPE + QK_ROPE)
    s = (jnp.einsum('bqhd,bkhd->bhqk', q_nope, k_nope)
         + jnp.einsum('bqhr,bkr->bhqk', q_rope, k_rope))
    p = jax.nn.softmax(s.astype(jnp.float32) * scale, axis=-1).astype(v.dtype)
    return jnp.einsum('bhqk,bkhd->bqhd', p, v)


def _blocked_attention(q_nope, q_rope, k_nope, k_rope, v):
    B, N = q_nope.shape[:2]
    nblk = N // Q_BLOCK

    def to_blocks(a):
        return a.reshape(B, nblk, Q_BLOCK, *a.shape[2:]).swapaxes(0, 1)

    def one_block(blk):
        qn, qr = blk
        return _attend(qn, qr, k_nope, k_rope, v)

    o = lax.map(one_block, (to_blocks(q_nope), to_blocks(q_rope)))
    return o.swapaxes(0, 1).reshape(B, N, MLA_W)


def _multiscale_pool(u, w_pool, pool_scale):
    B, L, _ = u.shape
    uf = u.astype(jnp.float32).reshape(B, L, POOL_GROUPS, POOL_CH)
    cs = jnp.concatenate([jnp.zeros((B, 1, POOL_GROUPS, POOL_CH), jnp.float32),
                          jnp.cumsum(uf, axis=1)], axis=1)
    t = jnp.arange(L, dtype=jnp.int32)[:, None]
    half = jnp.array(POOL_WINDOWS, dtype=jnp.int32)[None, :] // 2
    lo = jnp.clip(t - half, 0, L)
    hi = jnp.clip(t + half, 0, L)
    gidx = jnp.arange(POOL_GROUPS, dtype=jnp.int32)[None, :]
    window_sum = cs[:, hi, gidx] - cs[:, lo, gidx]
    count = (hi - lo).astype(jnp.float32)[None, :, :, None]
    mix = (window_sum / count - uf).astype(u.dtype)
    y = jnp.einsum('blgc,gcd->blgd', mix, w_pool).reshape(B, L, POOL_W)
    return y * pool_scale


def _moe(h, router_w, router_b, w_gate_up, b_gate_up, w_down, b_down):
    B, L, D = h.shape
    hf = h.reshape(B * L, D)
    T = hf.shape[0]
    logits = hf.astype(jnp.float32) @ router_w.astype(jnp.float32) + router_b.astype(jnp.float32)
    top_val, top_idx = lax.top_k(logits, TOP_K)
    gates = jax.nn.softmax(top_val, axis=-1)
    A = T * TOP_K
    expert = top_idx.reshape(A).astype(jnp.int32)
    token = jnp.repeat(jnp.arange(T, dtype=jnp.int32), TOP_K)
    weight = gates.reshape(A)
    order = jnp.argsort(expert)
    e_s, tok_s, w_s = expert[order], token[order], weight[order]
    counts = jnp.zeros((N_EXPERTS,), jnp.int32).at[expert].add(1)
    padded = (counts + MOE_BLOCK - 1) // MOE_BLOCK * MOE_BLOCK
    starts = jnp.cumsum(counts) - counts
    pad_ends = jnp.cumsum(padded)
    pad_starts = pad_ends - padded
    dest = pad_starts[e_s] + (jnp.arange(A, dtype=jnp.int32) - starts[e_s])
    n_blocks = -(-A // MOE_BLOCK) + N_EXPERTS
    P = n_blocks * MOE_BLOCK
    slot_tok = jnp.zeros((P,), jnp.int32).at[dest].set(tok_s)
    slot_w = jnp.zeros((P,), jnp.float32).at[dest].set(w_s)
    block_expert = jnp.minimum(
        jnp.searchsorted(pad_ends, jnp.arange(n_blocks, dtype=jnp.int32) * MOE_BLOCK, side='right'),
        N_EXPERTS - 1)

    def expert_block(args):
        tok, e = args
        xb = hf[tok]
        gu = xb @ w_gate_up[e] + b_gate_up[e]
        g, lin = gu[..., :D_EXPERT], gu[..., D_EXPERT:]
        g = jnp.minimum(g, SWIGLU_LIMIT)
        lin = jnp.clip(lin, -SWIGLU_LIMIT, SWIGLU_LIMIT)
        act = g * jax.nn.sigmoid(SWIGLU_ALPHA * g) * (lin + 1)
        return act @ w_down[e] + b_down[e]

    y = lax.map(expert_block, (slot_tok.reshape(n_blocks, MOE_BLOCK), block_expert))
    y = y.reshape(P, D) * slot_w[:, None].astype(y.dtype)
    out = jnp.zeros((T, D), h.dtype).at[slot_tok].add(y)
    return out.reshape(B, L, D)


def setup_inputs(seed: int = 0) -> dict:
    key = jax.random.key(seed)
    ks = jax.random.split(key, 24)
    f32 = jnp.float32
    D = D_MODEL

    def nrm(k, shape, scale):
        return jax.random.normal(k, shape, f32) * scale

    return {
        "x": nrm(ks[0], (BATCH, SEQ, D), 1.0),
        "c": nrm(ks[1], (BATCH, D), 1.0),
        "ctx": nrm(ks[2], (BATCH, CTX_LEN, D), 1.0),
        "c_ctx": nrm(ks[3], (D,), 1.0),
        "w_mod": nrm(ks[4], (DEPTH, D, N_MOD * D), 0.5 * D ** -0.5),
        "b_mod": nrm(ks[5], (DEPTH, N_MOD * D), 0.02),
        "norm1_g": 1.0 + nrm(ks[6], (DEPTH, D), 0.05),
        "w_in": nrm(ks[7], (DEPTH, D, IN_W), D ** -0.5),
        "q_norm_g": 1.0 + nrm(ks[8], (DEPTH, Q_LORA), 0.05),
        "kv_norm_g": 1.0 + nrm(ks[9], (DEPTH, KV_LORA), 0.05),
        "w_uq": nrm(ks[10], (DEPTH, Q_LORA, MLA_HEADS * (QK_NOPE + QK_ROPE)), Q_LORA ** -0.5),
        "w_ukv": nrm(ks[11], (DEPTH, KV_LORA, MLA_HEADS * (QK_NOPE + V_DIM)), KV_LORA ** -0.5),
        "w_pool": nrm(ks[12], (DEPTH, POOL_GROUPS, POOL_CH, POOL_CH), POOL_CH ** -0.5),
        "pool_scale": 1.0 + nrm(ks[13], (DEPTH, POOL_W), 0.1),
        "w_out": nrm(ks[14], (DEPTH, MIX_W, D), MIX_W ** -0.5),
        "norm2_g": 1.0 + nrm(ks[15], (DEPTH, D), 0.05),
        "router_w": nrm(ks[16], (DEPTH, D, N_EXPERTS), D ** -0.5),
        "router_b": nrm(ks[17], (DEPTH, N_EXPERTS), 0.01),
        "w_gate_up": nrm(ks[18], (DEPTH, N_EXPERTS, D, 2 * D_EXPERT), D ** -0.5),
        "b_gate_up": nrm(ks[19], (DEPTH, N_EXPERTS, 2 * D_EXPERT), 0.02),
        "w_down": nrm(ks[20], (DEPTH, N_EXPERTS, D_EXPERT, D), D_EXPERT ** -0.5),
        "b_down": nrm(ks[21], (DEPTH, N_EXPERTS, D), 0.02),
        "final_g": 1.0 + nrm(ks[22], (D,), 0.05),
    }


def reference(x, c, ctx, c_ctx, w_mod, b_mod, norm1_g, w_in, q_norm_g, kv_norm_g, w_uq, w_ukv,
              w_pool, pool_scale, w_out, norm2_g, router_w, router_b, w_gate_up, b_gate_up,
              w_down, b_down, final_g):
    n_lat = x.shape[1]
    rows = n_lat // GRID_W
    cos, sin = _axial_rope_angles(rows)
    kv_lo, kv_hi = Q_LORA, Q_LORA + KV_LORA + QK_ROPE

    for l in range(DEPTH):
        mod_lat = (jax.nn.silu(c) @ w_mod[l] + b_mod[l])[:, None, :]
        mod_ctx = (jax.nn.silu(c_ctx) @ w_mod[l] + b_mod[l])[None, None, :]
        sh1, sc1, g1, sh2, sc2, g2 = jnp.split(mod_lat, N_MOD, axis=-1)
        csh1, csc1, cg1, csh2, csc2, cg2 = jnp.split(mod_ctx, N_MOD, axis=-1)

        h_lat = _modulate(x, norm1_g[l], sh1, sc1)
        h_ctx = _modulate(ctx, norm1_g[l], csh1, csc1)

        z = h_lat @ w_in[l]
        cq, ckv, kr_lat, u_lat = jnp.split(z, [Q_LORA, Q_LORA + KV_LORA, kv_hi], axis=-1)
        qn_lat, qr_lat = _queries(cq, q_norm_g[l], w_uq[l])
        qr_lat = _apply_rope(qr_lat, cos[:, None, :], sin[:, None, :])
        kn_lat, kr_lat, v_lat = _keys_values(ckv, _apply_rope(kr_lat, cos, sin), kv_norm_g[l], w_ukv[l])

        z_ckv = h_ctx @ w_in[l][:, kv_lo:kv_hi]
        kn_ctx, kr_ctx, v_ctx = _keys_values(z_ckv[..., :KV_LORA], z_ckv[..., KV_LORA:],
                                             kv_norm_g[l], w_ukv[l])

        k_nope = jnp.concatenate([kn_ctx, kn_lat], axis=1)
        k_rope = jnp.concatenate([kr_ctx, kr_lat], axis=1)
        v_all = jnp.concatenate([v_ctx, v_lat], axis=1)
        att_lat = _blocked_attention(qn_lat, qr_lat, k_nope, k_rope, v_all)
        pool_lat = _multiscale_pool(u_lat, w_pool[l], pool_scale[l])
        mix_lat = jnp.concatenate([att_lat, pool_lat], axis=-1) @ w_out[l]
        x = x + g1 * mix_lat

        h2 = _modulate(x, norm2_g[l], sh2, sc2)
        x = x + g2 * _moe(h2, router_w[l], router_b[l], w_gate_up[l], b_gate_up[l], w_down[l], b_down[l])

        if l < DEPTH - 1:
            qn_ctx, qr_ctx = _queries(h_ctx @ w_in[l][:, :Q_LORA], q_norm_g[l], w_uq[l])
            att_ctx = _attend(qn_ctx, qr_ctx, kn_ctx, kr_ctx, v_ctx).reshape(ctx.shape[0], CTX_LEN, MLA_W)
            pool_ctx = _multiscale_pool(h_ctx @ w_in[l][:, kv_hi:], w_pool[l], pool_scale[l])
            ctx = ctx + cg1 * (jnp.concatenate([att_ctx, pool_ctx], axis=-1) @ w_out[l])
            h2c = _modulate(ctx, norm2_g[l], csh2, csc2)
            ctx = ctx + cg2 * _moe(h2c, router_w[l], router_b[l], w_gate_up[l], b_gate_up[l],
                                   w_down[l], b_down[l])

    return _rmsnorm(x, final_g)
```

```python
import math
from contextlib import ExitStack

import numpy as np
import concourse.bass as bass
import concourse.mybir as mybir
from concourse.bass_utils import run_bass_kernel_spmd

F32 = mybir.dt.float32
BF16 = mybir.dt.bfloat16
I32 = mybir.dt.int32
U32 = mybir.dt.uint32
AF = mybir.ActivationFunctionType
ALU = mybir.AluOpType

D = 1024
SEQ = 4096
CTX = 256
NK = SEQ + CTX
NKT = NK // 128
NQ = 2048
H = 8
EPS = 1e-6
NE = 32
SCALE = 1.0 / math.sqrt(96.0)
KB = 1024
ARENA_BYTES = 180 * KB
DEBUG = False
TPP = 4


class Sem:
    def __init__(self, h, name):
        self.h = h
        self.name = name
        self.cnt = 0


class Buf:
    __slots__ = ("name", "w", "r")

    def __init__(self, name=""):
        self.name = name
        self.w = None
        self.r = {}


class Eng:
    def __init__(self, name, h, sem):
        self.name = name
        self.h = h
        self.sem = sem
        self.seen = {}


class Sched:
    def __init__(self, nc, es):
        self.nc = nc
        self.es = es
        self.all_sems = []
        self.pe = Eng("pe", nc.tensor, self.mksem("s_pe"))
        self.act = Eng("act", nc.scalar, self.mksem("s_act"))
        self.dve = Eng("dve", nc.vector, self.mksem("s_dve"))
        self.pool = Eng("pool", nc.gpsimd, self.mksem("s_pool"))
        self.sp = Eng("sp", nc.sync, self.mksem("s_sp"))
        self.engs = [self.pe, self.act, self.dve, self.pool, self.sp]

    def mksem(self, name):
        s = Sem(self.es.enter_context(self.nc.semaphore(name)), name)
        self.all_sems.append(s)
        return s

    def _deps(self, reads, writes):
        toks = {}

        def add(t):
            if t is None:
                return
            sem, val = t
            if toks.get(sem, 0) < val:
                toks[sem] = val

        for b in reads:
            add(b.w)
        for b in writes:
            add(b.w)
            for t in b.r.values():
                add(t)
        return toks

    def _waits(self, e, toks):
        for sem, val in toks.items():
            if sem is e.sem and e is self.pe:
                continue
            if e.seen.get(sem, 0) >= val:
                continue
            e.h.wait_ge(sem.h, val)
            e.seen[sem] = val

    def _update(self, tok, reads, writes):
        for b in reads:
            b.r[tok[0]] = tok
        for b in writes:
            b.w = tok
            b.r = {}

    def op(self, e, fn, reads=(), writes=()):
        self._waits(e, self._deps(reads, writes))
        inst = fn()
        e.sem.cnt += 1
        inst.then_inc(e.sem.h, 1)
        tok = (e.sem, e.sem.cnt)
        self._update(tok, reads, writes)
        return tok

    def dma(self, q, out, in_, dsem, reads=(), writes=()):
        self._waits(q, self._deps(reads, writes))
        inst = q.h.dma_start(out=out, in_=in_)
        dsem.cnt += 16
        inst.then_inc(dsem.h, 16)
        tok = (dsem, dsem.cnt)
        self._update(tok, reads, writes)
        return tok

    def cond_snapshot(self):
        return ({s: s.cnt for s in self.all_sems}, {e.name: dict(e.seen) for e in self.engs})

    def cond_compensate(self, snap, owners):
        before, _ = snap
        for s in self.all_sems:
            delta = s.cnt - before[s]
            if delta == 0:
                continue
            e = None
            for en in self.engs:
                if en.sem is s:
                    e = en
            if e is None:
                e = owners[s]
            if before[s] > 0:
                e.h.wait_ge(s.h, before[s])
            e.h.sem_inc(s.h, delta)

    def cond_restore_seen(self, snap):
        _, seen = snap
        for e in self.engs:
            e.seen = dict(seen[e.name])

    def barrier(self):
        toks = {s: s.cnt for s in self.all_sems if s.cnt > 0}
        for e in self.engs:
            self._waits(e, toks)


class Arena:
    def __init__(self, t):
        self.t = t

    def view(self, off, shape, dt, parts=None):
        esz = 2 if dt == BF16 else 4
        n = 1
        for s in shape[1:]:
            n *= s
        nbytes = n * esz
        assert off % 4 == 0 and off + nbytes <= ARENA_BYTES, (off, shape)
        ap = self.t[:, off // 2:(off + nbytes) // 2]
        if dt != BF16:
            ap = ap.bitcast(dt)
        if len(shape) == 3:
            ap = ap.rearrange("p (a b) -> p a b", b=shape[2])
        elif len(shape) == 4:
            ap = ap.rearrange("p (a b c) -> p a b c", b=shape[2], c=shape[3])
        if shape[0] != 128:
            ap = ap[0:shape[0]]
        return ap


def build_program():
    nc = bass.Bass("TRN2", target_bir_lowering=False)

    def din(name, shape):
        return nc.dram_tensor(name, list(shape), F32, kind="ExternalInput").ap()

    xe = din("xe", [36 * 128, D])
    trig = din("trig", [2, 32, NK])
    ccol_d = din("ccol", [128, 8, 2])
    hmask_d = din("hmask", [128, 16])
    invc_d = din("invc", [128, 4, 16])
    w_mod = din("w_mod", [D, 6 * D])
    bmod2_d = din("bmod2", [2, 6 * D])
    n1col_d = din("n1col", [128, 8])
    n2col_d = din("n2col", [128, 8])
    w_in = din("w_in", [D, 928])
    qg_d = din("qgcol", [128, 2])
    kvg_d = din("kvgcol", [128, 1])
    w_uq = din("w_uq", [256, 768])
    w_ukv = din("w_ukv", [128, 1024])
    w_pool = din("w_pool", [4, 128, 128])
    pscol_d = din("pscol", [128, 4])
    w_out = din("w_out", [D, D])
    router_w = din("router_w", [D, NE])
    rb_d = din("rbrep", [128, NE])
    w_gu = din("w_gu", [NE, D, 2 * D])
    b_gu = din("b_gu", [NE, 2 * D])
    n2row_d = din("n2row", [1, D])
    iota_d = din("iota32", [128, NE])
    ustr_d = din("ustr", [128, 128])
    tokrow_d = nc.dram_tensor("tokrow", [128, 16, 2], I32, kind="ExternalInput").ap()
    h2d = nc.dram_tensor("h2d", [NQ, D], BF16).ap()
    xnew_d = nc.dram_tensor("xnew_d", [NQ, D], F32).ap()
    tokl = nc.dram_tensor("tokl", [NE * 2048, 2], I32).ap()
    Yd = nc.dram_tensor("Yd", [NE * 2048, D], F32).ap()
    nt_d = nc.dram_tensor("nt_d", [1, NE + 1], I32).ap()
    w_dn = din("w_dn", [NE, D, D])
    b_dn = din("b_dn", [NE, D])
    fg_d = din("fgrep", [128, D])
    ident_d = din("ident", [128, 128])
    y = nc.dram_tensor("y", [NQ, D], F32, kind="ExternalOutput").ap()
    with ExitStack() as es:
        T = Sched(nc, es)
        pe, act, dve, pool, sp = T.pe, T.act, T.dve, T.pool, T.sp

        def sb(name, shape, dt=F32):
            return es.enter_context(nc.sbuf_tensor("sb_" + name, list(shape), dt))

        arena_t = sb("arena", [128, ARENA_BYTES // 2], BF16)
        AR = Arena(arena_t)
        ps_t = es.enter_context(nc.psum_tensor("ps", [128, 4096], F32))
        PB = [Buf(f"psb{i}") for i in range(8)]

        def bank(i, n=512, parts=128, nb=1):
            return ps_t[0:parts, i * 512:i * 512 + (n if nb == 1 else nb * 512)]

        identF = sb("identF", [128, 128]); identB = sb("identB", [128, 128], BF16)
        onesF = sb("onesF", [128, 128])
        ccol = sb("ccolT", [128, 8, 2]); scT = sb("scT", [128, 8, 2])
        modcol = sb("modcol", [128, 32, 2])
        n1col = sb("n1col", [128, 8]); n2col = sb("n2col", [128, 8])
        G1L = sb("G1L", [128, 8]); G1C = sb("G1C", [128, 8]); G2c = sb("G2c", [128, 8])
        zb = sb("zb", [128, 16])
        qgc = sb("qgc", [128, 2]); kvgc = sb("kvgc", [128, 1]); pscol = sb("pscol", [128, 4])
        hmask = sb("hmask", [128, 16]); invc = sb("invc", [128, 4, 16])
        g2B = sb("g2B", [128, D])
        iota32 = sb("iota32s", [128, NE]); ustrF = sb("ustrF", [128, 128]); UstrB = sb("UstrB", [128, 128], BF16)
        onesB = sb("onesB", [128, 128], BF16); tokrow = sb("tokrows", [128, 16, 2], I32)
        rbB = sb("rbB", [128, NE]); rwt = sb("rwt", [128, 8, NE])
        bdS = sb("bdS", [32, D])
        ssv = sb("ssv", [128, 8]); rstdv = sb("rstdv", [128, 8])
        junk = sb("junk", [128, D], BF16)
        smallA = sb("smallA", [128, 64])
        b_const = Buf("const")

        ldA = T.mksem("ldA")
        for dst, src in ((identF, ident_d), (ccol, ccol_d), (n1col, n1col_d), (n2col, n2col_d), (qgc, qg_d),
                         (kvgc, kvg_d), (pscol, pscol_d), (hmask, hmask_d), (invc, invc_d), (iota32, iota_d),
                         (ustrF, ustr_d), (tokrow, tokrow_d), (rbB, rb_d)):
            T.dma(sp, dst[:], src, ldA, writes=[Buf()])
        T.dma(sp, rwt[:], router_w.rearrange("(k q) e -> q k e", q=128), ldA, writes=[Buf()])
        T.dma(sp, bdS[:], b_dn, ldA, writes=[Buf()])
        T.barrier()

        b_c2 = Buf("c2")
        epsT = sb("epsT", [128, 1])
        T.op(dve, lambda: nc.vector.memset(epsT[:], EPS), writes=[b_c2])
        T.op(dve, lambda: nc.vector.memset(onesF[:], 1.0), writes=[b_c2])
        T.op(dve, lambda: nc.vector.tensor_copy(out=identB[:], in_=identF[:]), reads=[b_const], writes=[b_c2])
        T.op(dve, lambda: nc.vector.memset(onesB[:], 1.0), writes=[b_c2])
        T.op(dve, lambda: nc.vector.tensor_copy(out=UstrB[:], in_=ustrF[:]), reads=[b_const], writes=[b_c2])

        modrow = AR.view(108 * KB, [2, 6 * D], F32)
        bmod2 = AR.view(132 * KB, [2, 6 * D], F32)
        wmod_ring = [AR.view(i * 16 * KB, [128, 8, 512], F32) for i in range(2)]
        win_f = AR.view(32 * KB, [128, 8, 928], F32)
        wuq_f = AR.view(61 * KB, [128, 2, 768], F32)
        wukv_f = AR.view(67 * KB, [128, 1024], F32)
        w_kn = AR.view(86 * KB, [128, 8, 64], BF16)
        w_v = AR.view(87 * KB, [128, 512], BF16)
        w_inL = AR.view(88 * KB, [128, 8, 416], BF16)
        w_inC = AR.view(95 * KB, [128, 8, 160], BF16)
        w_rotL = AR.view(98 * KB, [128, 8, 96], BF16)
        w_rotC = AR.view(100 * KB, [128, 8, 96], BF16)
        w_uqS = AR.view(102 * KB, [128, 2, 768], BF16)
        w_uqR = AR.view(105 * KB, [128, 2, 768], BF16)
        win_rotf = AR.view(156 * KB, [128, 8, 96], F32)

        b_ph0 = Buf("ph0")
        rows2 = AR.view(172 * KB, [1, 2, D], F32)
        n2row = AR.view(160 * KB, [1, D], F32)
        zt = AR.view(164 * KB, [128, 1024], I32)
        b_tokl0 = Buf("tokl0")
        zsem = T.mksem("zs")
        T.dma(sp, n2row, n2row_d, ldA, writes=[Buf()])
        T.op(pool, lambda: nc.gpsimd.memset(zt, 0), writes=[b_tokl0])
        T.dma(sp, tokl.rearrange("(p a) b -> p (a b)", p=128), zt, zsem, reads=[b_tokl0], writes=[b_tokl0])
        T.dma(sp, bmod2, bmod2_d, ldA, writes=[Buf()])
        T.dma(sp, win_f, w_in.rearrange("(k q) f -> q k f", q=128), ldA, writes=[Buf()])
        T.dma(sp, wuq_f, w_uq.rearrange("(k q) f -> q k f", q=128), ldA, writes=[Buf()])
        T.dma(sp, wukv_f, w_ukv, ldA, writes=[Buf()])
        T.barrier()

        T.op(act, lambda: nc.scalar.activation(out=scT[:], in_=ccol[:], func=AF.Silu), reads=[b_const], writes=[b_ph0])

        wm_sem = [T.mksem(f"wm{i}") for i in range(2)]
        wm_buf = [Buf(f"wm{i}") for i in range(2)]
        b_modrow = Buf("modrow")
        for ns in range(12):
            s = ns % 2
            T.dma(sp, wmod_ring[s], w_mod.rearrange("(k q) n -> q k n", q=128)[:, :, ns * 512:(ns + 1) * 512],
                  wm_sem[s], writes=[wm_buf[s]])
            pb = ns % 2

            def mm(s=s, pb=pb):
                last = None
                for k in range(8):
                    last = nc.tensor.matmul(bank(pb, parts=2), lhsT=scT[:, k, :], rhs=wmod_ring[s][:, k, :],
                                            start=(k == 0), stop=(k == 7))
                return last
            T.op(pe, mm, reads=[wm_buf[s], b_ph0], writes=[PB[pb]])
            T.op(dve, lambda pb=pb, ns=ns: nc.vector.tensor_tensor(
                out=modrow[:, ns * 512:(ns + 1) * 512], in0=bank(pb, parts=2), in1=bmod2[:, ns * 512:(ns + 1) * 512],
                op=ALU.add), reads=[PB[pb], b_const], writes=[b_modrow])

        chunk_src = [0 * 8 + i for i in range(8)] + [1 * 8 + i for i in range(8)] + \
                    [3 * 8 + i for i in range(8)] + [4 * 8 + i for i in range(8)]

        def tr_mod():
            last = None
            for i, ch in enumerate(chunk_src):
                last = nc.tensor.transpose(out=bank(2, 64)[:, 2 * i:2 * i + 2], in_=modrow[:, ch * 128:(ch + 1) * 128],
                                           identity=identF[0:2, 0:2])
            return last
        T.op(pe, tr_mod, reads=[b_modrow, b_const], writes=[PB[2]])
        T.op(dve, lambda: nc.vector.tensor_copy(out=modcol[:].rearrange("p a b -> p (a b)"), in_=bank(2, 64)),
             reads=[PB[2]], writes=[b_ph0])
        T.op(dve, lambda: nc.vector.scalar_tensor_tensor(out=G1L[:], in0=modcol[:, 8:16, 0], scalar=1.0, in1=n1col[:],
                                                         op0=ALU.add, op1=ALU.mult), reads=[b_ph0, b_const], writes=[b_ph0])
        T.op(dve, lambda: nc.vector.scalar_tensor_tensor(out=G1C[:], in0=modcol[:, 8:16, 1], scalar=1.0, in1=n1col[:],
                                                         op0=ALU.add, op1=ALU.mult), reads=[b_ph0, b_const], writes=[b_ph0])
        T.op(dve, lambda: nc.vector.scalar_tensor_tensor(out=G2c[:], in0=modcol[:, 24:32, 0], scalar=1.0, in1=n2col[:],
                                                         op0=ALU.add, op1=ALU.mult), reads=[b_ph0, b_const], writes=[b_ph0])
        for hf in range(2):
            T.op(pe, lambda hf=hf: nc.tensor.matmul(bank(3 + hf), lhsT=onesF[0:1, :],
                                                    rhs=modrow[0:1, 5 * D + hf * 512:5 * D + (hf + 1) * 512],
                                                    start=True, stop=True), reads=[b_modrow, b_const], writes=[PB[3 + hf]])
            T.op(dve, lambda hf=hf: nc.vector.tensor_copy(out=g2B[:, hf * 512:(hf + 1) * 512], in_=bank(3 + hf)),
                 reads=[PB[3 + hf]], writes=[b_ph0])
        g1row = sb("g1row", [1, D])
        T.op(act, lambda: nc.scalar.copy(out=g1row[:], in_=modrow[0:1, 2 * D:3 * D]), reads=[b_modrow], writes=[b_ph0])
        T.op(dve, lambda: nc.vector.scalar_tensor_tensor(out=rows2[0:1, 0, :], in0=modrow[0:1, 4 * D:5 * D], scalar=1.0, in1=n2row,
                                                         op0=ALU.add, op1=ALU.mult), reads=[b_modrow, b_const], writes=[b_ph0])
        T.op(dve, lambda: nc.vector.tensor_copy(out=rows2[0:1, 1, :], in_=modrow[0:1, 3 * D:4 * D]), reads=[b_modrow], writes=[b_ph0])
        T.op(dve, lambda: nc.vector.tensor_tensor(out=bdS[:], in0=bdS[:], in1=g2B[0:32, :], op=ALU.mult),
             reads=[b_const, b_ph0], writes=[b_ph0])

        T.op(dve, lambda: nc.vector.memset(win_rotf, 0.0), writes=[b_ph0])
        T.op(dve, lambda: nc.vector.tensor_scalar(out=win_rotf[:, :, 64:80], in0=win_f[:, :, 400:416], scalar1=-1.0,
                                                  scalar2=None, op0=ALU.mult), reads=[b_const], writes=[b_ph0])
        T.op(dve, lambda: nc.vector.tensor_copy(out=win_rotf[:, :, 80:96], in_=win_f[:, :, 384:400]),
             reads=[b_const], writes=[b_ph0])

        zb_specs = [(win_f, 0, 128, 0, 128), (win_f, 128, 256, 0, 128), (win_f, 256, 384, 0, 128),
                    (win_f, 320, 416, 0, 96), (win_rotf, 0, 96, 0, 96),
                    (win_f, 416, 544, 0, 128), (win_f, 544, 672, 0, 128), (win_f, 672, 800, 0, 128),
                    (win_f, 800, 928, 0, 128),
                    (win_f, 256, 384, 1, 128), (win_f, 320, 416, 1, 96), (win_rotf, 0, 96, 1, 96)]

        def zb_mm():
            last = None
            for j, (wt, c0, c1, which, m) in enumerate(zb_specs):
                for k in range(8):
                    last = nc.tensor.matmul(bank(5, 16)[0:m, j:j + 1], lhsT=wt[:, k, c0:c1],
                                            rhs=modcol[:, k, which:which + 1], start=(k == 0), stop=(k == 7))
            return last
        T.op(pe, zb_mm, reads=[b_const, b_ph0], writes=[PB[5]])
        T.op(dve, lambda: nc.vector.memset(zb[:], 0.0), writes=[b_ph0])
        for j, (wt, c0, c1, which, m) in enumerate(zb_specs):
            T.op(dve, lambda j=j, m=m: nc.vector.tensor_copy(out=zb[0:m, j:j + 1], in_=bank(5, 16)[0:m, j:j + 1]),
                 reads=[PB[5]], writes=[b_ph0])

        for k in range(8):
            T.op(dve, lambda k=k: nc.vector.tensor_scalar(out=w_inL[:, k, :], in0=win_f[:, k, 0:416], scalar1=G1L[:, k:k + 1],
                                                          scalar2=None, op0=ALU.mult), reads=[b_const, b_ph0], writes=[b_ph0])
            T.op(dve, lambda k=k: nc.vector.tensor_scalar(out=w_inC[:, k, :], in0=win_f[:, k, 256:416], scalar1=G1C[:, k:k + 1],
                                                          scalar2=None, op0=ALU.mult), reads=[b_const, b_ph0], writes=[b_ph0])
        for wr, ws, o in ((w_rotL, w_inL, 384), (w_rotC, w_inC, 128)):
            T.op(dve, lambda wr=wr: nc.vector.memset(wr, 0.0), writes=[b_ph0])
            T.op(dve, lambda wr=wr, ws=ws, o=o: nc.vector.tensor_scalar(out=wr[:, :, 64:80], in0=ws[:, :, o + 16:o + 32],
                                                                        scalar1=-1.0, scalar2=None, op0=ALU.mult),
                 reads=[b_ph0], writes=[b_ph0])
            T.op(dve, lambda wr=wr, ws=ws, o=o: nc.vector.tensor_copy(out=wr[:, :, 80:96], in_=ws[:, :, o:o + 16]),
                 reads=[b_ph0], writes=[b_ph0])
        T.op(dve, lambda: nc.vector.memset(w_uqR, 0.0), writes=[b_ph0])
        for c in range(2):
            T.op(dve, lambda c=c: nc.vector.tensor_scalar(out=w_uqS[:, c, :], in0=wuq_f[:, c, :], scalar1=qgc[:, c:c + 1],
                                                          scalar2=None, op0=ALU.mult), reads=[b_const], writes=[b_ph0])
            sv = w_uqS[:, c, :].rearrange("p (h e) -> p h e", e=96)
            rv = w_uqR[:, c, :].rearrange("p (h e) -> p h e", e=96)
            T.op(dve, lambda sv=sv, rv=rv: nc.vector.tensor_scalar(out=rv[:, :, 64:80], in0=sv[:, :, 80:96], scalar1=-1.0,
                                                                   scalar2=None, op0=ALU.mult), reads=[b_ph0], writes=[b_ph0])
            T.op(dve, lambda sv=sv, rv=rv: nc.vector.tensor_copy(out=rv[:, :, 80:96], in_=sv[:, :, 64:80]),
                 reads=[b_ph0], writes=[b_ph0])
        kvv = wukv_f.rearrange("p (h e) -> p h e", e=128)
        T.op(dve, lambda: nc.vector.tensor_scalar(out=w_kn, in0=kvv[:, :, 0:64], scalar1=kvgc[:, 0:1], scalar2=None,
                                                  op0=ALU.mult), reads=[b_const], writes=[b_ph0])
        T.op(dve, lambda: nc.vector.tensor_scalar(out=w_v.rearrange("p (h e) -> p h e", e=64), in0=kvv[:, :, 64:128],
                                                  scalar1=kvgc[:, 0:1], scalar2=None, op0=ALU.mult),
             reads=[b_const], writes=[b_ph0])
        T.barrier()

        x_sem = [T.mksem(f"xs{i}") for i in range(3)]
        x_buf = [Buf(f"xb{i}") for i in range(3)]
        xn_buf = [Buf(f"xn{i}") for i in range(2)]
        ss_buf = [Buf(f"ss{i}") for i in range(8)]
        state = {"xi": 0, "ni": 0, "si": 0, "tp": 0}

        def norm_tile_to_hT(x_ring, xn_ring, src_rows, hT_ap, hT_b, col0, tp_banks):
            xi = state["xi"] % len(x_ring); state["xi"] += 1
            ni = state["ni"] % 2; state["ni"] += 1
            si = state["si"] % 8; state["si"] += 1
            tb = tp_banks[state["tp"] % len(tp_banks)]; state["tp"] += 1
            xt = x_ring[xi]; xn = xn_ring[ni]
            T.dma(sp, xt, xe[src_rows * 128:(src_rows + 1) * 128, :], x_sem[xi], writes=[x_buf[xi]])
            T.op(act, lambda: nc.scalar.activation(out=junk[:], in_=xt, func=AF.Square, accum_out=ssv[:, si:si + 1]),
                 reads=[x_buf[xi]], writes=[ss_buf[si]])
            T.op(act, lambda: nc.scalar.activation(out=rstdv[:, si:si + 1], in_=ssv[:, si:si + 1], func=AF.Sqrt,
                                                   scale=1.0 / D, bias=epsT[:]), reads=[ss_buf[si]], writes=[ss_buf[si]])
            T.op(dve, lambda: nc.vector.reciprocal(out=rstdv[:, si:si + 1], in_=rstdv[:, si:si + 1]),
                 reads=[ss_buf[si]], writes=[ss_buf[si]])
            T.op(act, lambda: nc.scalar.activation(out=xn, in_=xt, func=AF.Copy, scale=rstdv[:, si:si + 1]),
                 reads=[x_buf[xi], ss_buf[si]], writes=[xn_buf[ni]])
            tpv = bank(tb).bitcast(BF16)

            def tr():
                last = None
                for c in range(8):
                    last = nc.tensor.transpose(out=tpv[:, c * 128:(c + 1) * 128], in_=xn[:, c * 128:(c + 1) * 128],
                                               identity=identB[:])
                return last
            T.op(pe, tr, reads=[xn_buf[ni], b_const], writes=[PB[tb]])
            T.op(dve, lambda: nc.vector.tensor_copy(out=hT_ap[:, :, col0:col0 + 128],
                                                    in_=tpv.rearrange("p (c t) -> p c t", t=128)),
                 reads=[PB[tb]], writes=[hT_b])

        def rstd_bcast(psb, n, out_ap, inv_n, bufs_r, bufs_w):
            T.op(act, lambda: nc.scalar.activation(out=out_ap, in_=bank(psb, n), func=AF.Sqrt, scale=inv_n, bias=epsT[:]),
                 reads=bufs_r, writes=bufs_w)
            T.op(dve, lambda: nc.vector.reciprocal(out=out_ap, in_=out_ap), reads=bufs_w, writes=bufs_w)

        QT_all = AR.view(0, [96, 8, NQ], BF16)
        V_ext = AR.view(32 * KB, [128, NKT, 8, 65], BF16)
        ckvnT = AR.view(68 * KB, [128, NK], BF16)
        krT = AR.view(77 * KB, [96, NK], BF16)
        x_ring = [AR.view((108 + 4 * i) * KB, [128, D], F32) for i in range(3)]
        xn_ring = [AR.view((120 + 2 * i) * KB, [128, D], BF16) for i in range(2)]
        hT_ring = [AR.view((124 + 8 * i) * KB, [128, 8, 512], BF16) for i in range(2)]
        cos_ring = [AR.view((140 + 4 * i) * KB, [128, 512], F32) for i in range(2)]
        sin_ring = [AR.view((142 + 4 * i) * KB, [128, 512], F32) for i in range(2)]
        ckv_sb = AR.view(148 * KB, [128, 512], F32)
        sq_kv = AR.view(150 * KB, [128, 512], F32)
        cq_sb = AR.view(152 * KB, [128, 2, 512], F32)
        sq_q = AR.view(156 * KB, [128, 2, 512], F32)
        rstd_kv = AR.view(160 * KB, [128, 512], F32)
        rstd_q = AR.view(162 * KB, [128, 512], F32)
        cqn = AR.view(164 * KB, [128, 2, 512], BF16)
        t1 = AR.view(168 * KB, [128, 512], F32)
        t2 = AR.view(170 * KB, [128, 512], F32)

        b_QT = Buf("QT"); b_V = Buf("V"); b_ckvn = Buf("ckvn"); b_kr = Buf("kr")
        hT_b = [Buf("hT0"), Buf("hT1")]
        tg_sem = [T.mksem(f"tg{i}") for i in range(2)]
        tg_b = [Buf("tg0"), Buf("tg1")]
        b_ckvsb = Buf("ckvsb"); b_sqkv = Buf("sqkv"); b_cqsb = Buf("cqsb"); b_sqq = Buf("sqq")
        b_rkv = Buf("rkv"); b_rq = Buf("rq"); b_cqn = Buf("cqn"); b_t1 = Buf("t1"); b_t2 = Buf("t2")

        T.op(dve, lambda: nc.vector.memset(V_ext, 1.0), writes=[b_V])

        mmb = {"i": 0}

        def nextbank():
            b = 2 + (mmb["i"] % 6)
            mmb["i"] += 1
            return b

        groups = []
        for g in range(4):
            groups.append(("own", [4 * g + i for i in range(4)], g * 512))
        for g in range(4):
            groups.append(("other", [18 + 4 * g + i for i in range(4)], 2048 + g * 512))
        groups.append(("ctx", [34, 35], 4096))

        def norm_group_1a(gi):
            kind, tiles, kc0 = groups[gi]
            n = 128 * len(tiles)
            hb = gi % 2
            T.dma(sp, cos_ring[hb][64:96, 0:n], trig[0, :, kc0:kc0 + n], tg_sem[hb], writes=[tg_b[hb]])
            T.dma(sp, sin_ring[hb][64:96, 0:n], trig[1, :, kc0:kc0 + n], tg_sem[hb], writes=[tg_b[hb]])
            for ti, t in enumerate(tiles):
                norm_tile_to_hT(x_ring, xn_ring, t, hT_ring[hb], hT_b[hb], ti * 128, (0, 1))

        for gi, (kind, tiles, kc0) in enumerate(groups):
            n = 128 * len(tiles)
            hb = gi % 2
            hT = hT_ring[hb]
            W = w_inC if kind == "ctx" else w_inL
            WR = w_rotC if kind == "ctx" else w_rotL
            off = -256 if kind == "ctx" else 0
            zj = (9, 10, 11) if kind == "ctx" else (2, 3, 4)
            if gi == 0:
                norm_group_1a(0)
            if gi + 1 < len(groups):
                norm_group_1a(gi + 1)
            pb = nextbank()

            def mm_ckv(pb=pb, W=W, off=off, hT=hT, n=n):
                last = None
                for k in range(8):
                    last = nc.tensor.matmul(bank(pb, n), lhsT=W[:, k, 256 + off:384 + off], rhs=hT[:, k, 0:n],
                                            start=(k == 0), stop=(k == 7))
                return last
            T.op(pe, mm_ckv, reads=[hT_b[hb], b_ph0], writes=[PB[pb]])
            T.op(act, lambda pb=pb, n=n, zj=zj: nc.scalar.activation(out=ckv_sb[:, 0:n], in_=bank(pb, n), func=AF.Identity,
                                                                    bias=zb[:, zj[0]:zj[0] + 1]),
                 reads=[PB[pb], b_ph0], writes=[b_ckvsb])
            T.op(act, lambda n=n: nc.scalar.activation(out=sq_kv[:, 0:n], in_=ckv_sb[:, 0:n], func=AF.Square),
                 reads=[b_ckvsb], writes=[b_sqkv])
            pb2 = nextbank()
            T.op(pe, lambda pb2=pb2, n=n: nc.tensor.matmul(bank(pb2, n), lhsT=onesF[:], rhs=sq_kv[:, 0:n], start=True, stop=True),
                 reads=[b_sqkv, b_const], writes=[PB[pb2]])
            rstd_bcast(pb2, n, rstd_kv[:, 0:n], 1.0 / 128, [PB[pb2]], [b_rkv])
            T.op(dve, lambda n=n, kc0=kc0: nc.vector.tensor_tensor(out=ckvnT[:, kc0:kc0 + n], in0=ckv_sb[:, 0:n],
                                                                   in1=rstd_kv[:, 0:n], op=ALU.mult),
                 reads=[b_ckvsb, b_rkv], writes=[b_ckvn])
            for ti in range(len(tiles)):
                kt = kc0 // 128 + ti
                pb3 = nextbank()
                T.op(pe, lambda pb3=pb3, kt=kt: nc.tensor.matmul(bank(pb3), lhsT=ckvnT[:, kt * 128:(kt + 1) * 128], rhs=w_v,
                                                                start=True, stop=True), reads=[b_ckvn, b_ph0], writes=[PB[pb3]])
                T.op(act, lambda pb3=pb3, kt=kt: nc.scalar.copy(out=V_ext[:, kt, :, 0:64],
                                                                in_=bank(pb3).rearrange("p (h e) -> p h e", e=64)),
                     reads=[PB[pb3]], writes=[b_V])
            pr = nextbank(); pq = nextbank()

            def mm_kr(pr=pr, pq=pq, W=W, WR=WR, off=off, hT=hT, n=n):
                last = None
                for k in range(8):
                    last = nc.tensor.matmul(bank(pr, n, 96), lhsT=W[:, k, 320 + off:416 + off], rhs=hT[:, k, 0:n],
                                            start=(k == 0), stop=(k == 7))
                for k in range(8):
                    last = nc.tensor.matmul(bank(pq, n, 96), lhsT=WR[:, k, :], rhs=hT[:, k, 0:n],
                                            start=(k == 0), stop=(k == 7))
                return last
            T.op(pe, mm_kr, reads=[hT_b[hb], b_ph0], writes=[PB[pr], PB[pq]])
            T.op(dve, lambda pr=pr, n=n, zj=zj, hb=hb: nc.vector.scalar_tensor_tensor(
                out=t1[64:96, 0:n], in0=bank(pr, n)[64:96], scalar=zb[64:96, zj[1]:zj[1] + 1], in1=cos_ring[hb][64:96, 0:n],
                op0=ALU.add, op1=ALU.mult), reads=[PB[pr], tg_b[hb], b_ph0], writes=[b_t1])
            T.op(dve, lambda pq=pq, n=n, zj=zj, hb=hb: nc.vector.scalar_tensor_tensor(
                out=t2[64:96, 0:n], in0=bank(pq, n)[64:96], scalar=zb[64:96, zj[2]:zj[2] + 1], in1=sin_ring[hb][64:96, 0:n],
                op0=ALU.add, op1=ALU.mult), reads=[PB[pq], tg_b[hb], b_ph0], writes=[b_t2])
            T.op(dve, lambda n=n, kc0=kc0: nc.vector.tensor_tensor(out=krT[64:96, kc0:kc0 + n], in0=t1[64:96, 0:n],
                                                                   in1=t2[64:96, 0:n], op=ALU.add),
                 reads=[b_t1, b_t2], writes=[b_kr])
            if kind != "own":
                continue
            for c in range(2):
                pc = nextbank()

                def mm_cq(pc=pc, c=c, hT=hT):
                    last = None
                    for k in range(8):
                        last = nc.tensor.matmul(bank(pc), lhsT=w_inL[:, k, c * 128:(c + 1) * 128], rhs=hT[:, k, :],
                                                start=(k == 0), stop=(k == 7))
                    return last
                T.op(pe, mm_cq, reads=[hT_b[hb], b_ph0], writes=[PB[pc]])
                T.op(act, lambda pc=pc, c=c: nc.scalar.activation(out=cq_sb[:, c, :], in_=bank(pc), func=AF.Identity,
                                                                  bias=zb[:, c:c + 1]), reads=[PB[pc], b_ph0], writes=[b_cqsb])
                T.op(act, lambda c=c: nc.scalar.activation(out=sq_q[:, c, :], in_=cq_sb[:, c, :], func=AF.Square),
                     reads=[b_cqsb], writes=[b_sqq])
            pss = nextbank()

            def mm_ssq(pss=pss):
                nc.tensor.matmul(bank(pss), lhsT=onesF[:], rhs=sq_q[:, 0, :], start=True, stop=False)
                return nc.tensor.matmul(bank(pss), lhsT=onesF[:], rhs=sq_q[:, 1, :], start=False, stop=True)
            T.op(pe, mm_ssq, reads=[b_sqq, b_const], writes=[PB[pss]])
            rstd_bcast(pss, 512, rstd_q, 1.0 / 256, [PB[pss]], [b_rq])
            for c in range(2):
                T.op(dve, lambda c=c: nc.vector.tensor_tensor(out=cqn[:, c, :], in0=cq_sb[:, c, :], in1=rstd_q,
                                                              op=ALU.mult), reads=[b_cqsb, b_rq], writes=[b_cqn])
            for h in range(H):
                pr = nextbank(); pq = nextbank()

                def mm_q(pr=pr, pq=pq, h=h):
                    last = None
                    for c in range(2):
                        last = nc.tensor.matmul(bank(pr, 512, 96), lhsT=w_uqS[:, c, h * 96:(h + 1) * 96], rhs=cqn[:, c, :],
                                                start=(c == 0), stop=(c == 1))
                    for c in range(2):
                        last = nc.tensor.matmul(bank(pq, 512, 96), lhsT=w_uqR[:, c, h * 96:(h + 1) * 96], rhs=cqn[:, c, :],
                                                start=(c == 0), stop=(c == 1))
                    return last
                T.op(pe, mm_q, reads=[b_cqn, b_ph0], writes=[PB[pr], PB[pq]])
                T.op(act, lambda pr=pr, h=h, kc0=kc0: nc.scalar.copy(out=QT_all[0:64, h, kc0:kc0 + 512], in_=bank(pr)[0:64]),
                     reads=[PB[pr]], writes=[b_QT])
                T.op(dve, lambda pr=pr, hb=hb: nc.vector.tensor_tensor(out=t1[64:96, :], in0=bank(pr)[64:96],
                                                                       in1=cos_ring[hb][64:96, :], op=ALU.mult),
                     reads=[PB[pr], tg_b[hb]], writes=[b_t1])
                T.op(dve, lambda pq=pq, hb=hb: nc.vector.tensor_tensor(out=t2[64:96, :], in0=bank(pq)[64:96],
                                                                       in1=sin_ring[hb][64:96, :], op=ALU.mult),
                     reads=[PB[pq], tg_b[hb]], writes=[b_t2])
                T.op(dve, lambda h=h, kc0=kc0: nc.vector.tensor_tensor(out=QT_all[64:96, h, kc0:kc0 + 512], in0=t1[64:96, :],
                                                                       in1=t2[64:96, :], op=ALU.add),
                     reads=[b_t1, b_t2], writes=[b_QT])
        T.barrier()

        KTs = [AR.view((88 + 9 * i) * KB, [96, NK], BF16) for i in range(2)]
        PT = [AR.view((106 + 2 * i) * KB, [128, 1024], BF16) for i in range(3)]
        att_tok = AR.view(112 * KB, [128, 16, 512], BF16)
        catT = AR.view(128 * KB, [128, 8, NQ], BF16)
        b_KT = [Buf("KT0"), Buf("KT1")]
        b_PT = [Buf(f"PT{i}") for i in range(3)]
        b_att = Buf("att"); b_cat = Buf("cat"); b_rc = Buf("rc")
        rc = smallA

        for i in range(2):
            T.op(pool, lambda i=i: nc.gpsimd.tensor_copy(out=KTs[i][64:96, :], in_=krT[64:96, :]), reads=[b_kr], writes=[b_KT[i]])

        kgroups = [(g * 512, 512) for g in range(8)] + [(4096, 256)]

        def build_KT(h):
            for gi2, (k0, n) in enumerate(kgroups):
                pb = 6 + (gi2 % 2)
                T.op(pe, lambda pb=pb, k0=k0, n=n: nc.tensor.matmul(bank(pb, n, 64), lhsT=w_kn[:, h, :], rhs=ckvnT[:, k0:k0 + n],
                                                                    start=True, stop=True), reads=[b_ckvn, b_ph0], writes=[PB[pb]])
                T.op(dve, lambda pb=pb, k0=k0, n=n: nc.vector.tensor_copy(out=KTs[h % 2][0:64, k0:k0 + n], in_=bank(pb, n, 64)),
                     reads=[PB[pb]], writes=[b_KT[h % 2]])

        build_KT(0)
        it = 0
        for h in range(H):
            if h + 1 < H:
                build_KT(h + 1)
            KT = KTs[h % 2]
            for qg in range(2):
                q0 = qg * 1024

                def S_mm(kt, sb_):
                    def f():
                        nc.tensor.matmul(bank(sb_), lhsT=KT[:, kt * 128:(kt + 1) * 128], rhs=QT_all[:, h, q0:q0 + 512],
                                         start=True, stop=True)
                        return nc.tensor.matmul(bank(sb_ + 1), lhsT=KT[:, kt * 128:(kt + 1) * 128],
                                                rhs=QT_all[:, h, q0 + 512:q0 + 1024], start=True, stop=True)
                    T.op(pe, f, reads=[b_KT[h % 2], b_QT], writes=[PB[sb_], PB[sb_ + 1]])

                S_mm(0, 0)
                for kt in range(NKT):
                    sb_ = 2 * (kt % 2)
                    if kt + 1 < NKT:
                        S_mm(kt + 1, 2 * ((kt + 1) % 2))
                    pi = it % 3; it += 1
                    T.op(act, lambda sb_=sb_, pi=pi: nc.scalar.activation(out=PT[pi], in_=bank(sb_, nb=2), func=AF.Exp, scale=SCALE),
                         reads=[PB[sb_], PB[sb_ + 1]], writes=[b_PT[pi]])

                    def PV(kt=kt, pi=pi):
                        last = None
                        for j in range(8):
                            ob = 4 + j // 4
                            last = nc.tensor.matmul(bank(ob)[:, (j % 4) * 65:(j % 4) * 65 + 65],
                                                    lhsT=PT[pi][:, j * 128:(j + 1) * 128], rhs=V_ext[:, kt, h, :],
                                                    start=(kt == 0 and j % 4 == 0), stop=(kt == NKT - 1),
                                                    skip_group_check=True)
                        return last
                    T.op(pe, PV, reads=[b_PT[pi], b_V], writes=[PB[4], PB[5]])
                for ob in (4, 5):
                    ov = bank(ob, 260).rearrange("p (s e) -> p s e", e=65)
                    T.op(dve, lambda ov=ov, ob=ob: nc.vector.reciprocal(out=rc[:, (ob - 4) * 4:(ob - 4) * 4 + 4], in_=ov[:, :, 64]),
                         reads=[PB[ob]], writes=[b_rc])
                    for s in range(4):
                        j = (ob - 4) * 4 + s
                        T.op(dve, lambda ov=ov, s=s, j=j: nc.vector.tensor_scalar(
                            out=att_tok[:, qg * 8 + j, h * 64:(h + 1) * 64], in0=ov[:, s, 0:64], scalar1=rc[:, j:j + 1],
                            scalar2=None, op0=ALU.mult), reads=[PB[ob], b_rc], writes=[b_att])
        for qt in range(16):
            tb = 6 + (qt % 2)
            tpv = bank(tb).bitcast(BF16)

            def tr(qt=qt, tpv=tpv):
                last = None
                for c in range(4):
                    last = nc.tensor.transpose(out=tpv[:, c * 128:(c + 1) * 128], in_=att_tok[:, qt, c * 128:(c + 1) * 128],
                                               identity=identB[:])
                return last
            T.op(pe, tr, reads=[b_att, b_const], writes=[PB[tb]])
            T.op(dve, lambda qt=qt, tpv=tpv: nc.vector.tensor_copy(out=catT[:, 0:4, qt * 128:(qt + 1) * 128],
                                                                   in_=tpv[:, 0:512].rearrange("p (c t) -> p c t", t=128)),
                 reads=[PB[tb]], writes=[b_cat])
        T.barrier()

        x_ring = [AR.view(4 * i * KB, [128, D], F32) for i in range(3)]
        xn_ring = [AR.view((12 + 2 * i) * KB, [128, D], BF16) for i in range(2)]
        hT_ring = [AR.view((16 + 8 * i) * KB, [128, 8, 512], BF16) for i in range(2)]
        w_inU = AR.view(32 * KB, [128, 8, 512], BF16)
        NU = NQ + 16
        uT = AR.view(40 * KB, [128, 4, NU], F32)
        tmpA = AR.view(73 * KB, [128, NU], F32)
        tmpB = AR.view(82 * KB, [128, NU], F32)
        mixB = [AR.view((91 + 4 * i) * KB, [128, NQ], BF16) for i in range(2)]
        w_poolB = AR.view(99 * KB, [128, 4, 128], BF16)
        winu_f = AR.view(100 * KB, [128, 8, 512], F32)
        b_wu = Buf("wu"); b_uT = Buf("uT"); b_tA = Buf("tA"); b_tB = Buf("tB"); b_mix = [Buf("mix0"), Buf("mix1")]
        ldP = T.mksem("ldP")
        b_ldP = Buf("ldP")
        T.dma(sp, winu_f, w_in.rearrange("(k q) f -> q k f", q=128)[:, :, 416:928], ldP, writes=[b_ldP])
        T.dma(pool, w_poolB, w_pool.rearrange("g c d -> c g d"), ldP, writes=[b_ldP])
        for k in range(8):
            T.op(dve, lambda k=k: nc.vector.tensor_scalar(out=w_inU[:, k, :], in0=winu_f[:, k, :], scalar1=G1L[:, k:k + 1],
                                                          scalar2=None, op0=ALU.mult), reads=[b_ldP, b_ph0], writes=[b_wu])
        pgroups = [("halo", [16, 17])] + [("own", [4 * g + i for i in range(4)]) for g in range(4)]
        og = 0
        def norm_group_p(gi):
            for ti, t in enumerate(pgroups[gi][1]):
                norm_tile_to_hT(x_ring, xn_ring, t, hT_ring[gi % 2], hT_b[gi % 2], ti * 128, (0, 1))

        norm_group_p(0)
        for gi, (kind, tiles) in enumerate(pgroups):
            n = 128 * len(tiles)
            hb = gi % 2
            hT = hT_ring[hb]
            if gi + 1 < len(pgroups):
                norm_group_p(gi + 1)
            for g in range(4):
                pb = nextbank()

                def mm_u(pb=pb, g=g, hT=hT, n=n):
                    last = None
                    for k in range(8):
                        last = nc.tensor.matmul(bank(pb, n), lhsT=w_inU[:, k, g * 128:(g + 1) * 128], rhs=hT[:, k, 0:n],
                                                start=(k == 0), stop=(k == 7))
                    return last
                T.op(pe, mm_u, reads=[hT_b[hb], b_wu], writes=[PB[pb]])
                if kind == "own":
                    c0 = 8 + og * 512
                    T.op(act, lambda pb=pb, g=g, c0=c0: nc.scalar.activation(out=uT[:, g, c0:c0 + 512], in_=bank(pb),
                                                                             func=AF.Identity, bias=zb[:, 5 + g:6 + g]),
                         reads=[PB[pb], b_ph0], writes=[b_uT])
                else:
                    T.op(dve, lambda pb=pb, g=g: nc.vector.scalar_tensor_tensor(
                        out=uT[:, g, 0:8], in0=bank(pb)[:, 120:128], scalar=zb[:, 5 + g:6 + g], in1=hmask[:, 0:8],
                        op0=ALU.add, op1=ALU.mult), reads=[PB[pb], b_ph0, b_const], writes=[b_uT])
                    T.op(dve, lambda pb=pb, g=g: nc.vector.scalar_tensor_tensor(
                        out=uT[:, g, NU - 8:NU], in0=bank(pb)[:, 128:136], scalar=zb[:, 5 + g:6 + g], in1=hmask[:, 8:16],
                        op0=ALU.add, op1=ALU.mult), reads=[PB[pb], b_ph0, b_const], writes=[b_uT])
            if kind == "own":
                og += 1

        def tt_add(e, o, a, b_, rb, wb):
            if e is pool:
                T.op(pool, lambda: nc.gpsimd.tensor_tensor(out=o, in0=a, in1=b_, op=ALU.add), reads=rb, writes=wb)
            else:
                T.op(dve, lambda: nc.vector.tensor_tensor(out=o, in0=a, in1=b_, op=ALU.add), reads=rb, writes=wb)

        LO, HI = 8, 8 + NQ
        for g in range(4):
            U = uT[:, g, :]
            wwin = (2, 4, 8, 16)[g]
            eng = dve
            if g == 0:
                tt_add(eng, tmpA[:, LO:HI], U[:, LO - 1:HI - 1], U[:, LO:HI], [b_uT], [b_tA]); S = tmpA; bS = b_tA
            else:
                tt_add(eng, tmpA[:, 1:NU], U[:, 0:NU - 1], U[:, 1:NU], [b_uT], [b_tA])
                if g == 1:
                    tt_add(eng, tmpB[:, LO:HI], tmpA[:, LO - 1:HI - 1], tmpA[:, LO + 1:HI + 1], [b_tA], [b_tB]); S = tmpB; bS = b_tB
                else:
                    tt_add(eng, tmpB[:, 2:NU - 1], tmpA[:, 1:NU - 2], tmpA[:, 3:NU], [b_tA], [b_tB])
                    if g == 2:
                        tt_add(eng, tmpA[:, LO:HI], tmpB[:, LO - 2:HI - 2], tmpB[:, LO + 2:HI + 2], [b_tB], [b_tA]); S = tmpA; bS = b_tA
                    else:
                        tt_add(eng, tmpA[:, 4:NU - 3], tmpB[:, 2:NU - 5], tmpB[:, 6:NU - 1], [b_tB], [b_tA])
                        tt_add(eng, tmpB[:, LO:HI], tmpA[:, LO - 4:HI - 4], tmpA[:, LO + 4:HI + 4], [b_tA], [b_tB]); S = tmpB; bS = b_tB
            mb = mixB[g % 2]
            T.op(dve, lambda S=S, U=U, mb=mb, wwin=wwin: nc.vector.scalar_tensor_tensor(
                out=mb, in0=S[:, LO:HI], scalar=1.0 / wwin, in1=U[:, LO:HI], op0=ALU.mult, op1=ALU.subtract),
                reads=[bS, b_uT], writes=[b_mix[g % 2]])
            for (a0, i0) in ((0, 0), (NQ - 8, 8)):
                T.op(dve, lambda S=S, a0=a0, i0=i0, g=g: nc.vector.tensor_tensor(
                    out=smallA[:, 16:24], in0=S[:, LO + a0:LO + a0 + 8], in1=invc[:, g, i0:i0 + 8], op=ALU.mult),
                    reads=[bS, b_const], writes=[b_rc])
                T.op(dve, lambda U=U, a0=a0, mb=mb: nc.vector.tensor_tensor(
                    out=mb[:, a0:a0 + 8], in0=smallA[:, 16:24], in1=U[:, LO + a0:LO + a0 + 8], op=ALU.subtract),
                    reads=[b_rc, b_uT], writes=[b_mix[g % 2]])
            for tg in range(4):
                pb = nextbank()
                T.op(pe, lambda pb=pb, g=g, mb=mb, tg=tg: nc.tensor.matmul(bank(pb), lhsT=w_poolB[:, g, :],
                                                                          rhs=mb[:, tg * 512:(tg + 1) * 512], start=True, stop=True),
                     reads=[b_mix[g % 2], b_ldP], writes=[PB[pb]])
                T.op(act, lambda pb=pb, g=g, tg=tg: nc.scalar.activation(out=catT[:, 4 + g, tg * 512:(tg + 1) * 512], in_=bank(pb),
                                                                         func=AF.Copy, scale=pscol[:, g:g + 1]),
                     reads=[PB[pb], b_const], writes=[b_cat])
        T.barrier()
        w_outS = AR.view(0, [128, 8, D], BF16)
        wo_stg = [AR.view((16 + 4 * i) * KB, [128, D], F32) for i in range(2)]
        x2_ring = [AR.view((24 + 4 * i) * KB, [128, D], F32) for i in range(2)]
        xnew = [AR.view((32 + 4 * i) * KB, [128, D], F32) for i in range(2)]
        xn2 = [AR.view((40 + 4 * i) * KB, [128, D], F32) for i in range(2)]
        h2Tf = [AR.view((48 + 4 * i) * KB, [128, 8, 128], F32) for i in range(2)]
        h2tok = [AR.view((56 + 2 * i) * KB, [128, D], BF16) for i in range(2)]
        G2B = AR.view(60 * KB, [128, D], F32)
        sh2B = AR.view(64 * KB, [128, D], F32)
        g1B = AR.view(68 * KB, [128, D], F32)
        tmpF = AR.view(72 * KB, [128, D], F32)
        b_wo = Buf("wo"); b_g1B = Buf("g1B"); b_GB = Buf("GB")
        wo_sem = [T.mksem(f"wo{i}") for i in range(2)]; b_wos = [Buf("wos0"), Buf("wos1")]
        x2_sem = [T.mksem(f"x2{i}") for i in range(2)]; b_x2 = [Buf("x20"), Buf("x21")]
        b_xnew = [Buf("xnew0"), Buf("xnew1")]
        b_xn2 = [Buf("xn20"), Buf("xn21")]; b_h2Tf = [Buf("h2Tf0"), Buf("h2Tf1")]
        b_h2tok = [Buf("h2tok0"), Buf("h2tok1")]; b_tmpF = Buf("tmpF")
        h2_sem = [T.mksem(f"h2s{i}") for i in range(2)]
        xw_sem = [T.mksem(f"xw{i}") for i in range(2)]
        sc_sem = T.mksem("scat")
        b_sm = Buf("sm"); b_rt = Buf("rt"); b_cum = Buf("cum")

        logit = sb("logit", [128, NE]); m8 = sb("m8", [128, 8]); idx8 = sb("idx8", [128, 8], U32)
        e4 = sb("e4", [128, 4]); gk4 = sb("gk4", [128, 4]); ekf = sb("ekf", [128, 4]); rkk = sb("rkk", [128, 4])
        slotf = sb("slotf", [128, 4]); gsum = sb("gsum", [128, 4]); gsumF = sb("gsumF", [128, 2])
        oh = sb("oh", [128, 4, NE]); Mb = sb("Mb", [128, NE], BF16); M2 = sb("M2", [128, 2, NE])
        gd = sb("gd", [128, NE]); rk = sb("rk", [128, NE]); cum = sb("cum", [128, NE]); junk32 = sb("junk32", [128, NE])
        gT = sb("gT", [32, 128])
        gk_all = sb("gk_all", [128, 16, 4]); slot_all = sb("slot_all", [128, 16, 4], I32)
        cnt_i = sb("cnt_i", [128, NE + 1], I32); cmaxf = sb("cmaxf", [128, 1])

        T.op(dve, lambda: nc.vector.memset(cum[:], 0.0), writes=[b_cum])
        for hf in range(2):
            for (row, dst, bb) in ((g1row[0:1, :], g1B, b_g1B), (rows2[0:1, 0, :], G2B, b_GB), (rows2[0:1, 1, :], sh2B, b_GB)):
                pb = nextbank()
                T.op(pe, lambda pb=pb, hf=hf, row=row: nc.tensor.matmul(bank(pb), lhsT=onesF[0:1, :], rhs=row[:, hf * 512:(hf + 1) * 512],
                                                                        start=True, stop=True), reads=[b_ph0, b_const], writes=[PB[pb]])
                T.op(dve, lambda pb=pb, hf=hf, dst=dst: nc.vector.tensor_copy(out=dst[:, hf * 512:(hf + 1) * 512], in_=bank(pb)),
                     reads=[PB[pb]], writes=[bb])
        for k in range(8):
            s = k % 2
            T.dma(sp, wo_stg[s], w_out[k * 128:(k + 1) * 128, :], wo_sem[s], writes=[b_wos[s]])
            T.op(dve, lambda k=k, s=s: nc.vector.tensor_tensor(out=w_outS[:, k, :], in0=wo_stg[s], in1=g1B, op=ALU.mult),
                 reads=[b_wos[s], b_g1B], writes=[b_wo])

        b_smF = Buf("smF")

        def front_2a(qt):
            s = qt % 2
            T.dma(sp, x2_ring[s], xe[qt * 128:(qt + 1) * 128, :], x2_sem[s], writes=[b_x2[s]])
            for hf in range(2):
                pb = nextbank()

                def mm_o(pb=pb, hf=hf, qt=qt):
                    last = None
                    for k in range(8):
                        last = nc.tensor.matmul(bank(pb), lhsT=catT[:, k, qt * 128:(qt + 1) * 128],
                                                rhs=w_outS[:, k, hf * 512:(hf + 1) * 512], start=(k == 0), stop=(k == 7))
                    return last
                T.op(pe, mm_o, reads=[b_cat, b_wo], writes=[PB[pb]])
                T.op(dve, lambda pb=pb, hf=hf, s=s: nc.vector.tensor_tensor(
                    out=xnew[s][:, hf * 512:(hf + 1) * 512], in0=bank(pb), in1=x2_ring[s][:, hf * 512:(hf + 1) * 512], op=ALU.add),
                    reads=[PB[pb], b_x2[s]], writes=[b_xnew[s]])
            T.op(act, lambda s=s: nc.scalar.activation(out=junk[:], in_=xnew[s], func=AF.Square, accum_out=gsumF[:, 0:1]),
                 reads=[b_xnew[s]], writes=[b_smF])
            T.op(act, lambda: nc.scalar.activation(out=gsumF[:, 1:2], in_=gsumF[:, 0:1], func=AF.Sqrt, scale=1.0 / D,
                                                   bias=epsT[:]), reads=[b_smF], writes=[b_smF])
            T.op(dve, lambda: nc.vector.reciprocal(out=gsumF[:, 1:2], in_=gsumF[:, 1:2]), reads=[b_smF], writes=[b_smF])
            T.op(dve, lambda s=s: nc.vector.scalar_tensor_tensor(out=tmpF, in0=xnew[s], scalar=gsumF[:, 1:2], in1=G2B,
                                                                 op0=ALU.mult, op1=ALU.mult),
                 reads=[b_xnew[s], b_smF, b_GB], writes=[b_tmpF])
            T.op(dve, lambda s=s: nc.vector.tensor_tensor(out=xn2[s], in0=tmpF, in1=sh2B, op=ALU.add),
                 reads=[b_tmpF, b_GB], writes=[b_xn2[s]])
            T.op(act, lambda s=s: nc.scalar.copy(out=h2tok[s], in_=xn2[s]), reads=[b_xn2[s]], writes=[b_h2tok[s]])
            T.dma(act, h2d[qt * 128:(qt + 1) * 128, :], h2tok[s], h2_sem[s], reads=[b_h2tok[s]])

            def tr2(s=s):
                last = None
                for c in range(8):
                    last = nc.tensor.transpose(out=bank(c // 4)[:, (c % 4) * 128:(c % 4) * 128 + 128],
                                               in_=xn2[s][:, c * 128:(c + 1) * 128], identity=identF[:])
                return last
            T.op(pe, tr2, reads=[b_xn2[s], b_const], writes=[PB[0], PB[1]])
            T.op(act, lambda s=s: nc.scalar.copy(out=h2Tf[s], in_=bank(0, nb=2).rearrange("p (c t) -> p c t", t=128)),
                 reads=[PB[0], PB[1]], writes=[b_h2Tf[s]])

        def tail_2a(qt):
            s = qt % 2
            pbr = nextbank()

            def mm_r(pbr=pbr, s=s):
                last = None
                for c in range(8):
                    last = nc.tensor.matmul(bank(pbr, NE), lhsT=h2Tf[s][:, c, :], rhs=rwt[:, c, :], start=(c == 0), stop=(c == 7))
                return last
            T.op(pe, mm_r, reads=[b_h2Tf[s], b_const], writes=[PB[pbr]])
            T.op(dve, lambda pbr=pbr: nc.vector.tensor_tensor(out=logit[:], in0=bank(pbr, NE), in1=rbB[:], op=ALU.add),
                 reads=[PB[pbr], b_const], writes=[b_rt])
            T.op(dve, lambda: nc.vector.max(out=m8[:], in_=logit[:]), reads=[b_rt], writes=[b_rt])
            T.op(dve, lambda: nc.vector.max_index(out=idx8[:], in_max=m8[:], in_values=logit[:]), reads=[b_rt], writes=[b_rt])
            T.op(dve, lambda: nc.vector.tensor_scalar(out=gsum[:, 2:3], in0=m8[:, 0:1], scalar1=-1.0, scalar2=None,
                                                      op0=ALU.mult), reads=[b_rt, b_sm], writes=[b_sm])
            T.op(act, lambda: nc.scalar.activation(out=e4[:], in_=m8[:, 0:4], func=AF.Exp, bias=gsum[:, 2:3]),
                 reads=[b_rt, b_sm], writes=[b_rt])
            T.op(dve, lambda: nc.vector.tensor_reduce(out=gsum[:, 3:4], in_=e4[:], axis=mybir.AxisListType.X, op=ALU.add),
                 reads=[b_rt, b_sm], writes=[b_sm])
            T.op(dve, lambda: nc.vector.reciprocal(out=gsum[:, 3:4], in_=gsum[:, 3:4]), reads=[b_sm], writes=[b_sm])
            T.op(dve, lambda: nc.vector.tensor_scalar(out=gk4[:], in0=e4[:], scalar1=gsum[:, 3:4], scalar2=None,
                                                      op0=ALU.mult), reads=[b_rt, b_sm], writes=[b_rt])
            T.op(dve, lambda qt=qt: nc.vector.tensor_scalar(out=gk_all[:, qt, :], in0=gk4[:], scalar1=1.0 / 1.702, scalar2=None,
                                                            op0=ALU.mult), reads=[b_rt], writes=[b_rt])
            T.op(dve, lambda: nc.vector.tensor_copy(out=ekf[:], in_=idx8[:, 0:4]), reads=[b_rt], writes=[b_rt])
            for k in range(4):
                T.op(dve, lambda k=k: nc.vector.tensor_scalar(out=oh[:, k, :], in0=iota32[:], scalar1=ekf[:, k:k + 1], scalar2=None,
                                                              op0=ALU.is_equal), reads=[b_rt, b_const], writes=[b_rt])
            T.op(dve, lambda: nc.vector.tensor_tensor(out=M2[:], in0=oh[:, 0:2, :], in1=oh[:, 2:4, :], op=ALU.add),
                 reads=[b_rt], writes=[b_rt])
            T.op(dve, lambda: nc.vector.tensor_tensor(out=Mb[:], in0=M2[:, 0, :], in1=M2[:, 1, :], op=ALU.add),
                 reads=[b_rt], writes=[b_rt])
            T.op(dve, lambda: nc.vector.tensor_scalar(out=gd[:], in0=oh[:, 0, :], scalar1=gk4[:, 0:1], scalar2=None, op0=ALU.mult),
                 reads=[b_rt], writes=[b_rt])
            for k in range(1, 4):
                T.op(dve, lambda k=k: nc.vector.scalar_tensor_tensor(out=gd[:], in0=oh[:, k, :], scalar=gk4[:, k:k + 1], in1=gd[:],
                                                                     op0=ALU.mult, op1=ALU.add), reads=[b_rt], writes=[b_rt])
            pbt = nextbank()
            T.op(pe, lambda pbt=pbt: nc.tensor.transpose(out=bank(pbt, 128, 32), in_=gd[:], identity=identF[:]),
                 reads=[b_rt, b_const], writes=[PB[pbt]])
            T.op(act, lambda pbt=pbt: nc.scalar.copy(out=gT[:], in_=bank(pbt, 128, 32)), reads=[PB[pbt]], writes=[b_sm])
            for hf in range(2):
                pb = nextbank()
                T.op(pe, lambda pb=pb, hf=hf: nc.tensor.matmul(bank(pb), lhsT=gT[:], rhs=bdS[:, hf * 512:(hf + 1) * 512],
                                                               start=True, stop=True), reads=[b_sm, b_ph0], writes=[PB[pb]])
                T.op(dve, lambda pb=pb, hf=hf, s=s: nc.vector.tensor_tensor(
                    out=xnew[s][:, hf * 512:(hf + 1) * 512], in0=bank(pb), in1=xnew[s][:, hf * 512:(hf + 1) * 512], op=ALU.add),
                    reads=[PB[pb], b_xnew[s]], writes=[b_xnew[s]])
            T.dma(act, xnew_d[qt * 128:(qt + 1) * 128, :], xnew[s], xw_sem[s], reads=[b_xnew[s]])
            pbk = nextbank(); pbc = nextbank()
            T.op(pe, lambda pbk=pbk: nc.tensor.matmul(bank(pbk, NE), lhsT=UstrB[:], rhs=Mb[:], start=True, stop=True),
                 reads=[b_rt, b_c2], writes=[PB[pbk]])
            T.op(pe, lambda pbc=pbc: nc.tensor.matmul(bank(pbc, NE), lhsT=onesB[:], rhs=Mb[:], start=True, stop=True),
                 reads=[b_rt, b_c2], writes=[PB[pbc]])
            T.op(dve, lambda pbk=pbk: nc.vector.tensor_tensor(out=rk[:], in0=bank(pbk, NE), in1=cum[:], op=ALU.add),
                 reads=[PB[pbk], b_cum], writes=[b_rt])
            T.op(dve, lambda pbc=pbc: nc.vector.tensor_tensor(out=cum[:], in0=bank(pbc, NE), in1=cum[:], op=ALU.add),
                 reads=[PB[pbc], b_cum, b_rt], writes=[b_cum])
            for k in range(4):
                T.op(dve, lambda k=k: nc.vector.scalar_tensor_tensor(out=junk32[:], in0=oh[:, k, :], scalar=1.0, in1=rk[:],
                                                                     op0=ALU.mult, op1=ALU.mult, accum_out=rkk[:, k:k + 1]),
                     reads=[b_rt], writes=[b_rt])
            T.op(dve, lambda: nc.vector.scalar_tensor_tensor(out=slotf[:], in0=ekf[:], scalar=2048.0, in1=rkk[:],
                                                             op0=ALU.mult, op1=ALU.add), reads=[b_rt], writes=[b_rt])
            b_slot = Buf("slot")
            T.op(dve, lambda qt=qt: nc.vector.tensor_copy(out=slot_all[:, qt, :], in_=slotf[:]), reads=[b_rt], writes=[b_slot])
            for k in range(4):
                T._waits(pool, T._deps([b_slot, b_tokl0, b_const], []))
                inst = nc.gpsimd.indirect_dma_start(out=tokl, out_offset=bass.IndirectOffsetOnAxis(ap=slot_all[:, qt, k:k + 1], axis=0),
                                                    in_=tokrow[:, qt, :], in_offset=None)
                sc_sem.cnt += 16
                inst.then_inc(sc_sem.h, 16)
                T._update((sc_sem, sc_sem.cnt), [b_slot, b_tokl0, b_const], [])

        front_2a(0)
        for qt in range(16):
            if qt + 1 < 16:
                front_2a(qt + 1)
            tail_2a(qt)
        nt_sem = T.mksem("nts")
        b_nt = Buf("nt")
        cntf = sb("cntf", [128, NE + 1])
        T.op(dve, lambda: nc.vector.tensor_reduce(out=cmaxf[:], in_=cum[:], axis=mybir.AxisListType.X, op=ALU.max),
             reads=[b_cum], writes=[b_nt])
        T.op(dve, lambda: nc.vector.tensor_scalar(out=cntf[:, 0:NE], in0=cum[:], scalar1=-1.0, scalar2=4096.0,
                                                  op0=ALU.mult, op1=ALU.add), reads=[b_cum, b_nt], writes=[b_nt])
        T.op(dve, lambda: nc.vector.tensor_scalar(out=cntf[:, NE:NE + 1], in0=cmaxf[:], scalar1=-1.0, scalar2=4096.0,
                                                  op0=ALU.mult, op1=ALU.add), reads=[b_nt], writes=[b_nt])
        T.op(dve, lambda: nc.vector.tensor_copy(out=cnt_i[:], in_=cntf[:]), reads=[b_nt], writes=[b_nt])
        T.dma(sp, nt_d, cnt_i[0:1, :], nt_sem, reads=[b_nt], writes=[b_nt])
        T.barrier()

        NS = 3
        Wgu = [AR.view(32 * i * KB, [128, 8, 2 * D], BF16) for i in range(NS)]
        Wdn = [AR.view((96 + 16 * i) * KB, [128, 8, D], BF16) for i in range(NS)]
        bias_reg = AR.view(144 * KB, [128, 2 * D], BF16)
        bgur = [bias_reg[32 * i:32 * i + 1, :] for i in range(NS)]
        xb = [AR.view((148 + 2 * i) * KB, [128, D], BF16) for i in range(2)]
        xbT = [AR.view((152 + 2 * i) * KB, [128, 8, 128], BF16) for i in range(2)]
        Ag = AR.view(156 * KB, [128, D], F32)
        Qs = AR.view(160 * KB, [128, D], BF16)
        Cp = AR.view(162 * KB, [128, D], BF16)
        actS = [AR.view((164 + 2 * i) * KB, [128, D], BF16) for i in range(2)]
        actT = [AR.view((168 + 2 * i) * KB, [128, 8, 128], BF16) for i in range(2)]
        ysb = [AR.view((172 + 4 * i) * KB, [128, D], F32) for i in range(2)]
        idxT = [sb(f"idxT{i}", [128, 2], I32) for i in range(2)]
        w_sem = [T.mksem(f"wg{i}") for i in range(NS)]
        i_sem = [T.mksem(f"is{i}") for i in range(2)]; b_idx = [Buf("idx0"), Buf("idx1")]
        g_sem = [T.mksem(f"gs{i}") for i in range(2)]; b_xb = [Buf("xb0"), Buf("xb1")]
        y_sem = [T.mksem(f"ys{i}") for i in range(2)]; b_ysb = [Buf("ysb0"), Buf("ysb1")]
        b_xbT = [Buf("xbT0"), Buf("xbT1")]; b_Ag = Buf("Ag"); b_Qs = Buf("Qs"); b_Cp = Buf("Cp")
        b_actS = [Buf("actS0"), Buf("actS1")]; b_actT = [Buf("actT0"), Buf("actT1")]
        owners = {}
        for i in range(2):
            owners[i_sem[i]] = sp; owners[y_sem[i]] = act; owners[g_sem[i]] = pool
        for i in range(NS):
            owners[w_sem[i]] = pool

        b_Wc = [[Buf(f"Wc{i}_{c}") for c in range(13)] for i in range(NS)]

        def load_chunks(e, lo, hi):
            s = e % NS
            for ci in range(lo, hi):
                if ci < 8:
                    T.dma(pool, Wgu[s][:, ci, :], w_gu[e][ci * 128:(ci + 1) * 128, :], w_sem[s], writes=[b_Wc[s][ci]])
                else:
                    k0 = (ci - 8) * 2
                    T.dma(pool, Wdn[s][:, k0:k0 + 2, :], w_dn[e].rearrange("(k q) n -> q k n", q=128)[:, k0:k0 + 2, :],
                          w_sem[s], writes=[b_Wc[s][ci]])
                if ci == 0:
                    T.dma(pool, bgur[s], b_gu[e:e + 1, :], w_sem[s], writes=[b_Wc[s][12]])

        for eng in T.engs:
            T._waits(eng, {nt_sem: nt_sem.cnt})
        cnt_regs = []
        for e in range(NE + 1):
            rs = nc.alloc_registers(f"cnt{e}")
            for reg in rs:
                nc.reg_load(reg, nt_d[0:1, e:e + 1])
            cnt_regs.append(rs)

        def piece_A(e, j, s, u):
            r0 = e * 2048 + j * 128
            T.dma(sp, idxT[u][:], tokl[r0:r0 + 128, :], i_sem[u], writes=[b_idx[u]])
            T._waits(pool, T._deps([b_idx[u]], [b_xb[u]]))
            inst = nc.gpsimd.indirect_dma_start(out=xb[u], out_offset=None, in_=h2d,
                                                in_offset=bass.IndirectOffsetOnAxis(ap=idxT[u][:, 0:1], axis=0))
            g_sem[u].cnt += 16
            inst.then_inc(g_sem[u].h, 16)
            T._update((g_sem[u], g_sem[u].cnt), [b_idx[u]], [b_xb[u]])
            tp0 = bank(0).bitcast(BF16)

            def tr_in():
                last = None
                for c in range(8):
                    last = nc.tensor.transpose(out=tp0[:, c * 128:(c + 1) * 128], in_=xb[u][:, c * 128:(c + 1) * 128],
                                               identity=identB[:])
                return last
            T.op(pe, tr_in, reads=[b_xb[u], b_const], writes=[PB[0]])
            T.op(dve, lambda: nc.vector.tensor_copy(out=xbT[u], in_=tp0.rearrange("p (c t) -> p c t", t=128)),
                 reads=[PB[0]], writes=[b_xbT[u]])

            def mm_gu(c0):
                last = None
                for c in range(c0, c0 + 2):
                    nc.tensor.matmul(bank(1 + c), lhsT=onesB[32 * s:32 * s + 1, :], rhs=bgur[s][:, c * 512:(c + 1) * 512],
                                     start=True, stop=False)
                    for k in range(8):
                        last = nc.tensor.matmul(bank(1 + c), lhsT=xbT[u][:, k, :], rhs=Wgu[s][:, k, c * 512:(c + 1) * 512],
                                                start=False, stop=(k == 7))
                return last
            T.op(pe, lambda: mm_gu(0), reads=[b_xbT[u], b_c2] + b_Wc[s], writes=[PB[1], PB[2]])
            T.op(dve, lambda: nc.vector.tensor_scalar(out=Ag, in0=bank(1, nb=2), scalar1=7.0, scalar2=None, op0=ALU.min),
                 reads=[PB[1], PB[2]], writes=[b_Ag])
            T.op(pe, lambda: mm_gu(2), reads=[b_xbT[u], b_c2] + b_Wc[s], writes=[PB[3], PB[4]])
            T.op(act, lambda: nc.scalar.activation(out=Qs, in_=Ag, func=AF.Silu, scale=1.702), reads=[b_Ag], writes=[b_Qs])

        def piece_B(e, j, s, u):
            T.op(dve, lambda: nc.vector.tensor_scalar(out=Cp, in0=bank(3, nb=2), scalar1=1.0, scalar2=8.0,
                                                      op0=ALU.add, op1=ALU.min), reads=[PB[3], PB[4]], writes=[b_Cp])
            T.op(dve, lambda: nc.vector.scalar_tensor_tensor(out=actS[u], in0=Cp, scalar=-6.0, in1=Qs,
                                                             op0=ALU.max, op1=ALU.mult),
                 reads=[b_Cp, b_Qs], writes=[b_actS[u]])

        def piece_C(e, j, s, u):
            tp5 = bank(5).bitcast(BF16)

            def tr_act():
                last = None
                for c in range(8):
                    last = nc.tensor.transpose(out=tp5[:, c * 128:(c + 1) * 128], in_=actS[u][:, c * 128:(c + 1) * 128],
                                               identity=identB[:])
                return last
            T.op(pe, tr_act, reads=[b_actS[u], b_const], writes=[PB[5]])
            T.op(dve, lambda: nc.vector.tensor_copy(out=actT[u], in_=tp5.rearrange("p (c t) -> p c t", t=128)),
                 reads=[PB[5]], writes=[b_actT[u]])

        def piece_D(e, j, s, u):
            r0 = e * 2048 + j * 128

            def mm_dn():
                last = None
                for hf in range(2):
                    for k in range(8):
                        last = nc.tensor.matmul(bank(6 + hf), lhsT=actT[u][:, k, :], rhs=Wdn[s][:, k, hf * 512:(hf + 1) * 512],
                                                start=(k == 0), stop=(k == 7))
                return last
            T.op(pe, mm_dn, reads=[b_actT[u]] + b_Wc[s], writes=[PB[6], PB[7]])
            T.op(act, lambda: nc.scalar.copy(out=ysb[u], in_=bank(6, nb=2)), reads=[PB[6], PB[7]], writes=[b_ysb[u]])
            T.dma(act, Yd[r0:r0 + 128, :], ysb[u], y_sem[u], reads=[b_ysb[u]])

        def cond(fn, e, j, s, u):
            snap = T.cond_snapshot()
            with nc.If_lt(cnt_regs[e], 4096 - 128 * j):
                fn(e, j, s, u)
            with nc.Else():
                T.cond_compensate(snap, owners)
            T.cond_restore_seen(snap)

        def expert_tiles(e, s, j0, j1):
            for j in range(j0, j1 + 1):
                if j < j1:
                    cond(piece_A, e, j, s, j % 2)
                if j > j0:
                    cond(piece_C, e, j - 1, s, (j - 1) % 2)
                if j < j1:
                    cond(piece_B, e, j, s, j % 2)
                if j > j0:
                    cond(piece_D, e, j - 1, s, (j - 1) % 2)
                if j0 == 0 and 1 <= j <= 3 and e + 2 < NE and TPP == 4:
                    load_chunks(e + 2, 4 * (j - 1), 4 * j)

        load_chunks(0, 0, 12)
        load_chunks(1, 0, 12)
        for e in range(NE):
            expert_tiles(e, e % NS, 0, TPP)
            if TPP != 4 and e + 2 < NE:
                load_chunks(e + 2, 0, 12)
        for r in range(1, 16 // TPP):
            gsnap = T.cond_snapshot()
            with nc.If_lt(cnt_regs[NE], 4096 - 128 * TPP * r):
                for e in range(NE):
                    esnap = T.cond_snapshot()
                    with nc.If_lt(cnt_regs[e], 4096 - 128 * TPP * r):
                        load_chunks(e, 0, 12)
                        expert_tiles(e, e % NS, TPP * r, TPP * r + TPP)
                    with nc.Else():
                        T.cond_compensate(esnap, owners)
                    T.cond_restore_seen(esnap)
            with nc.Else():
                T.cond_compensate(gsnap, owners)
            T.cond_restore_seen(gsnap)
        T.barrier()

        fgB = AR.view(0, [128, D], F32)
        xa = [AR.view((4 + 4 * i) * KB, [128, D], F32) for i in range(2)]
        yk = [AR.view((12 + 4 * i) * KB, [128, D], F32) for i in range(4)]
        tacc = AR.view(28 * KB, [128, D], F32)
        ostg = [AR.view((32 + 4 * i) * KB, [128, D], F32) for i in range(2)]
        b_fg = Buf("fg"); b_os = [Buf("os0"), Buf("os1")]; b_xa = [Buf("xa0"), Buf("xa1")]
        b_yk = [Buf(f"yk{i}") for i in range(4)]; b_tacc = Buf("tacc")
        o_sem = [T.mksem("o0"), T.mksem("o1")]
        xa_sem = [T.mksem("xa0"), T.mksem("xa1")]
        yk_sem = [T.mksem(f"yk{i}") for i in range(4)]
        T.dma(sp, fgB, fg_d, ldP, writes=[b_fg])
        for tt in range(16):
            s = tt % 2
            T.dma(sp, xa[s], xnew_d[tt * 128:(tt + 1) * 128, :], xa_sem[s], writes=[b_xa[s]])
            for k in range(4):
                T._waits(pool, T._deps([], [b_yk[k]]))
                inst = nc.gpsimd.indirect_dma_start(out=yk[k], out_offset=None, in_=Yd,
                                                    in_offset=bass.IndirectOffsetOnAxis(ap=slot_all[:, tt, k:k + 1], axis=0))
                yk_sem[k].cnt += 16
                inst.then_inc(yk_sem[k].h, 16)
                T._update((yk_sem[k], yk_sem[k].cnt), [], [b_yk[k]])
            T.op(dve, lambda tt=tt: nc.vector.tensor_scalar(out=tacc, in0=yk[0], scalar1=gk_all[:, tt, 0:1], scalar2=None,
                                                            op0=ALU.mult), reads=[b_yk[0]], writes=[b_tacc])
            for k in range(1, 4):
                T.op(dve, lambda tt=tt, k=k: nc.vector.scalar_tensor_tensor(out=tacc, in0=yk[k], scalar=gk_all[:, tt, k:k + 1],
                                                                            in1=tacc, op0=ALU.mult, op1=ALU.add),
                     reads=[b_yk[k], b_tacc], writes=[b_tacc])
            T.op(dve, lambda: nc.vector.tensor_tensor(out=tacc, in0=tacc, in1=g2B[:], op=ALU.mult), reads=[b_tacc], writes=[b_tacc])
            T.op(dve, lambda s=s: nc.vector.tensor_tensor(out=xa[s], in0=xa[s], in1=tacc, op=ALU.add),
                 reads=[b_tacc, b_xa[s]], writes=[b_xa[s]])
            T.op(act, lambda s=s: nc.scalar.activation(out=junk[:], in_=xa[s], func=AF.Square, accum_out=gsum[:, 0:1]),
                 reads=[b_xa[s]], writes=[b_sm])
            T.op(act, lambda: nc.scalar.activation(out=gsum[:, 1:2], in_=gsum[:, 0:1], func=AF.Sqrt, scale=1.0 / D,
                                                   bias=epsT[:]), reads=[b_sm], writes=[b_sm])
            T.op(dve, lambda: nc.vector.reciprocal(out=gsum[:, 1:2], in_=gsum[:, 1:2]), reads=[b_sm], writes=[b_sm])
            T.op(dve, lambda s=s: nc.vector.scalar_tensor_tensor(out=ostg[s], in0=xa[s], scalar=gsum[:, 1:2],
                                                                 in1=fgB, op0=ALU.mult, op1=ALU.mult),
                 reads=[b_xa[s], b_sm, b_fg], writes=[b_os[s]])
            T.dma(act, y[tt * 128:(tt + 1) * 128, :], ostg[s], o_sem[s], reads=[b_os[s]])
        T.barrier()
    return nc


def _rope_tables():
    rows = SEQ // 64
    row = np.repeat(np.arange(rows, dtype=np.float32), 64)
    col = np.tile(np.arange(64, dtype=np.float32), rows)
    freqs = (np.float32(10000.0) ** (-np.arange(8, dtype=np.float32) / np.float32(8))).astype(np.float32)
    ang = np.concatenate([row[:, None] * freqs, col[:, None] * freqs], axis=-1).astype(np.float32)
    return np.cos(ang).astype(np.float32), np.sin(ang).astype(np.float32)


def _core_inputs(core, x, c, ctx, c_ctx, shared, cos, sin):
    b, half = core // 2, core % 2
    q0 = half * NQ
    o0 = (1 - half) * NQ
    xe = np.zeros((36 * 128, D), np.float32)
    xe[0:NQ] = x[b, q0:q0 + NQ]
    hm = np.zeros((128, 16), np.float32)
    if half == 1:
        xe[16 * 128:17 * 128] = x[b, q0 - 128:q0]
        hm[:, 0:8] = 1.0
    else:
        xe[17 * 128:18 * 128] = x[b, q0 + NQ:q0 + NQ + 128]
        hm[:, 8:16] = 1.0
    xe[18 * 128:34 * 128] = x[b, o0:o0 + NQ]
    xe[34 * 128:36 * 128] = ctx[b]
    trig = np.zeros((2, 32, NK), np.float32)
    for (dst, src) in ((0, q0), (NQ, o0)):
        cs = cos[src:src + NQ].T
        sn = sin[src:src + NQ].T
        trig[0, 0:16, dst:dst + NQ] = cs
        trig[0, 16:32, dst:dst + NQ] = cs
        trig[1, 0:16, dst:dst + NQ] = sn
        trig[1, 16:32, dst:dst + NQ] = sn
    trig[0, :, 2 * NQ:] = 1.0
    ccol = np.stack([c[b].reshape(8, 128).T, c_ctx.reshape(8, 128).T], axis=-1).astype(np.float32)
    invc = np.zeros((128, 4, 16), np.float32)
    for g, w in enumerate((2, 4, 8, 16)):
        hw = w // 2
        for i in range(16):
            t = q0 + i if i < 8 else q0 + NQ - 16 + i
            lo = max(t - hw, 0)
            hi = min(t + hw, SEQ)
            invc[:, g, i] = np.float32(1.0) / np.float32(hi - lo)
    m = {"xe": xe, "trig": trig, "ccol": np.ascontiguousarray(ccol), "hmask": hm, "invc": invc}
    m.update(shared)
    return m


_NC_CACHE = {}


def kernel(x, c, ctx, c_ctx, w_mod, b_mod, norm1_g, w_in, q_norm_g, kv_norm_g, w_uq, w_ukv, w_pool, pool_scale,
           w_out, norm2_g, router_w, router_b, w_gate_up, b_gate_up, w_down, b_down, final_g):
    f = lambda a: np.ascontiguousarray(np.asarray(a, dtype=np.float32))
    x, c, ctx, c_ctx = f(x), f(c), f(ctx), f(c_ctx)
    col = lambda v: np.ascontiguousarray(f(v).reshape(-1, 128).T)
    shared = {
        "w_mod": f(w_mod)[0], "bmod2": np.ascontiguousarray(np.broadcast_to(f(b_mod)[0][None, :], (2, 6 * D))),
        "n1col": col(f(norm1_g)[0]), "n2col": col(f(norm2_g)[0]), "w_in": f(w_in)[0],
        "qgcol": col(f(q_norm_g)[0]), "kvgcol": col(f(kv_norm_g)[0]), "w_uq": f(w_uq)[0], "w_ukv": f(w_ukv)[0],
        "w_pool": f(w_pool)[0], "pscol": col(f(pool_scale)[0]), "w_out": f(w_out)[0], "router_w": f(router_w)[0],
        "rbrep": np.ascontiguousarray(np.broadcast_to(f(router_b)[0][None, :], (128, NE))),
        "w_gu": f(w_gate_up)[0],
        "b_gu": f(b_gate_up)[0], "n2row": np.ascontiguousarray(f(norm2_g)[0][None, :]),
        "iota32": np.ascontiguousarray(np.broadcast_to(np.arange(NE, dtype=np.float32)[None, :], (128, NE))),
        "ustr": np.triu(np.ones((128, 128), np.float32), 1),
        "tokrow": np.ascontiguousarray(np.broadcast_to(
            (np.arange(16, dtype=np.int32)[None, :, None] * 128 + np.arange(128, dtype=np.int32)[:, None, None]), (128, 16, 2))),
        "w_dn": f(w_down)[0], "b_dn": f(b_down)[0],
        "fgrep": np.ascontiguousarray(np.broadcast_to(f(final_g)[None, :], (128, D))),
        "ident": np.eye(128, dtype=np.float32),
    }
    cos, sin = _rope_tables()
    in_maps = [_core_inputs(core, x, c, ctx, c_ctx, shared, cos, sin) for core in range(8)]
    if "nc" not in _NC_CACHE:
        _NC_CACHE["nc"] = build_program()
    res = run_bass_kernel_spmd(_NC_CACHE["nc"], in_maps, core_ids=list(range(8)))
    out = np.zeros((4, SEQ, D), np.float32)
    for core in range(8):
        b, half = core // 2, core % 2
        out[b, half * NQ:(half + 1) * NQ] = res.results[core]["y"]
    return out
```

```python
import math
from contextlib import ExitStack

import numpy as np
import concourse.bass as bass
import concourse.mybir as mybir
from concourse.bass_utils import run_bass_kernel_spmd

F32 = mybir.dt.float32
BF16 = mybir.dt.bfloat16
I32 = mybir.dt.int32
U32 = mybir.dt.uint32
AF = mybir.ActivationFunctionType
ALU = mybir.AluOpType

D = 1024
SEQ = 4096
CTX = 256
NK = SEQ + CTX
NKT = NK // 128
NQ = 2048
H = 8
EPS = 1e-6
NE = 32
SCALE = 1.0 / math.sqrt(96.0)
KB = 1024
ARENA_BYTES = 180 * KB
DEBUG = False
TPP = 4


class Sem:
    def __init__(self, h, name):
        self.h = h
        self.name = name
        self.cnt = 0


class Buf:
    __slots__ = ("name", "w", "r")

    def __init__(self, name=""):
        self.name = name
        self.w = None
        self.r = {}


class Eng:
    def __init__(self, name, h, sem):
        self.name = name
        self.h = h
        self.sem = sem
        self.seen = {}


class Sched:
    def __init__(self, nc, es):
        self.nc = nc
        self.es = es
        self.all_sems = []
        self.pe = Eng("pe", nc.tensor, self.mksem("s_pe"))
        self.act = Eng("act", nc.scalar, self.mksem("s_act"))
        self.dve = Eng("dve", nc.vector, self.mksem("s_dve"))
        self.pool = Eng("pool", nc.gpsimd, self.mksem("s_pool"))
        self.sp = Eng("sp", nc.sync, self.mksem("s_sp"))
        self.engs = [self.pe, self.act, self.dve, self.pool, self.sp]

    def mksem(self, name):
        s = Sem(self.es.enter_context(self.nc.semaphore(name)), name)
        self.all_sems.append(s)
        return s

    def _deps(self, reads, writes):
        toks = {}

        def add(t):
            if t is None:
                return
            sem, val = t
            if toks.get(sem, 0) < val:
                toks[sem] = val

        for b in reads:
            add(b.w)
        for b in writes:
            add(b.w)
            for t in b.r.values():
                add(t)
        return toks

    def _waits(self, e, toks):
        for sem, val in toks.items():
            if sem is e.sem and e is self.pe:
                continue
            if e.seen.get(sem, 0) >= val:
                continue
            e.h.wait_ge(sem.h, val)
            e.seen[sem] = val

    def _update(self, tok, reads, writes):
        for b in reads:
            b.r[tok[0]] = tok
        for b in writes:
            b.w = tok
            b.r = {}

    def op(self, e, fn, reads=(), writes=()):
        self._waits(e, self._deps(reads, writes))
        inst = fn()
        e.sem.cnt += 1
        inst.then_inc(e.sem.h, 1)
        tok = (e.sem, e.sem.cnt)
        self._update(tok, reads, writes)
        return tok

    def dma(self, q, out, in_, dsem, reads=(), writes=()):
        self._waits(q, self._deps(reads, writes))
        inst = q.h.dma_start(out=out, in_=in_)
        dsem.cnt += 16
        inst.then_inc(dsem.h, 16)
        tok = (dsem, dsem.cnt)
        self._update(tok, reads, writes)
        return tok

    def cond_snapshot(self):
        return ({s: s.cnt for s in self.all_sems}, {e.name: dict(e.seen) for e in self.engs})

    def cond_compensate(self, snap, owners):
        before, _ = snap
        for s in self.all_sems:
            delta = s.cnt - before[s]
            if delta == 0:
                continue
            e = None
            for en in self.engs:
                if en.sem is s:
                    e = en
            if e is None:
                e = owners[s]
            if before[s] > 0:
                e.h.wait_ge(s.h, before[s])
            e.h.sem_inc(s.h, delta)

    def cond_restore_seen(self, snap):
        _, seen = snap
        for e in self.engs:
            e.seen = dict(seen[e.name])

    def barrier(self):
        toks = {s: s.cnt for s in self.all_sems if s.cnt > 0}
        for e in self.engs:
            self._waits(e, toks)


class Arena:
    def __init__(self, t):
        self.t = t

    def view(self, off, shape, dt, parts=None):
        esz = 2 if dt == BF16 else 4
        n = 1
        for s in shape[1:]:
            n *= s
        nbytes = n * esz
        assert off % 4 == 0 and off + nbytes <= ARENA_BYTES, (off, shape)
        ap = self.t[:, off // 2:(off + nbytes) // 2]
        if dt != BF16:
            ap = ap.bitcast(dt)
        if len(shape) == 3:
            ap = ap.rearrange("p (a b) -> p a b", b=shape[2])
        elif len(shape) == 4:
            ap = ap.rearrange("p (a b c) -> p a b c", b=shape[2], c=shape[3])
        if shape[0] != 128:
            ap = ap[0:shape[0]]
        return ap


def build_program():
    nc = bass.Bass("TRN2", target_bir_lowering=False)

    def din(name, shape):
        return nc.dram_tensor(name, list(shape), F32, kind="ExternalInput").ap()

    xe = din("xe", [36 * 128, D])
    trig = din("trig", [2, 32, NK])
    ccol_d = din("ccol", [128, 8, 2])
    hmask_d = din("hmask", [128, 16])
    invc_d = din("invc", [128, 4, 16])
    w_mod = din("w_mod", [D, 6 * D])
    bmod2_d = din("bmod2", [2, 6 * D])
    n1col_d = din("n1col", [128, 8])
    n2col_d = din("n2col", [128, 8])
    w_in = din("w_in", [D, 928])
    qg_d = din("qgcol", [128, 2])
    kvg_d = din("kvgcol", [128, 1])
    w_uq = din("w_uq", [256, 768])
    w_ukv = din("w_ukv", [128, 1024])
    w_pool = din("w_pool", [4, 128, 128])
    pscol_d = din("pscol", [128, 4])
    w_out = din("w_out", [D, D])
    router_w = din("router_w", [D, NE])
    rb_d = din("rbrep", [128, NE])
    w_gu = din("w_gu", [NE, D, 2 * D])
    b_gu = din("b_gu", [NE, 2 * D])
    n2row_d = din("n2row", [1, D])
    iota_d = din("iota32", [128, NE])
    ustr_d = din("ustr", [128, 128])
    tokrow_d = nc.dram_tensor("tokrow", [128, 16, 2], I32, kind="ExternalInput").ap()
    h2d = nc.dram_tensor("h2d", [NQ, D], BF16).ap()
    xnew_d = nc.dram_tensor("xnew_d", [NQ, D], F32).ap()
    tokl = nc.dram_tensor("tokl", [NE * 2048, 2], I32).ap()
    Yd = nc.dram_tensor("Yd", [NE * 2048, D], F32).ap()
    nt_d = nc.dram_tensor("nt_d", [1, NE + 1], I32).ap()
    w_dn = din("w_dn", [NE, D, D])
    b_dn = din("b_dn", [NE, D])
    fg_d = din("fgrep", [128, D])
    ident_d = din("ident", [128, 128])
    y = nc.dram_tensor("y", [NQ, D], F32, kind="ExternalOutput").ap()
    with ExitStack() as es:
        T = Sched(nc, es)
        pe, act, dve, pool, sp = T.pe, T.act, T.dve, T.pool, T.sp

        def sb(name, shape, dt=F32):
            return es.enter_context(nc.sbuf_tensor("sb_" + name, list(shape), dt))

        arena_t = sb("arena", [128, ARENA_BYTES // 2], BF16)
        AR = Arena(arena_t)
        ps_t = es.enter_context(nc.psum_tensor("ps", [128, 4096], F32))
        PB = [Buf(f"psb{i}") for i in range(8)]

        def bank(i, n=512, parts=128, nb=1):
            return ps_t[0:parts, i * 512:i * 512 + (n if nb == 1 else nb * 512)]

        identF = sb("identF", [128, 128]); identB = sb("identB", [128, 128], BF16)
        onesF = sb("onesF", [128, 128])
        ccol = sb("ccolT", [128, 8, 2]); scT = sb("scT", [128, 8, 2])
        modcol = sb("modcol", [128, 32, 2])
        n1col = sb("n1col", [128, 8]); n2col = sb("n2col", [128, 8])
        G1L = sb("G1L", [128, 8]); G1C = sb("G1C", [128, 8]); G2c = sb("G2c", [128, 8])
        zb = sb("zb", [128, 16])
        qgc = sb("qgc", [128, 2]); kvgc = sb("kvgc", [128, 1]); pscol = sb("pscol", [128, 4])
        hmask = sb("hmask", [128, 16]); invc = sb("invc", [128, 4, 16])
        g2B = sb("g2B", [128, D])
        iota32 = sb("iota32s", [128, NE]); ustrF = sb("ustrF", [128, 128]); UstrB = sb("UstrB", [128, 128], BF16)
        onesB = sb("onesB", [128, 128], BF16); tokrow = sb("tokrows", [128, 16, 2], I32)
        rbB = sb("rbB", [128, NE]); rwt = sb("rwt", [128, 8, NE])
        bdS = sb("bdS", [32, D])
        ssv = sb("ssv", [128, 8]); rstdv = sb("rstdv", [128, 8])
        junk = sb("junk", [128, D], BF16)
        smallA = sb("smallA", [128, 64])
        b_const = Buf("const")

        ldA = T.mksem("ldA")
        for dst, src in ((identF, ident_d), (ccol, ccol_d), (n1col, n1col_d), (n2col, n2col_d), (qgc, qg_d),
                         (kvgc, kvg_d), (pscol, pscol_d), (hmask, hmask_d), (invc, invc_d), (iota32, iota_d),
                         (ustrF, ustr_d), (tokrow, tokrow_d), (rbB, rb_d)):
            T.dma(sp, dst[:], src, ldA, writes=[Buf()])
        T.dma(sp, rwt[:], router_w.rearrange("(k q) e -> q k e", q=128), ldA, writes=[Buf()])
        T.dma(sp, bdS[:], b_dn, ldA, writes=[Buf()])
        T.barrier()

        b_c2 = Buf("c2")
        epsT = sb("epsT", [128, 1])
        T.op(dve, lambda: nc.vector.memset(epsT[:], EPS), writes=[b_c2])
        T.op(dve, lambda: nc.vector.memset(onesF[:], 1.0), writes=[b_c2])
        T.op(dve, lambda: nc.vector.tensor_copy(out=identB[:], in_=identF[:]), reads=[b_const], writes=[b_c2])
        T.op(dve, lambda: nc.vector.memset(onesB[:], 1.0), writes=[b_c2])
        T.op(dve, lambda: nc.vector.tensor_copy(out=UstrB[:], in_=ustrF[:]), reads=[b_const], writes=[b_c2])

        modrow = AR.view(108 * KB, [2, 6 * D], F32)
        bmod2 = AR.view(132 * KB, [2, 6 * D], F32)
        wmod_ring = [AR.view(i * 16 * KB, [128, 8, 512], F32) for i in range(2)]
        win_f = AR.view(32 * KB, [128, 8, 928], F32)
        wuq_f = AR.view(61 * KB, [128, 2, 768], F32)
        wukv_f = AR.view(67 * KB, [128, 1024], F32)
        w_kn = AR.view(86 * KB, [128, 8, 64], BF16)
        w_v = AR.view(87 * KB, [128, 512], BF16)
        w_inL = AR.view(88 * KB, [128, 8, 416], BF16)
        w_inC = AR.view(95 * KB, [128, 8, 160], BF16)
        w_rotL = AR.view(98 * KB, [128, 8, 96], BF16)
        w_rotC = AR.view(100 * KB, [128, 8, 96], BF16)
        w_uqS = AR.view(102 * KB, [128, 2, 768], BF16)
        w_uqR = AR.view(105 * KB, [128, 2, 768], BF16)
        win_rotf = AR.view(156 * KB, [128, 8, 96], F32)

        b_ph0 = Buf("ph0")
        rows2 = AR.view(172 * KB, [1, 2, D], F32)
        n2row = AR.view(160 * KB, [1, D], F32)
        zt = AR.view(164 * KB, [128, 1024], I32)
        b_tokl0 = Buf("tokl0")
        zsem = T.mksem("zs")
        T.dma(sp, n2row, n2row_d, ldA, writes=[Buf()])
        T.op(pool, lambda: nc.gpsimd.memset(zt, 0), writes=[b_tokl0])
        T.dma(sp, tokl.rearrange("(p a) b -> p (a b)", p=128), zt, zsem, reads=[b_tokl0], writes=[b_tokl0])
        T.dma(sp, bmod2, bmod2_d, ldA, writes=[Buf()])
        T.dma(sp, win_f, w_in.rearrange("(k q) f -> q k f", q=128), ldA, writes=[Buf()])
        T.dma(sp, wuq_f, w_uq.rearrange("(k q) f -> q k f", q=128), ldA, writes=[Buf()])
        T.dma(sp, wukv_f, w_ukv, ldA, writes=[Buf()])
        T.barrier()

        T.op(act, lambda: nc.scalar.activation(out=scT[:], in_=ccol[:], func=AF.Silu), reads=[b_const], writes=[b_ph0])

        wm_sem = [T.mksem(f"wm{i}") for i in range(2)]
        wm_buf = [Buf(f"wm{i}") for i in range(2)]
        b_modrow = Buf("modrow")
        for ns in range(12):
            s = ns % 2
            T.dma(sp, wmod_ring[s], w_mod.rearrange("(k q) n -> q k n", q=128)[:, :, ns * 512:(ns + 1) * 512],
                  wm_sem[s], writes=[wm_buf[s]])
            pb = ns % 2

            def mm(s=s, pb=pb):
                last = None
                for k in range(8):
                    last = nc.tensor.matmul(bank(pb, parts=2), lhsT=scT[:, k, :], rhs=wmod_ring[s][:, k, :],
                                            start=(k == 0), stop=(k == 7))
                return last
            T.op(pe, mm, reads=[wm_buf[s], b_ph0], writes=[PB[pb]])
            T.op(dve, lambda pb=pb, ns=ns: nc.vector.tensor_tensor(
                out=modrow[:, ns * 512:(ns + 1) * 512], in0=bank(pb, parts=2), in1=bmod2[:, ns * 512:(ns + 1) * 512],
                op=ALU.add), reads=[PB[pb], b_const], writes=[b_modrow])

        chunk_src = [0 * 8 + i for i in range(8)] + [1 * 8 + i for i in range(8)] + \
                    [3 * 8 + i for i in range(8)] + [4 * 8 + i for i in range(8)]

        def tr_mod():
            last = None
            for i, ch in enumerate(chunk_src):
                last = nc.tensor.transpose(out=bank(2, 64)[:, 2 * i:2 * i + 2], in_=modrow[:, ch * 128:(ch + 1) * 128],
                                           identity=identF[0:2, 0:2])
            return last
        T.op(pe, tr_mod, reads=[b_modrow, b_const], writes=[PB[2]])
        T.op(dve, lambda: nc.vector.tensor_copy(out=modcol[:].rearrange("p a b -> p (a b)"), in_=bank(2, 64)),
             reads=[PB[2]], writes=[b_ph0])
        T.op(dve, lambda: nc.vector.scalar_tensor_tensor(out=G1L[:], in0=modcol[:, 8:16, 0], scalar=1.0, in1=n1col[:],
                                                         op0=ALU.add, op1=ALU.mult), reads=[b_ph0, b_const], writes=[b_ph0])
        T.op(dve, lambda: nc.vector.scalar_tensor_tensor(out=G1C[:], in0=modcol[:, 8:16, 1], scalar=1.0, in1=n1col[:],
                                                         op0=ALU.add, op1=ALU.mult), reads=[b_ph0, b_const], writes=[b_ph0])
        T.op(dve, lambda: nc.vector.scalar_tensor_tensor(out=G2c[:], in0=modcol[:, 24:32, 0], scalar=1.0, in1=n2col[:],
                                                         op0=ALU.add, op1=ALU.mult), reads=[b_ph0, b_const], writes=[b_ph0])
        for hf in range(2):
            T.op(pe, lambda hf=hf: nc.tensor.matmul(bank(3 + hf), lhsT=onesF[0:1, :],
                                                    rhs=modrow[0:1, 5 * D + hf * 512:5 * D + (hf + 1) * 512],
                                                    start=True, stop=True), reads=[b_modrow, b_const], writes=[PB[3 + hf]])
            T.op(dve, lambda hf=hf: nc.vector.tensor_copy(out=g2B[:, hf * 512:(hf + 1) * 512], in_=bank(3 + hf)),
                 reads=[PB[3 + hf]], writes=[b_ph0])
        g1row = sb("g1row", [1, D])
        T.op(act, lambda: nc.scalar.copy(out=g1row[:], in_=modrow[0:1, 2 * D:3 * D]), reads=[b_modrow], writes=[b_ph0])
        T.op(dve, lambda: nc.vector.scalar_tensor_tensor(out=rows2[0:1, 0, :], in0=modrow[0:1, 4 * D:5 * D], scalar=1.0, in1=n2row,
                                                         op0=ALU.add, op1=ALU.mult), reads=[b_modrow, b_const], writes=[b_ph0])
        T.op(dve, lambda: nc.vector.tensor_copy(out=rows2[0:1, 1, :], in_=modrow[0:1, 3 * D:4 * D]), reads=[b_modrow], writes=[b_ph0])
        T.op(dve, lambda: nc.vector.tensor_tensor(out=bdS[:], in0=bdS[:], in1=g2B[0:32, :], op=ALU.mult),
             reads=[b_const, b_ph0], writes=[b_ph0])

        T.op(dve, lambda: nc.vector.memset(win_rotf, 0.0), writes=[b_ph0])
        T.op(dve, lambda: nc.vector.tensor_scalar(out=win_rotf[:, :, 64:80], in0=win_f[:, :, 400:416], scalar1=-1.0,
                                                  scalar2=None, op0=ALU.mult), reads=[b_const], writes=[b_ph0])
        T.op(dve, lambda: nc.vector.tensor_copy(out=win_rotf[:, :, 80:96], in_=win_f[:, :, 384:400]),
             reads=[b_const], writes=[b_ph0])

        zb_specs = [(win_f, 0, 128, 0, 128), (win_f, 128, 256, 0, 128), (win_f, 256, 384, 0, 128),
                    (win_f, 320, 416, 0, 96), (win_rotf, 0, 96, 0, 96),
                    (win_f, 416, 544, 0, 128), (win_f, 544, 672, 0, 128), (win_f, 672, 800, 0, 128),
                    (win_f, 800, 928, 0, 128),
                    (win_f, 256, 384, 1, 128), (win_f, 320, 416, 1, 96), (win_rotf, 0, 96, 1, 96)]

        def zb_mm():
            last = None
            for j, (wt, c0, c1, which, m) in enumerate(zb_specs):
                for k in range(8):
                    last = nc.tensor.matmul(bank(5, 16)[0:m, j:j + 1], lhsT=wt[:, k, c0:c1],
                                            rhs=modcol[:, k, which:which + 1], start=(k == 0), stop=(k == 7))
            return last
        T.op(pe, zb_mm, reads=[b_const, b_ph0], writes=[PB[5]])
        T.op(dve, lambda: nc.vector.memset(zb[:], 0.0), writes=[b_ph0])
        for j, (wt, c0, c1, which, m) in enumerate(zb_specs):
            T.op(dve, lambda j=j, m=m: nc.vector.tensor_copy(out=zb[0:m, j:j + 1], in_=bank(5, 16)[0:m, j:j + 1]),
                 reads=[PB[5]], writes=[b_ph0])

        for k in range(8):
            T.op(dve, lambda k=k: nc.vector.tensor_scalar(out=w_inL[:, k, :], in0=win_f[:, k, 0:416], scalar1=G1L[:, k:k + 1],
                                                          scalar2=None, op0=ALU.mult), reads=[b_const, b_ph0], writes=[b_ph0])
            T.op(dve, lambda k=k: nc.vector.tensor_scalar(out=w_inC[:, k, :], in0=win_f[:, k, 256:416], scalar1=G1C[:, k:k + 1],
                                                          scalar2=None, op0=ALU.mult), reads=[b_const, b_ph0], writes=[b_ph0])
        for wr, ws, o in ((w_rotL, w_inL, 384), (w_rotC, w_inC, 128)):
            T.op(dve, lambda wr=wr: nc.vector.memset(wr, 0.0), writes=[b_ph0])
            T.op(dve, lambda wr=wr, ws=ws, o=o: nc.vector.tensor_scalar(out=wr[:, :, 64:80], in0=ws[:, :, o + 16:o + 32],
                                                                        scalar1=-1.0, scalar2=None, op0=ALU.mult),
                 reads=[b_ph0], writes=[b_ph0])
            T.op(dve, lambda wr=wr, ws=ws, o=o: nc.vector.tensor_copy(out=wr[:, :, 80:96], in_=ws[:, :, o:o + 16]),
                 reads=[b_ph0], writes=[b_ph0])
        T.op(dve, lambda: nc.vector.memset(w_uqR, 0.0), writes=[b_ph0])
        for c in range(2):
            T.op(dve, lambda c=c: nc.vector.tensor_scalar(out=w_uqS[:, c, :], in0=wuq_f[:, c, :], scalar1=qgc[:, c:c + 1],
                                                          scalar2=None, op0=ALU.mult), reads=[b_const], writes=[b_ph0])
            sv = w_uqS[:, c, :].rearrange("p (h e) -> p h e", e=96)
            rv = w_uqR[:, c, :].rearrange("p (h e) -> p h e", e=96)
            T.op(dve, lambda sv=sv, rv=rv: nc.vector.tensor_scalar(out=rv[:, :, 64:80], in0=sv[:, :, 80:96], scalar1=-1.0,
                                                                   scalar2=None, op0=ALU.mult), reads=[b_ph0], writes=[b_ph0])
            T.op(dve, lambda sv=sv, rv=rv: nc.vector.tensor_copy(out=rv[:, :, 80:96], in_=sv[:, :, 64:80]),
                 reads=[b_ph0], writes=[b_ph0])
        kvv = wukv_f.rearrange("p (h e) -> p h e", e=128)
        T.op(dve, lambda: nc.vector.tensor_scalar(out=w_kn, in0=kvv[:, :, 0:64], scalar1=kvgc[:, 0:1], scalar2=None,
                                                  op0=ALU.mult), reads=[b_const], writes=[b_ph0])
        T.op(dve, lambda: nc.vector.tensor_scalar(out=w_v.rearrange("p (h e) -> p h e", e=64), in0=kvv[:, :, 64:128],
                                                  scalar1=kvgc[:, 0:1], scalar2=None, op0=ALU.mult),
             reads=[b_const], writes=[b_ph0])
        T.barrier()

        x_sem = [T.mksem(f"xs{i}") for i in range(3)]
        x_buf = [Buf(f"xb{i}") for i in range(3)]
        xn_buf = [Buf(f"xn{i}") for i in range(2)]
        ss_buf = [Buf(f"ss{i}") for i in range(8)]
        state = {"xi": 0, "ni": 0, "si": 0, "tp": 0}

        def norm_tile_to_hT(x_ring, xn_ring, src_rows, hT_ap, hT_b, col0, tp_banks):
            xi = state["xi"] % len(x_ring); state["xi"] += 1
            ni = state["ni"] % 2; state["ni"] += 1
            si = state["si"] % 8; state["si"] += 1
            tb = tp_banks[state["tp"] % len(tp_banks)]; state["tp"] += 1
            xt = x_ring[xi]; xn = xn_ring[ni]
            T.dma(sp, xt, xe[src_rows * 128:(src_rows + 1) * 128, :], x_sem[xi], writes=[x_buf[xi]])
            T.op(act, lambda: nc.scalar.activation(out=junk[:], in_=xt, func=AF.Square, accum_out=ssv[:, si:si + 1]),
                 reads=[x_buf[xi]], writes=[ss_buf[si]])
            T.op(act, lambda: nc.scalar.activation(out=rstdv[:, si:si + 1], in_=ssv[:, si:si + 1], func=AF.Sqrt,
                                                   scale=1.0 / D, bias=epsT[:]), reads=[ss_buf[si]], writes=[ss_buf[si]])
            T.op(dve, lambda: nc.vector.reciprocal(out=rstdv[:, si:si + 1], in_=rstdv[:, si:si + 1]),
                 reads=[ss_buf[si]], writes=[ss_buf[si]])
            T.op(act, lambda: nc.scalar.activation(out=xn, in_=xt, func=AF.Copy, scale=rstdv[:, si:si + 1]),
                 reads=[x_buf[xi], ss_buf[si]], writes=[xn_buf[ni]])
            tpv = bank(tb).bitcast(BF16)

            def tr():
                last = None
                for c in range(8):
                    last = nc.tensor.transpose(out=tpv[:, c * 128:(c + 1) * 128], in_=xn[:, c * 128:(c + 1) * 128],
                                               identity=identB[:])
                return last
            T.op(pe, tr, reads=[xn_buf[ni], b_const], writes=[PB[tb]])
            T.op(dve, lambda: nc.vector.tensor_copy(out=hT_ap[:, :, col0:col0 + 128],
                                                    in_=tpv.rearrange("p (c t) -> p c t", t=128)),
                 reads=[PB[tb]], writes=[hT_b])

        def rstd_bcast(psb, n, out_ap, inv_n, bufs_r, bufs_w):
            T.op(act, lambda: nc.scalar.activation(out=out_ap, in_=bank(psb, n), func=AF.Sqrt, scale=inv_n, bias=epsT[:]),
                 reads=bufs_r, writes=bufs_w)
            T.op(dve, lambda: nc.vector.reciprocal(out=out_ap, in_=out_ap), reads=bufs_w, writes=bufs_w)

        QT_all = AR.view(0, [96, 8, NQ], BF16)
        V_ext = AR.view(32 * KB, [128, NKT, 8, 65], BF16)
        ckvnT = AR.view(68 * KB, [128, NK], BF16)
        krT = AR.view(77 * KB, [96, NK], BF16)
        x_ring = [AR.view((108 + 4 * i) * KB, [128, D], F32) for i in range(3)]
        xn_ring = [AR.view((120 + 2 * i) * KB, [128, D], BF16) for i in range(2)]
        hT_ring = [AR.view((124 + 8 * i) * KB, [128, 8, 512], BF16) for i in range(2)]
        cos_ring = [AR.view((140 + 4 * i) * KB, [128, 512], F32) for i in range(2)]
        sin_ring = [AR.view((142 + 4 * i) * KB, [128, 512], F32) for i in range(2)]
        ckv_sb = AR.view(148 * KB, [128, 512], F32)
        sq_kv = AR.view(150 * KB, [128, 512], F32)
        cq_sb = AR.view(152 * KB, [128, 2, 512], F32)
        sq_q = AR.view(156 * KB, [128, 2, 512], F32)
        rstd_kv = AR.view(160 * KB, [128, 512], F32)
        rstd_q = AR.view(162 * KB, [128, 512], F32)
        cqn = AR.view(164 * KB, [128, 2, 512], BF16)
        t1 = AR.view(168 * KB, [128, 512], F32)
        t2 = AR.view(170 * KB, [128, 512], F32)

        b_QT = Buf("QT"); b_V = Buf("V"); b_ckvn = Buf("ckvn"); b_kr = Buf("kr")
        hT_b = [Buf("hT0"), Buf("hT1")]
        tg_sem = [T.mksem(f"tg{i}") for i in range(2)]
        tg_b = [Buf("tg0"), Buf("tg1")]
        b_ckvsb = Buf("ckvsb"); b_sqkv = Buf("sqkv"); b_cqsb = Buf("cqsb"); b_sqq = Buf("sqq")
        b_rkv = Buf("rkv"); b_rq = Buf("rq"); b_cqn = Buf("cqn"); b_t1 = Buf("t1"); b_t2 = Buf("t2")

        T.op(dve, lambda: nc.vector.memset(V_ext, 1.0), writes=[b_V])

        mmb = {"i": 0}

        def nextbank():
            b = 2 + (mmb["i"] % 6)
            mmb["i"] += 1
            return b

        groups = []
        for g in range(4):
            groups.append(("own", [4 * g + i for i in range(4)], g * 512))
        for g in range(4):
            groups.append(("other", [18 + 4 * g + i for i in range(4)], 2048 + g * 512))
        groups.append(("ctx", [34, 35], 4096))

        def norm_group_1a(gi):
            kind, tiles, kc0 = groups[gi]
            n = 128 * len(tiles)
            hb = gi % 2
            T.dma(sp, cos_ring[hb][64:96, 0:n], trig[0, :, kc0:kc0 + n], tg_sem[hb], writes=[tg_b[hb]])
            T.dma(sp, sin_ring[hb][64:96, 0:n], trig[1, :, kc0:kc0 + n], tg_sem[hb], writes=[tg_b[hb]])
            for ti, t in enumerate(tiles):
                norm_tile_to_hT(x_ring, xn_ring, t, hT_ring[hb], hT_b[hb], ti * 128, (0, 1))

        for gi, (kind, tiles, kc0) in enumerate(groups):
            n = 128 * len(tiles)
            hb = gi % 2
            hT = hT_ring[hb]
            W = w_inC if kind == "ctx" else w_inL
            WR = w_rotC if kind == "ctx" else w_rotL
            off = -256 if kind == "ctx" else 0
            zj = (9, 10, 11) if kind == "ctx" else (2, 3, 4)
            if gi == 0:
                norm_group_1a(0)
            if gi + 1 < len(groups):
                norm_group_1a(gi + 1)
            pb = nextbank()

            def mm_ckv(pb=pb, W=W, off=off, hT=hT, n=n):
                last = None
                for k in range(8):
                    last = nc.tensor.matmul(bank(pb, n), lhsT=W[:, k, 256 + off:384 + off], rhs=hT[:, k, 0:n],
                                            start=(k == 0), stop=(k == 7))
                return last
            T.op(pe, mm_ckv, reads=[hT_b[hb], b_ph0], writes=[PB[pb]])
            T.op(act, lambda pb=pb, n=n, zj=zj: nc.scalar.activation(out=ckv_sb[:, 0:n], in_=bank(pb, n), func=AF.Identity,
                                                                    bias=zb[:, zj[0]:zj[0] + 1]),
                 reads=[PB[pb], b_ph0], writes=[b_ckvsb])
            T.op(act, lambda n=n: nc.scalar.activation(out=sq_kv[:, 0:n], in_=ckv_sb[:, 0:n], func=AF.Square),
                 reads=[b_ckvsb], writes=[b_sqkv])
            pb2 = nextbank()
            T.op(pe, lambda pb2=pb2, n=n: nc.tensor.matmul(bank(pb2, n), lhsT=onesF[:], rhs=sq_kv[:, 0:n], start=True, stop=True),
                 reads=[b_sqkv, b_const], writes=[PB[pb2]])
            rstd_bcast(pb2, n, rstd_kv[:, 0:n], 1.0 / 128, [PB[pb2]], [b_rkv])
            T.op(dve, lambda n=n, kc0=kc0: nc.vector.tensor_tensor(out=ckvnT[:, kc0:kc0 + n], in0=ckv_sb[:, 0:n],
                                                                   in1=rstd_kv[:, 0:n], op=ALU.mult),
                 reads=[b_ckvsb, b_rkv], writes=[b_ckvn])
            for ti in range(len(tiles)):
                kt = kc0 // 128 + ti
                pb3 = nextbank()
                T.op(pe, lambda pb3=pb3, kt=kt: nc.tensor.matmul(bank(pb3), lhsT=ckvnT[:, kt * 128:(kt + 1) * 128], rhs=w_v,
                                                                start=True, stop=True), reads=[b_ckvn, b_ph0], writes=[PB[pb3]])
                T.op(act, lambda pb3=pb3, kt=kt: nc.scalar.copy(out=V_ext[:, kt, :, 0:64],
                                                                in_=bank(pb3).rearrange("p (h e) -> p h e", e=64)),
                     reads=[PB[pb3]], writes=[b_V])
            pr = nextbank(); pq = nextbank()

            def mm_kr(pr=pr, pq=pq, W=W, WR=WR, off=off, hT=hT, n=n):
                last = None
                for k in range(8):
                    last = nc.tensor.matmul(bank(pr, n, 96), lhsT=W[:, k, 320 + off:416 + off], rhs=hT[:, k, 0:n],
                                            start=(k == 0), stop=(k == 7))
                for k in range(8):
                    last = nc.tensor.matmul(bank(pq, n, 96), lhsT=WR[:, k, :], rhs=hT[:, k, 0:n],
                                            start=(k == 0), stop=(k == 7))
                return last
            T.op(pe, mm_kr, reads=[hT_b[hb], b_ph0], writes=[PB[pr], PB[pq]])
            T.op(dve, lambda pr=pr, n=n, zj=zj, hb=hb: nc.vector.scalar_tensor_tensor(
                out=t1[64:96, 0:n], in0=bank(pr, n)[64:96], scalar=zb[64:96, zj[1]:zj[1] + 1], in1=cos_ring[hb][64:96, 0:n],
                op0=ALU.add, op1=ALU.mult), reads=[PB[pr], tg_b[hb], b_ph0], writes=[b_t1])
            T.op(dve, lambda pq=pq, n=n, zj=zj, hb=hb: nc.vector.scalar_tensor_tensor(
                out=t2[64:96, 0:n], in0=bank(pq, n)[64:96], scalar=zb[64:96, zj[2]:zj[2] + 1], in1=sin_ring[hb][64:96, 0:n],
                op0=ALU.add, op1=ALU.mult), reads=[PB[pq], tg_b[hb], b_ph0], writes=[b_t2])
            T.op(dve, lambda n=n, kc0=kc0: nc.vector.tensor_tensor(out=krT[64:96, kc0:kc0 + n], in0=t1[64:96, 0:n],
                                                                   in1=t2[64:96, 0:n], op=ALU.add),
                 reads=[b_t1, b_t2], writes=[b_kr])
            if kind != "own":
                continue
            for c in range(2):
                pc = nextbank()

                def mm_cq(pc=pc, c=c, hT=hT):
                    last = None
                    for k in range(8):
                        last = nc.tensor.matmul(bank(pc), lhsT=w_inL[:, k, c * 128:(c + 1) * 128], rhs=hT[:, k, :],
                                                start=(k == 0), stop=(k == 7))
                    return last
                T.op(pe, mm_cq, reads=[hT_b[hb], b_ph0], writes=[PB[pc]])
                T.op(act, lambda pc=pc, c=c: nc.scalar.activation(out=cq_sb[:, c, :], in_=bank(pc), func=AF.Identity,
                                                                  bias=zb[:, c:c + 1]), reads=[PB[pc], b_ph0], writes=[b_cqsb])
                T.op(act, lambda c=c: nc.scalar.activation(out=sq_q[:, c, :], in_=cq_sb[:, c, :], func=AF.Square),
                     reads=[b_cqsb], writes=[b_sqq])
            pss = nextbank()

            def mm_ssq(pss=pss):
                nc.tensor.matmul(bank(pss), lhsT=onesF[:], rhs=sq_q[:, 0, :], start=True, stop=False)
                return nc.tensor.matmul(bank(pss), lhsT=onesF[:], rhs=sq_q[:, 1, :], start=False, stop=True)
            T.op(pe, mm_ssq, reads=[b_sqq, b_const], writes=[PB[pss]])
            rstd_bcast(pss, 512, rstd_q, 1.0 / 256, [PB[pss]], [b_rq])
            for c in range(2):
                T.op(dve, lambda c=c: nc.vector.tensor_tensor(out=cqn[:, c, :], in0=cq_sb[:, c, :], in1=rstd_q,
                                                              op=ALU.mult), reads=[b_cqsb, b_rq], writes=[b_cqn])
            for h in range(H):
                pr = nextbank(); pq = nextbank()

                def mm_q(pr=pr, pq=pq, h=h):
                    last = None
                    for c in range(2):
                        last = nc.tensor.matmul(bank(pr, 512, 96), lhsT=w_uqS[:, c, h * 96:(h + 1) * 96], rhs=cqn[:, c, :],
                                                start=(c == 0), stop=(c == 1))
                    for c in range(2):
                        last = nc.tensor.matmul(bank(pq, 512, 96), lhsT=w_uqR[:, c, h * 96:(h + 1) * 96], rhs=cqn[:, c, :],
                                                start=(c == 0), stop=(c == 1))
                    return last
                T.op(pe, mm_q, reads=[b_cqn, b_ph0], writes=[PB[pr], PB[pq]])
                T.op(act, lambda pr=pr, h=h, kc0=kc0: nc.scalar.copy(out=QT_all[0:64, h, kc0:kc0 + 512], in_=bank(pr)[0:64]),
                     reads=[PB[pr]], writes=[b_QT])
                T.op(dve, lambda pr=pr, hb=hb: nc.vector.tensor_tensor(out=t1[64:96, :], in0=bank(pr)[64:96],
                                                                       in1=cos_ring[hb][64:96, :], op=ALU.mult),
                     reads=[PB[pr], tg_b[hb]], writes=[b_t1])
                T.op(dve, lambda pq=pq, hb=hb: nc.vector.tensor_tensor(out=t2[64:96, :], in0=bank(pq)[64:96],
                                                                       in1=sin_ring[hb][64:96, :], op=ALU.mult),
                     reads=[PB[pq], tg_b[hb]], writes=[b_t2])
                T.op(dve, lambda h=h, kc0=kc0: nc.vector.tensor_tensor(out=QT_all[64:96, h, kc0:kc0 + 512], in0=t1[64:96, :],
                                                                       in1=t2[64:96, :], op=ALU.add),
                     reads=[b_t1, b_t2], writes=[b_QT])
        T.barrier()

        KTs = [AR.view((88 + 9 * i) * KB, [96, NK], BF16) for i in range(2)]
        PT = [AR.view((106 + 2 * i) * KB, [128, 1024], BF16) for i in range(3)]
        att_tok = AR.view(112 * KB, [128, 16, 512], BF16)
        catT = AR.view(128 * KB, [128, 8, NQ], BF16)
        b_KT = [Buf("KT0"), Buf("KT1")]
        b_PT = [Buf(f"PT{i}") for i in range(3)]
        b_att = Buf("att"); b_cat = Buf("cat"); b_rc = Buf("rc")
        rc = smallA

        for i in range(2):
            T.op(pool, lambda i=i: nc.gpsimd.tensor_copy(out=KTs[i][64:96, :], in_=krT[64:96, :]), reads=[b_kr], writes=[b_KT[i]])

        kgroups = [(g * 512, 512) for g in range(8)] + [(4096, 256)]

        def build_KT(h):
            for gi2, (k0, n) in enumerate(kgroups):
                pb = 6 + (gi2 % 2)
                T.op(pe, lambda pb=pb, k0=k0, n=n: nc.tensor.matmul(bank(pb, n, 64), lhsT=w_kn[:, h, :], rhs=ckvnT[:, k0:k0 + n],
                                                                    start=True, stop=True), reads=[b_ckvn, b_ph0], writes=[PB[pb]])
                T.op(dve, lambda pb=pb, k0=k0, n=n: nc.vector.tensor_copy(out=KTs[h % 2][0:64, k0:k0 + n], in_=bank(pb, n, 64)),
                     reads=[PB[pb]], writes=[b_KT[h % 2]])

        build_KT(0)
        it = 0
        for h in range(H):
            if h + 1 < H:
                build_KT(h + 1)
            KT = KTs[h % 2]
            for qg in range(2):
                q0 = qg * 1024

                def S_mm(kt, sb_):
                    def f():
                        nc.tensor.matmul(bank(sb_), lhsT=KT[:, kt * 128:(kt + 1) * 128], rhs=QT_all[:, h, q0:q0 + 512],
                                         start=True, stop=True)
                        return nc.tensor.matmul(bank(sb_ + 1), lhsT=KT[:, kt * 128:(kt + 1) * 128],
                                                rhs=QT_all[:, h, q0 + 512:q0 + 1024], start=True, stop=True)
                    T.op(pe, f, reads=[b_KT[h % 2], b_QT], writes=[PB[sb_], PB[sb_ + 1]])

                S_mm(0, 0)
                for kt in range(NKT):
                    sb_ = 2 * (kt % 2)
                    if kt + 1 < NKT:
                        S_mm(kt + 1, 2 * ((kt + 1) % 2))
                    pi = it % 3; it += 1
                    T.op(act, lambda sb_=sb_, pi=pi: nc.scalar.activation(out=PT[pi], in_=bank(sb_, nb=2), func=AF.Exp, scale=SCALE),
                         reads=[PB[sb_], PB[sb_ + 1]], writes=[b_PT[pi]])

                    def PV(kt=kt, pi=pi):
                        last = None
                        for j in range(8):
                            ob = 4 + j // 4
                            last = nc.tensor.matmul(bank(ob)[:, (j % 4) * 65:(j % 4) * 65 + 65],
                                                    lhsT=PT[pi][:, j * 128:(j + 1) * 128], rhs=V_ext[:, kt, h, :],
                                                    start=(kt == 0 and j % 4 == 0), stop=(kt == NKT - 1),
                                                    skip_group_check=True)
                        return last
                    T.op(pe, PV, reads=[b_PT[pi], b_V], writes=[PB[4], PB[5]])
                for ob in (4, 5):
                    ov = bank(ob, 260).rearrange("p (s e) -> p s e", e=65)
                    T.op(dve, lambda ov=ov, ob=ob: nc.vector.reciprocal(out=rc[:, (ob - 4) * 4:(ob - 4) * 4 + 4], in_=ov[:, :, 64]),
                         reads=[PB[ob]], writes=[b_rc])
                    for s in range(4):
                        j = (ob - 4) * 4 + s
                        T.op(dve, lambda ov=ov, s=s, j=j: nc.vector.tensor_scalar(
                            out=att_tok[:, qg * 8 + j, h * 64:(h + 1) * 64], in0=ov[:, s, 0:64], scalar1=rc[:, j:j + 1],
                            scalar2=None, op0=ALU.mult), reads=[PB[ob], b_rc], writes=[b_att])
        for qt in range(16):
            tb = 6 + (qt % 2)
            tpv = bank(tb).bitcast(BF16)

            def tr(qt=qt, tpv=tpv):
                last = None
                for c in range(4):
                    last = nc.tensor.transpose(out=tpv[:, c * 128:(c + 1) * 128], in_=att_tok[:, qt, c * 128:(c + 1) * 128],
                                               identity=identB[:])
                return last
            T.op(pe, tr, reads=[b_att, b_const], writes=[PB[tb]])
            T.op(dve, lambda qt=qt, tpv=tpv: nc.vector.tensor_copy(out=catT[:, 0:4, qt * 128:(qt + 1) * 128],
                                                                   in_=tpv[:, 0:512].rearrange("p (c t) -> p c t", t=128)),
                 reads=[PB[tb]], writes=[b_cat])
        T.barrier()

        x_ring = [AR.view(4 * i * KB, [128, D], F32) for i in range(3)]
        xn_ring = [AR.view((12 + 2 * i) * KB, [128, D], BF16) for i in range(2)]
        hT_ring = [AR.view((16 + 8 * i) * KB, [128, 8, 512], BF16) for i in range(2)]
        w_inU = AR.view(32 * KB, [128, 8, 512], BF16)
        NU = NQ + 16
        uT = AR.view(40 * KB, [128, 4, NU], F32)
        tmpA = AR.view(73 * KB, [128, NU], F32)
        tmpB = AR.view(82 * KB, [128, NU], F32)
        mixB = [AR.view((91 + 4 * i) * KB, [128, NQ], BF16) for i in range(2)]
        w_poolB = AR.view(99 * KB, [128, 4, 128], BF16)
        winu_f = AR.view(100 * KB, [128, 8, 512], F32)
        b_wu = Buf("wu"); b_uT = Buf("uT"); b_tA = Buf("tA"); b_tB = Buf("tB"); b_mix = [Buf("mix0"), Buf("mix1")]
        ldP = T.mksem("ldP")
        b_ldP = Buf("ldP")
        T.dma(sp, winu_f, w_in.rearrange("(k q) f -> q k f", q=128)[:, :, 416:928], ldP, writes=[b_ldP])
        T.dma(pool, w_poolB, w_pool.rearrange("g c d -> c g d"), ldP, writes=[b_ldP])
        for k in range(8):
            T.op(dve, lambda k=k: nc.vector.tensor_scalar(out=w_inU[:, k, :], in0=winu_f[:, k, :], scalar1=G1L[:, k:k + 1],
                                                          scalar2=None, op0=ALU.mult), reads=[b_ldP, b_ph0], writes=[b_wu])
        pgroups = [("halo", [16, 17])] + [("own", [4 * g + i for i in range(4)]) for g in range(4)]
        og = 0
        def norm_group_p(gi):
            for ti, t in enumerate(pgroups[gi][1]):
                norm_tile_to_hT(x_ring, xn_ring, t, hT_ring[gi % 2], hT_b[gi % 2], ti * 128, (0, 1))

        norm_group_p(0)
        for gi, (kind, tiles) in enumerate(pgroups):
            n = 128 * len(tiles)
            hb = gi % 2
            hT = hT_ring[hb]
            if gi + 1 < len(pgroups):
                norm_group_p(gi + 1)
            for g in range(4):
                pb = nextbank()

                def mm_u(pb=pb, g=g, hT=hT, n=n):
                    last = None
                    for k in range(8):
                        last = nc.tensor.matmul(bank(pb, n), lhsT=w_inU[:, k, g * 128:(g + 1) * 128], rhs=hT[:, k, 0:n],
                                                start=(k == 0), stop=(k == 7))
                    return last
                T.op(pe, mm_u, reads=[hT_b[hb], b_wu], writes=[PB[pb]])
                if kind == "own":
                    c0 = 8 + og * 512
                    T.op(act, lambda pb=pb, g=g, c0=c0: nc.scalar.activation(out=uT[:, g, c0:c0 + 512], in_=bank(pb),
                                                                             func=AF.Identity, bias=zb[:, 5 + g:6 + g]),
                         reads=[PB[pb], b_ph0], writes=[b_uT])
                else:
                    T.op(dve, lambda pb=pb, g=g: nc.vector.scalar_tensor_tensor(
                        out=uT[:, g, 0:8], in0=bank(pb)[:, 120:128], scalar=zb[:, 5 + g:6 + g], in1=hmask[:, 0:8],
                        op0=ALU.add, op1=ALU.mult), reads=[PB[pb], b_ph0, b_const], writes=[b_uT])
                    T.op(dve, lambda pb=pb, g=g: nc.vector.scalar_tensor_tensor(
                        out=uT[:, g, NU - 8:NU], in0=bank(pb)[:, 128:136], scalar=zb[:, 5 + g:6 + g], in1=hmask[:, 8:16],
                        op0=ALU.add, op1=ALU.mult), reads=[PB[pb], b_ph0, b_const], writes=[b_uT])
            if kind == "own":
                og += 1

        def tt_add(e, o, a, b_, rb, wb):
            if e is pool:
                T.op(pool, lambda: nc.gpsimd.tensor_tensor(out=o, in0=a, in1=b_, op=ALU.add), reads=rb, writes=wb)
            else:
                T.op(dve, lambda: nc.vector.tensor_tensor(out=o, in0=a, in1=b_, op=ALU.add), reads=rb, writes=wb)

        LO, HI = 8, 8 + NQ
        for g in range(4):
            U = uT[:, g, :]
            wwin = (2, 4, 8, 16)[g]
            eng = dve
            if g == 0:
                tt_add(eng, tmpA[:, LO:HI], U[:, LO - 1:HI - 1], U[:, LO:HI], [b_uT], [b_tA]); S = tmpA; bS = b_tA
            else:
                tt_add(eng, tmpA[:, 1:NU], U[:, 0:NU - 1], U[:, 1:NU], [b_uT], [b_tA])
                if g == 1:
                    tt_add(eng, tmpB[:, LO:HI], tmpA[:, LO - 1:HI - 1], tmpA[:, LO + 1:HI + 1], [b_tA], [b_tB]); S = tmpB; bS = b_tB
                else:
                    tt_add(eng, tmpB[:, 2:NU - 1], tmpA[:, 1:NU - 2], tmpA[:, 3:NU], [b_tA], [b_tB])
                    if g == 2:
                        tt_add(eng, tmpA[:, LO:HI], tmpB[:, LO - 2:HI - 2], tmpB[:, LO + 2:HI + 2], [b_tB], [b_tA]); S = tmpA; bS = b_tA
                    else:
                        tt_add(eng, tmpA[:, 4:NU - 3], tmpB[:, 2:NU - 5], tmpB[:, 6:NU - 1], [b_tB], [b_tA])
                        tt_add(eng, tmpB[:, LO:HI], tmpA[:, LO - 4:HI - 4], tmpA[:, LO + 4:HI + 4], [b_tA], [b_tB]); S = tmpB; bS = b_tB
            mb = mixB[g % 2]
            T.op(dve, lambda S=S, U=U, mb=mb, wwin=wwin: nc.vector.scalar_tensor_tensor(
                out=mb, in0=S[:, LO:HI], scalar=1.0 / wwin, in1=U[:, LO:HI], op0=ALU.mult, op1=ALU.subtract),
                reads=[bS, b_uT], writes=[b_mix[g % 2]])
            for (a0, i0) in ((0, 0), (NQ - 8, 8)):
                T.op(dve, lambda S=S, a0=a0, i0=i0, g=g: nc.vector.tensor_tensor(
                    out=smallA[:, 16:24], in0=S[:, LO + a0:LO + a0 + 8], in1=invc[:, g, i0:i0 + 8], op=ALU.mult),
                    reads=[bS, b_const], writes=[b_rc])
                T.op(dve, lambda U=U, a0=a0, mb=mb: nc.vector.tensor_tensor(
                    out=mb[:, a0:a0 + 8], in0=smallA[:, 16:24], in1=U[:, LO + a0:LO + a0 + 8], op=ALU.subtract),
                    reads=[b_rc, b_uT], writes=[b_mix[g % 2]])
            for tg in range(4):
                pb = nextbank()
                T.op(pe, lambda pb=pb, g=g, mb=mb, tg=tg: nc.tensor.matmul(bank(pb), lhsT=w_poolB[:, g, :],
                                                                          rhs=mb[:, tg * 512:(tg + 1) * 512], start=True, stop=True),
                     reads=[b_mix[g % 2], b_ldP], writes=[PB[pb]])
                T.op(act, lambda pb=pb, g=g, tg=tg: nc.scalar.activation(out=catT[:, 4 + g, tg * 512:(tg + 1) * 512], in_=bank(pb),
                                                                         func=AF.Copy, scale=pscol[:, g:g + 1]),
                     reads=[PB[pb], b_const], writes=[b_cat])
        T.barrier()
        w_outS = AR.view(0, [128, 8, D], BF16)
        wo_stg = [AR.view((16 + 4 * i) * KB, [128, D], F32) for i in range(2)]
        x2_ring = [AR.view((24 + 4 * i) * KB, [128, D], F32) for i in range(2)]
        xnew = [AR.view((32 + 4 * i) * KB, [128, D], F32) for i in range(2)]
        xn2 = [AR.view((40 + 4 * i) * KB, [128, D], F32) for i in range(2)]
        h2Tf = [AR.view((48 + 4 * i) * KB, [128, 8, 128], F32) for i in range(2)]
        h2tok = [AR.view((56 + 2 * i) * KB, [128, D], BF16) for i in range(2)]
        G2B = AR.view(60 * KB, [128, D], F32)
        sh2B = AR.view(64 * KB, [128, D], F32)
        g1B = AR.view(68 * KB, [128, D], F32)
        tmpF = AR.view(72 * KB, [128, D], F32)
        b_wo = Buf("wo"); b_g1B = Buf("g1B"); b_GB = Buf("GB")
        wo_sem = [T.mksem(f"wo{i}") for i in range(2)]; b_wos = [Buf("wos0"), Buf("wos1")]
        x2_sem = [T.mksem(f"x2{i}") for i in range(2)]; b_x2 = [Buf("x20"), Buf("x21")]
        b_xnew = [Buf("xnew0"), Buf("xnew1")]
        b_xn2 = [Buf("xn20"), Buf("xn21")]; b_h2Tf = [Buf("h2Tf0"), Buf("h2Tf1")]
        b_h2tok = [Buf("h2tok0"), Buf("h2tok1")]; b_tmpF = Buf("tmpF")
        h2_sem = [T.mksem(f"h2s{i}") for i in range(2)]
        xw_sem = [T.mksem(f"xw{i}") for i in range(2)]
        sc_sem = T.mksem("scat")
        b_sm = Buf("sm"); b_rt = Buf("rt"); b_cum = Buf("cum")

        logit = sb("logit", [128, NE]); m8 = sb("m8", [128, 8]); idx8 = sb("idx8", [128, 8], U32)
        e4 = sb("e4", [128, 4]); gk4 = sb("gk4", [128, 4]); ekf = sb("ekf", [128, 4]); rkk = sb("rkk", [128, 4])
        slotf = sb("slotf", [128, 4]); gsum = sb("gsum", [128, 4]); gsumF = sb("gsumF", [128, 2])
        oh = sb("oh", [128, 4, NE]); Mb = sb("Mb", [128, NE], BF16); M2 = sb("M2", [128, 2, NE])
        gd = sb("gd", [128, NE]); rk = sb("rk", [128, NE]); cum = sb("cum", [128, NE]); junk32 = sb("junk32", [128, NE])
        gT = sb("gT", [32, 128])
        gd_all = sb("gd_all", [128, 16, NE]); gk_all = sb("gk_all", [128, 16, 4]); slot_all = sb("slot_all", [128, 16, 4], I32)
        cnt_i = sb("cnt_i", [128, NE + 1], I32); cmaxf = sb("cmaxf", [128, 1])

        T.op(dve, lambda: nc.vector.memset(cum[:], 0.0), writes=[b_cum])
        for hf in range(2):
            for (row, dst, bb) in ((g1row[0:1, :], g1B, b_g1B), (rows2[0:1, 0, :], G2B, b_GB), (rows2[0:1, 1, :], sh2B, b_GB)):
                pb = nextbank()
                T.op(pe, lambda pb=pb, hf=hf, row=row: nc.tensor.matmul(bank(pb), lhsT=onesF[0:1, :], rhs=row[:, hf * 512:(hf + 1) * 512],
                                                                        start=True, stop=True), reads=[b_ph0, b_const], writes=[PB[pb]])
                T.op(dve, lambda pb=pb, hf=hf, dst=dst: nc.vector.tensor_copy(out=dst[:, hf * 512:(hf + 1) * 512], in_=bank(pb)),
                     reads=[PB[pb]], writes=[bb])
        for k in range(8):
            s = k % 2
            T.dma(sp, wo_stg[s], w_out[k * 128:(k + 1) * 128, :], wo_sem[s], writes=[b_wos[s]])
            T.op(dve, lambda k=k, s=s: nc.vector.tensor_tensor(out=w_outS[:, k, :], in0=wo_stg[s], in1=g1B, op=ALU.mult),
                 reads=[b_wos[s], b_g1B], writes=[b_wo])

        b_smF = Buf("smF")

        def front_2a(qt):
            s = qt % 2
            T.dma(sp, x2_ring[s], xe[qt * 128:(qt + 1) * 128, :], x2_sem[s], writes=[b_x2[s]])
            for hf in range(2):
                pb = nextbank()

                def mm_o(pb=pb, hf=hf, qt=qt):
                    last = None
                    for k in range(8):
                        last = nc.tensor.matmul(bank(pb), lhsT=catT[:, k, qt * 128:(qt + 1) * 128],
                                                rhs=w_outS[:, k, hf * 512:(hf + 1) * 512], start=(k == 0), stop=(k == 7))
                    return last
                T.op(pe, mm_o, reads=[b_cat, b_wo], writes=[PB[pb]])
                T.op(dve, lambda pb=pb, hf=hf, s=s: nc.vector.tensor_tensor(
                    out=xnew[s][:, hf * 512:(hf + 1) * 512], in0=bank(pb), in1=x2_ring[s][:, hf * 512:(hf + 1) * 512], op=ALU.add),
                    reads=[PB[pb], b_x2[s]], writes=[b_xnew[s]])
            T.op(act, lambda s=s: nc.scalar.activation(out=junk[:], in_=xnew[s], func=AF.Square, accum_out=gsumF[:, 0:1]),
                 reads=[b_xnew[s]], writes=[b_smF])
            T.op(act, lambda: nc.scalar.activation(out=gsumF[:, 1:2], in_=gsumF[:, 0:1], func=AF.Sqrt, scale=1.0 / D,
                                                   bias=epsT[:]), reads=[b_smF], writes=[b_smF])
            T.op(dve, lambda: nc.vector.reciprocal(out=gsumF[:, 1:2], in_=gsumF[:, 1:2]), reads=[b_smF], writes=[b_smF])
            T.op(dve, lambda s=s: nc.vector.scalar_tensor_tensor(out=tmpF, in0=xnew[s], scalar=gsumF[:, 1:2], in1=G2B,
                                                                 op0=ALU.mult, op1=ALU.mult),
                 reads=[b_xnew[s], b_smF, b_GB], writes=[b_tmpF])
            T.op(dve, lambda s=s: nc.vector.tensor_tensor(out=xn2[s], in0=tmpF, in1=sh2B, op=ALU.add),
                 reads=[b_tmpF, b_GB], writes=[b_xn2[s]])
            T.op(act, lambda s=s: nc.scalar.copy(out=h2tok[s], in_=xn2[s]), reads=[b_xn2[s]], writes=[b_h2tok[s]])
            T.dma(act, h2d[qt * 128:(qt + 1) * 128, :], h2tok[s], h2_sem[s], reads=[b_h2tok[s]])

            def tr2(s=s):
                last = None
                for c in range(8):
                    last = nc.tensor.transpose(out=bank(c // 4)[:, (c % 4) * 128:(c % 4) * 128 + 128],
                                               in_=xn2[s][:, c * 128:(c + 1) * 128], identity=identF[:])
                return last
            T.op(pe, tr2, reads=[b_xn2[s], b_const], writes=[PB[0], PB[1]])
            T.op(act, lambda s=s: nc.scalar.copy(out=h2Tf[s], in_=bank(0, nb=2).rearrange("p (c t) -> p c t", t=128)),
                 reads=[PB[0], PB[1]], writes=[b_h2Tf[s]])

        logits_all = AR.view(76 * KB, [128, 16, NE], F32)
        m8_all = AR.view(78 * KB, [128, 16, 8], F32)
        idx8_all = AR.view(79 * KB, [128, 16, 8], U32)
        oh_all = AR.view(84 * KB, [128, 16, 4, NE], F32)
        ohw = AR.view(92 * KB, [128, 16, 4, NE], F32)
        rank_sb = AR.view(100 * KB, [128, 16, NE], F32)
        Mb_all = AR.view(102 * KB, [128, 16, NE], BF16)
        d4 = sb("d4", [128, 16, 4]); e4a = sb("e4a", [128, 16, 4]); gk4a = sb("gk4a", [128, 16, 4]); ekfa = sb("ekfa", [128, 16, 4])
        s4 = sb("s4", [128, 16]); rkka = sb("rkka", [128, 16, 4]); slotfa = sb("slotfa", [128, 16, 4])
        b_lg = Buf("lg")

        def router_2a(qt):
            s = qt % 2
            pbr = nextbank()

            def mm_r(pbr=pbr, s=s):
                last = None
                for c in range(8):
                    last = nc.tensor.matmul(bank(pbr, NE), lhsT=h2Tf[s][:, c, :], rhs=rwt[:, c, :], start=(c == 0), stop=(c == 7))
                return last
            T.op(pe, mm_r, reads=[b_h2Tf[s], b_const], writes=[PB[pbr]])
            T.op(dve, lambda pbr=pbr: nc.vector.tensor_tensor(out=logits_all[:, qt, :], in0=bank(pbr, NE), in1=rbB[:], op=ALU.add),
                 reads=[PB[pbr], b_const], writes=[b_lg])
            T.dma(act, xnew_d[qt * 128:(qt + 1) * 128, :], xnew[s], xw_sem[s], reads=[b_xnew[s]])

        front_2a(0)
        for qt in range(16):
            if qt + 1 < 16:
                front_2a(qt + 1)
            router_2a(qt)

        X = mybir.AxisListType.X
        for t in range(16):
            T.op(dve, lambda t=t: nc.vector.max(out=m8_all[:, t, :], in_=logits_all[:, t, :]), reads=[b_lg], writes=[b_rt])
        for t in range(16):
            T.op(dve, lambda t=t: nc.vector.max_index(out=idx8_all[:, t, :], in_max=m8_all[:, t, :], in_values=logits_all[:, t, :]),
                 reads=[b_lg, b_rt], writes=[b_rt])
        T.op(dve, lambda: nc.vector.tensor_tensor(out=d4[:], in0=m8_all[:, :, 0:4], in1=m8_all[:, :, 0:1].broadcast_to([128, 16, 4]),
                                                  op=ALU.subtract), reads=[b_rt], writes=[b_rt])
        T.op(act, lambda: nc.scalar.activation(out=e4a[:], in_=d4[:], func=AF.Exp), reads=[b_rt], writes=[b_rt])
        T.op(dve, lambda: nc.vector.tensor_reduce(out=s4[:], in_=e4a[:], axis=X, op=ALU.add), reads=[b_rt], writes=[b_rt])
        T.op(dve, lambda: nc.vector.reciprocal(out=s4[:], in_=s4[:]), reads=[b_rt], writes=[b_rt])
        T.op(dve, lambda: nc.vector.tensor_tensor(out=gk4a[:], in0=e4a[:], in1=s4[:].unsqueeze(2).broadcast_to([128, 16, 4]),
                                                  op=ALU.mult), reads=[b_rt], writes=[b_rt])
        T.op(dve, lambda: nc.vector.tensor_scalar(out=gk_all[:], in0=gk4a[:], scalar1=1.0 / 1.702, scalar2=None, op0=ALU.mult),
             reads=[b_rt], writes=[b_rt])
        T.op(dve, lambda: nc.vector.tensor_copy(out=ekfa[:], in_=idx8_all[:, :, 0:4]), reads=[b_rt], writes=[b_rt])
        T.op(dve, lambda: nc.vector.tensor_tensor(out=oh_all, in0=iota32[:].unsqueeze(1).unsqueeze(1).broadcast_to([128, 16, 4, NE]),
                                                  in1=ekfa[:].unsqueeze(3).broadcast_to([128, 16, 4, NE]), op=ALU.is_equal),
             reads=[b_rt, b_const], writes=[b_rt])
        T.op(dve, lambda: nc.vector.tensor_reduce(out=rank_sb, in_=oh_all.rearrange("p t k e -> p t e k"), axis=X, op=ALU.add),
             reads=[b_rt], writes=[b_rt])
        T.op(dve, lambda: nc.vector.tensor_copy(out=Mb_all, in_=rank_sb), reads=[b_rt], writes=[b_rt])
        T.op(dve, lambda: nc.vector.tensor_tensor(out=ohw, in0=oh_all, in1=gk4a[:].unsqueeze(3).broadcast_to([128, 16, 4, NE]),
                                                  op=ALU.mult), reads=[b_rt], writes=[b_rt])
        T.op(dve, lambda: nc.vector.tensor_reduce(out=gd_all[:], in_=ohw.rearrange("p t k e -> p t e k"), axis=X, op=ALU.add),
             reads=[b_rt], writes=[b_rt])

        def mm_rank():
            last = None
            for t in range(16):
                nc.tensor.matmul(bank(2)[:, t * NE:(t + 1) * NE], lhsT=UstrB[:], rhs=Mb_all[:, t, :], start=True, stop=(t == 0),
                                 skip_group_check=True)
                for t2 in range(t):
                    last = nc.tensor.matmul(bank(2)[:, t * NE:(t + 1) * NE], lhsT=onesB[:], rhs=Mb_all[:, t2, :], start=False,
                                            stop=(t2 == t - 1), skip_group_check=True)
            for t in range(16):
                last = nc.tensor.matmul(bank(3, NE), lhsT=onesB[:], rhs=Mb_all[:, t, :], start=(t == 0), stop=(t == 15))
            return last
        T.op(pe, mm_rank, reads=[b_rt, b_c2], writes=[PB[2], PB[3]])
        T.op(dve, lambda: nc.vector.tensor_copy(out=rank_sb, in_=bank(2).rearrange("p (t e) -> p t e", e=NE)), reads=[PB[2]], writes=[b_rt])
        T.op(dve, lambda: nc.vector.tensor_copy(out=cum[:], in_=bank(3, NE)), reads=[PB[3], b_cum], writes=[b_cum])
        T.op(dve, lambda: nc.vector.tensor_tensor(out=ohw, in0=oh_all, in1=rank_sb.unsqueeze(2).broadcast_to([128, 16, 4, NE]),
                                                  op=ALU.mult), reads=[b_rt], writes=[b_rt])
        T.op(dve, lambda: nc.vector.tensor_reduce(out=rkka[:], in_=ohw, axis=X, op=ALU.add), reads=[b_rt], writes=[b_rt])
        T.op(dve, lambda: nc.vector.scalar_tensor_tensor(out=slotfa[:], in0=ekfa[:], scalar=2048.0, in1=rkka[:],
                                                         op0=ALU.mult, op1=ALU.add), reads=[b_rt], writes=[b_rt])
        b_slot = Buf("slot")
        T.op(dve, lambda: nc.vector.tensor_copy(out=slot_all[:], in_=slotfa[:]), reads=[b_rt], writes=[b_slot])
        for qt in range(16):
            for k in range(4):
                T._waits(pool, T._deps([b_slot, b_tokl0, b_const], []))
                inst = nc.gpsimd.indirect_dma_start(out=tokl, out_offset=bass.IndirectOffsetOnAxis(ap=slot_all[:, qt, k:k + 1], axis=0),
                                                    in_=tokrow[:, qt, :], in_offset=None)
                sc_sem.cnt += 16
                inst.then_inc(sc_sem.h, 16)
                T._update((sc_sem, sc_sem.cnt), [b_slot, b_tokl0, b_const], [])
        nt_sem = T.mksem("nts")
        b_nt = Buf("nt")
        cntf = sb("cntf", [128, NE + 1])
        T.op(dve, lambda: nc.vector.tensor_reduce(out=cmaxf[:], in_=cum[:], axis=mybir.AxisListType.X, op=ALU.max),
             reads=[b_cum], writes=[b_nt])
        T.op(dve, lambda: nc.vector.tensor_scalar(out=cntf[:, 0:NE], in0=cum[:], scalar1=-1.0, scalar2=4096.0,
                                                  op0=ALU.mult, op1=ALU.add), reads=[b_cum, b_nt], writes=[b_nt])
        T.op(dve, lambda: nc.vector.tensor_scalar(out=cntf[:, NE:NE + 1], in0=cmaxf[:], scalar1=-1.0, scalar2=4096.0,
                                                  op0=ALU.mult, op1=ALU.add), reads=[b_nt], writes=[b_nt])
        T.op(dve, lambda: nc.vector.tensor_copy(out=cnt_i[:], in_=cntf[:]), reads=[b_nt], writes=[b_nt])
        T.dma(sp, nt_d, cnt_i[0:1, :], nt_sem, reads=[b_nt], writes=[b_nt])
        T.barrier()

        NS = 3
        Wgu = [AR.view(32 * i * KB, [128, 8, 2 * D], BF16) for i in range(NS)]
        Wdn = [AR.view((96 + 16 * i) * KB, [128, 8, D], BF16) for i in range(NS)]
        bias_reg = AR.view(144 * KB, [128, 2 * D], BF16)
        bgur = [bias_reg[32 * i:32 * i + 1, :] for i in range(NS)]
        xb = [AR.view((148 + 2 * i) * KB, [128, D], BF16) for i in range(2)]
        xbT = [AR.view((152 + 2 * i) * KB, [128, 8, 128], BF16) for i in range(2)]
        Ag = AR.view(156 * KB, [128, D], F32)
        Qs = AR.view(160 * KB, [128, D], BF16)
        Cp = AR.view(162 * KB, [128, D], BF16)
        actS = [AR.view((164 + 2 * i) * KB, [128, D], BF16) for i in range(2)]
        actT = [AR.view((168 + 2 * i) * KB, [128, 8, 128], BF16) for i in range(2)]
        ysb = [AR.view((172 + 4 * i) * KB, [128, D], F32) for i in range(2)]
        idxT = [sb(f"idxT{i}", [128, 2], I32) for i in range(2)]
        w_sem = [T.mksem(f"wg{i}") for i in range(NS)]
        i_sem = [T.mksem(f"is{i}") for i in range(2)]; b_idx = [Buf("idx0"), Buf("idx1")]
        g_sem = [T.mksem(f"gs{i}") for i in range(2)]; b_xb = [Buf("xb0"), Buf("xb1")]
        y_sem = [T.mksem(f"ys{i}") for i in range(2)]; b_ysb = [Buf("ysb0"), Buf("ysb1")]
        b_xbT = [Buf("xbT0"), Buf("xbT1")]; b_Ag = Buf("Ag"); b_Qs = Buf("Qs"); b_Cp = Buf("Cp")
        b_actS = [Buf("actS0"), Buf("actS1")]; b_actT = [Buf("actT0"), Buf("actT1")]
        owners = {}
        for i in range(2):
            owners[i_sem[i]] = sp; owners[y_sem[i]] = act; owners[g_sem[i]] = pool
        for i in range(NS):
            owners[w_sem[i]] = pool

        b_Wc = [[Buf(f"Wc{i}_{c}") for c in range(13)] for i in range(NS)]

        def load_chunks(e, lo, hi):
            s = e % NS
            for ci in range(lo, hi):
                if ci < 8:
                    T.dma(pool, Wgu[s][:, ci, :], w_gu[e][ci * 128:(ci + 1) * 128, :], w_sem[s], writes=[b_Wc[s][ci]])
                else:
                    k0 = (ci - 8) * 2
                    T.dma(pool, Wdn[s][:, k0:k0 + 2, :], w_dn[e].rearrange("(k q) n -> q k n", q=128)[:, k0:k0 + 2, :],
                          w_sem[s], writes=[b_Wc[s][ci]])
                if ci == 0:
                    T.dma(pool, bgur[s], b_gu[e:e + 1, :], w_sem[s], writes=[b_Wc[s][12]])

        for eng in T.engs:
            T._waits(eng, {nt_sem: nt_sem.cnt})
        cnt_regs = []
        for e in range(NE + 1):
            rs = nc.alloc_registers(f"cnt{e}")
            for reg in rs:
                nc.reg_load(reg, nt_d[0:1, e:e + 1])
            cnt_regs.append(rs)

        def piece_A(e, j, s, u):
            r0 = e * 2048 + j * 128
            T.dma(sp, idxT[u][:], tokl[r0:r0 + 128, :], i_sem[u], writes=[b_idx[u]])
            T._waits(pool, T._deps([b_idx[u]], [b_xb[u]]))
            inst = nc.gpsimd.indirect_dma_start(out=xb[u], out_offset=None, in_=h2d,
                                                in_offset=bass.IndirectOffsetOnAxis(ap=idxT[u][:, 0:1], axis=0))
            g_sem[u].cnt += 16
            inst.then_inc(g_sem[u].h, 16)
            T._update((g_sem[u], g_sem[u].cnt), [b_idx[u]], [b_xb[u]])
            tp0 = bank(0).bitcast(BF16)

            def tr_in():
                last = None
                for c in range(8):
                    last = nc.tensor.transpose(out=tp0[:, c * 128:(c + 1) * 128], in_=xb[u][:, c * 128:(c + 1) * 128],
                                               identity=identB[:])
                return last
            T.op(pe, tr_in, reads=[b_xb[u], b_const], writes=[PB[0]])
            T.op(dve, lambda: nc.vector.tensor_copy(out=xbT[u], in_=tp0.rearrange("p (c t) -> p c t", t=128)),
                 reads=[PB[0]], writes=[b_xbT[u]])

            def mm_gu(c0):
                last = None
                for c in range(c0, c0 + 2):
                    nc.tensor.matmul(bank(1 + c), lhsT=onesB[32 * s:32 * s + 1, :], rhs=bgur[s][:, c * 512:(c + 1) * 512],
                                     start=True, stop=False)
                    for k in range(8):
                        last = nc.tensor.matmul(bank(1 + c), lhsT=xbT[u][:, k, :], rhs=Wgu[s][:, k, c * 512:(c + 1) * 512],
                                                start=False, stop=(k == 7))
                return last
            T.op(pe, lambda: mm_gu(0), reads=[b_xbT[u], b_c2] + b_Wc[s], writes=[PB[1], PB[2]])
            T.op(dve, lambda: nc.vector.tensor_scalar(out=Ag, in0=bank(1, nb=2), scalar1=7.0, scalar2=None, op0=ALU.min),
                 reads=[PB[1], PB[2]], writes=[b_Ag])
            T.op(pe, lambda: mm_gu(2), reads=[b_xbT[u], b_c2] + b_Wc[s], writes=[PB[3], PB[4]])
            T.op(act, lambda: nc.scalar.activation(out=Qs, in_=Ag, func=AF.Silu, scale=1.702), reads=[b_Ag], writes=[b_Qs])

        def piece_B(e, j, s, u):
            T.op(dve, lambda: nc.vector.tensor_scalar(out=Cp, in0=bank(3, nb=2), scalar1=1.0, scalar2=8.0,
                                                      op0=ALU.add, op1=ALU.min), reads=[PB[3], PB[4]], writes=[b_Cp])
            T.op(dve, lambda: nc.vector.scalar_tensor_tensor(out=actS[u], in0=Cp, scalar=-6.0, in1=Qs,
                                                             op0=ALU.max, op1=ALU.mult),
                 reads=[b_Cp, b_Qs], writes=[b_actS[u]])

        def piece_C(e, j, s, u):
            tp5 = bank(5).bitcast(BF16)

            def tr_act():
                last = None
                for c in range(8):
                    last = nc.tensor.transpose(out=tp5[:, c * 128:(c + 1) * 128], in_=actS[u][:, c * 128:(c + 1) * 128],
                                               identity=identB[:])
                return last
            T.op(pe, tr_act, reads=[b_actS[u], b_const], writes=[PB[5]])
            T.op(dve, lambda: nc.vector.tensor_copy(out=actT[u], in_=tp5.rearrange("p (c t) -> p c t", t=128)),
                 reads=[PB[5]], writes=[b_actT[u]])

        def piece_D(e, j, s, u):
            r0 = e * 2048 + j * 128

            def mm_dn():
                last = None
                for hf in range(2):
                    for k in range(8):
                        last = nc.tensor.matmul(bank(6 + hf), lhsT=actT[u][:, k, :], rhs=Wdn[s][:, k, hf * 512:(hf + 1) * 512],
                                                start=(k == 0), stop=(k == 7))
                return last
            T.op(pe, mm_dn, reads=[b_actT[u]] + b_Wc[s], writes=[PB[6], PB[7]])
            T.op(act, lambda: nc.scalar.copy(out=ysb[u], in_=bank(6, nb=2)), reads=[PB[6], PB[7]], writes=[b_ysb[u]])
            T.dma(act, Yd[r0:r0 + 128, :], ysb[u], y_sem[u], reads=[b_ysb[u]])

        def cond(fn, e, j, s, u):
            snap = T.cond_snapshot()
            with nc.If_lt(cnt_regs[e], 4096 - 128 * j):
                fn(e, j, s, u)
            with nc.Else():
                T.cond_compensate(snap, owners)
            T.cond_restore_seen(snap)

        def expert_tiles(e, s, j0, j1):
            for j in range(j0, j1 + 1):
                if j < j1:
                    cond(piece_A, e, j, s, j % 2)
                if j > j0:
                    cond(piece_C, e, j - 1, s, (j - 1) % 2)
                if j < j1:
                    cond(piece_B, e, j, s, j % 2)
                if j > j0:
                    cond(piece_D, e, j - 1, s, (j - 1) % 2)
                if j0 == 0 and 1 <= j <= 3 and e + 2 < NE and TPP == 4:
                    load_chunks(e + 2, 4 * (j - 1), 4 * j)

        load_chunks(0, 0, 12)
        load_chunks(1, 0, 12)
        for e in range(NE):
            expert_tiles(e, e % NS, 0, TPP)
            if TPP != 4 and e + 2 < NE:
                load_chunks(e + 2, 0, 12)
        for r in range(1, 16 // TPP):
            gsnap = T.cond_snapshot()
            with nc.If_lt(cnt_regs[NE], 4096 - 128 * TPP * r):
                for e in range(NE):
                    esnap = T.cond_snapshot()
                    with nc.If_lt(cnt_regs[e], 4096 - 128 * TPP * r):
                        load_chunks(e, 0, 12)
                        expert_tiles(e, e % NS, TPP * r, TPP * r + TPP)
                    with nc.Else():
                        T.cond_compensate(esnap, owners)
                    T.cond_restore_seen(esnap)
            with nc.Else():
                T.cond_compensate(gsnap, owners)
            T.cond_restore_seen(gsnap)
        T.barrier()

        fgB = AR.view(0, [128, D], F32)
        xa = [AR.view((4 + 4 * i) * KB, [128, D], F32) for i in range(2)]
        yk = [AR.view((12 + 4 * i) * KB, [128, D], F32) for i in range(4)]
        tacc = AR.view(28 * KB, [128, D], F32)
        ostg = [AR.view((32 + 4 * i) * KB, [128, D], F32) for i in range(2)]
        b_fg = Buf("fg"); b_os = [Buf("os0"), Buf("os1")]; b_xa = [Buf("xa0"), Buf("xa1")]
        b_yk = [Buf(f"yk{i}") for i in range(4)]; b_tacc = Buf("tacc")
        gTc = [AR.view((40 + i) * KB, [32, 128], F32) for i in range(2)]
        b_gTc = [Buf("gTc0"), Buf("gTc1")]
        o_sem = [T.mksem("o0"), T.mksem("o1")]
        xa_sem = [T.mksem("xa0"), T.mksem("xa1")]
        yk_sem = [T.mksem(f"yk{i}") for i in range(4)]
        T.dma(sp, fgB, fg_d, ldP, writes=[b_fg])
        for tt in range(16):
            s = tt % 2
            T.dma(sp, xa[s], xnew_d[tt * 128:(tt + 1) * 128, :], xa_sem[s], writes=[b_xa[s]])
            for k in range(4):
                T._waits(pool, T._deps([], [b_yk[k]]))
                inst = nc.gpsimd.indirect_dma_start(out=yk[k], out_offset=None, in_=Yd,
                                                    in_offset=bass.IndirectOffsetOnAxis(ap=slot_all[:, tt, k:k + 1], axis=0))
                yk_sem[k].cnt += 16
                inst.then_inc(yk_sem[k].h, 16)
                T._update((yk_sem[k], yk_sem[k].cnt), [], [b_yk[k]])
            T.op(dve, lambda tt=tt: nc.vector.tensor_scalar(out=tacc, in0=yk[0], scalar1=gk_all[:, tt, 0:1], scalar2=None,
                                                            op0=ALU.mult), reads=[b_yk[0]], writes=[b_tacc])
            for k in range(1, 4):
                T.op(dve, lambda tt=tt, k=k: nc.vector.scalar_tensor_tensor(out=tacc, in0=yk[k], scalar=gk_all[:, tt, k:k + 1],
                                                                            in1=tacc, op0=ALU.mult, op1=ALU.add),
                     reads=[b_yk[k], b_tacc], writes=[b_tacc])
            T.op(dve, lambda: nc.vector.tensor_tensor(out=tacc, in0=tacc, in1=g2B[:], op=ALU.mult), reads=[b_tacc], writes=[b_tacc])
            pbt = nextbank()
            T.op(pe, lambda pbt=pbt, tt=tt: nc.tensor.transpose(out=bank(pbt, 128, 32), in_=gd_all[:, tt, :], identity=identF[:]),
                 reads=[b_const], writes=[PB[pbt]])
            T.op(act, lambda pbt=pbt, s=s: nc.scalar.copy(out=gTc[s], in_=bank(pbt, 128, 32)), reads=[PB[pbt]], writes=[b_gTc[s]])
            pbd = 0

            def mm_bd(s=s):
                nc.tensor.matmul(bank(0), lhsT=gTc[s], rhs=bdS[:, 0:512], start=True, stop=True)
                return nc.tensor.matmul(bank(1), lhsT=gTc[s], rhs=bdS[:, 512:1024], start=True, stop=True)
            T.op(pe, mm_bd, reads=[b_gTc[s], b_ph0], writes=[PB[0], PB[1]])
            T.op(dve, lambda: nc.vector.tensor_tensor(out=tacc, in0=bank(0, nb=2), in1=tacc, op=ALU.add),
                 reads=[PB[0], PB[1], b_tacc], writes=[b_tacc])
            T.op(dve, lambda s=s: nc.vector.tensor_tensor(out=xa[s], in0=xa[s], in1=tacc, op=ALU.add),
                 reads=[b_tacc, b_xa[s]], writes=[b_xa[s]])
            T.op(act, lambda s=s: nc.scalar.activation(out=junk[:], in_=xa[s], func=AF.Square, accum_out=gsum[:, 0:1]),
                 reads=[b_xa[s]], writes=[b_sm])
            T.op(act, lambda: nc.scalar.activation(out=gsum[:, 1:2], in_=gsum[:, 0:1], func=AF.Sqrt, scale=1.0 / D,
                                                   bias=epsT[:]), reads=[b_sm], writes=[b_sm])
            T.op(dve, lambda: nc.vector.reciprocal(out=gsum[:, 1:2], in_=gsum[:, 1:2]), reads=[b_sm], writes=[b_sm])
            T.op(dve, lambda s=s: nc.vector.scalar_tensor_tensor(out=ostg[s], in0=xa[s], scalar=gsum[:, 1:2],
                                                                 in1=fgB, op0=ALU.mult, op1=ALU.mult),
                 reads=[b_xa[s], b_sm, b_fg], writes=[b_os[s]])
            T.dma(act, y[tt * 128:(tt + 1) * 128, :], ostg[s], o_sem[s], reads=[b_os[s]])
        T.barrier()
    return nc


def _rope_tables():
    rows = SEQ // 64
    row = np.repeat(np.arange(rows, dtype=np.float32), 64)
    col = np.tile(np.arange(64, dtype=np.float32), rows)
    freqs = (np.float32(10000.0) ** (-np.arange(8, dtype=np.float32) / np.float32(8))).astype(np.float32)
    ang = np.concatenate([row[:, None] * freqs, col[:, None] * freqs], axis=-1).astype(np.float32)
    return np.cos(ang).astype(np.float32), np.sin(ang).astype(np.float32)


def _core_inputs(core, x, c, ctx, c_ctx, shared, cos, sin):
    b, half = core // 2, core % 2
    q0 = half * NQ
    o0 = (1 - half) * NQ
    xe = np.zeros((36 * 128, D), np.float32)
    xe[0:NQ] = x[b, q0:q0 + NQ]
    hm = np.zeros((128, 16), np.float32)
    if half == 1:
        xe[16 * 128:17 * 128] = x[b, q0 - 128:q0]
        hm[:, 0:8] = 1.0
    else:
        xe[17 * 128:18 * 128] = x[b, q0 + NQ:q0 + NQ + 128]
        hm[:, 8:16] = 1.0
    xe[18 * 128:34 * 128] = x[b, o0:o0 + NQ]
    xe[34 * 128:36 * 128] = ctx[b]
    trig = np.zeros((2, 32, NK), np.float32)
    for (dst, src) in ((0, q0), (NQ, o0)):
        cs = cos[src:src + NQ].T
        sn = sin[src:src + NQ].T
        trig[0, 0:16, dst:dst + NQ] = cs
        trig[0, 16:32, dst:dst + NQ] = cs
        trig[1, 0:16, dst:dst + NQ] = sn
        trig[1, 16:32, dst:dst + NQ] = sn
    trig[0, :, 2 * NQ:] = 1.0
    ccol = np.stack([c[b].reshape(8, 128).T, c_ctx.reshape(8, 128).T], axis=-1).astype(np.float32)
    invc = np.zeros((128, 4, 16), np.float32)
    for g, w in enumerate((2, 4, 8, 16)):
        hw = w // 2
        for i in range(16):
            t = q0 + i if i < 8 else q0 + NQ - 16 + i
            lo = max(t - hw, 0)
            hi = min(t + hw, SEQ)
            invc[:, g, i] = np.float32(1.0) / np.float32(hi - lo)
    m = {"xe": xe, "trig": trig, "ccol": np.ascontiguousarray(ccol), "hmask": hm, "invc": invc}
    m.update(shared)
    return m


_NC_CACHE = {}


def kernel(x, c, ctx, c_ctx, w_mod, b_mod, norm1_g, w_in, q_norm_g, kv_norm_g, w_uq, w_ukv, w_pool, pool_scale,
           w_out, norm2_g, router_w, router_b, w_gate_up, b_gate_up, w_down, b_down, final_g):
    f = lambda a: np.ascontiguousarray(np.asarray(a, dtype=np.float32))
    x, c, ctx, c_ctx = f(x), f(c), f(ctx), f(c_ctx)
    col = lambda v: np.ascontiguousarray(f(v).reshape(-1, 128).T)
    shared = {
        "w_mod": f(w_mod)[0], "bmod2": np.ascontiguousarray(np.broadcast_to(f(b_mod)[0][None, :], (2, 6 * D))),
        "n1col": col(f(norm1_g)[0]), "n2col": col(f(norm2_g)[0]), "w_in": f(w_in)[0],
        "qgcol": col(f(q_norm_g)[0]), "kvgcol": col(f(kv_norm_g)[0]), "w_uq": f(w_uq)[0], "w_ukv": f(w_ukv)[0],
        "w_pool": f(w_pool)[0], "pscol": col(f(pool_scale)[0]), "w_out": f(w_out)[0], "router_w": f(router_w)[0],
        "rbrep": np.ascontiguousarray(np.broadcast_to(f(router_b)[0][None, :], (128, NE))),
        "w_gu": f(w_gate_up)[0],
        "b_gu": f(b_gate_up)[0], "n2row": np.ascontiguousarray(f(norm2_g)[0][None, :]),
        "iota32": np.ascontiguousarray(np.broadcast_to(np.arange(NE, dtype=np.float32)[None, :], (128, NE))),
        "ustr": np.triu(np.ones((128, 128), np.float32), 1),
        "tokrow": np.ascontiguousarray(np.broadcast_to(
            (np.arange(16, dtype=np.int32)[None, :, None] * 128 + np.arange(128, dtype=np.int32)[:, None, None]), (128, 16, 2))),
        "w_dn": f(w_down)[0], "b_dn": f(b_down)[0],
        "fgrep": np.ascontiguousarray(np.broadcast_to(f(final_g)[None, :], (128, D))),
        "ident": np.eye(128, dtype=np.float32),
    }
    cos, sin = _rope_tables()
    in_maps = [_core_inputs(core, x, c, ctx, c_ctx, shared, cos, sin) for core in range(8)]
    if "nc" not in _NC_CACHE:
        _NC_CACHE["nc"] = build_program()
    res = run_bass_kernel_spmd(_NC_CACHE["nc"], in_maps, core_ids=list(range(8)))
    out = np.zeros((4, SEQ, D), np.float32)
    for core in range(8):
        b, half = core // 2, core % 2
        out[b, half * NQ:(half + 1) * NQ] = res.results[core]["y"]
    return out
```

```python
import math
from contextlib import ExitStack

import numpy as np
import concourse.bass as bass
import concourse.mybir as mybir
from concourse.bass_utils import run_bass_kernel_spmd

F32 = mybir.dt.float32
BF16 = mybir.dt.bfloat16
I32 = mybir.dt.int32
U32 = mybir.dt.uint32
AF = mybir.ActivationFunctionType
ALU = mybir.AluOpType

D = 1024
SEQ = 4096
CTX = 256
NK = SEQ + CTX
NKT = NK // 128
NQ = 2048
H = 8
EPS = 1e-6
NE = 32
SCALE = 1.0 / math.sqrt(96.0)
KB = 1024
ARENA_BYTES = 180 * KB
DEBUG = False
TPP = 4


class Sem:
    def __init__(self, h, name):
        self.h = h
        self.name = name
        self.cnt = 0


class Buf:
    __slots__ = ("name", "w", "r")

    def __init__(self, name=""):
        self.name = name
        self.w = None
        self.r = {}


class Eng:
    def __init__(self, name, h, sem):
        self.name = name
        self.h = h
        self.sem = sem
        self.seen = {}


class Sched:
    def __init__(self, nc, es):
        self.nc = nc
        self.es = es
        self.all_sems = []
        self.pe = Eng("pe", nc.tensor, self.mksem("s_pe"))
        self.act = Eng("act", nc.scalar, self.mksem("s_act"))
        self.dve = Eng("dve", nc.vector, self.mksem("s_dve"))
        self.pool = Eng("pool", nc.gpsimd, self.mksem("s_pool"))
        self.sp = Eng("sp", nc.sync, self.mksem("s_sp"))
        self.engs = [self.pe, self.act, self.dve, self.pool, self.sp]

    def mksem(self, name):
        s = Sem(self.es.enter_context(self.nc.semaphore(name)), name)
        self.all_sems.append(s)
        return s

    def _deps(self, reads, writes):
        toks = {}

        def add(t):
            if t is None:
                return
            sem, val = t
            if toks.get(sem, 0) < val:
                toks[sem] = val

        for b in reads:
            add(b.w)
        for b in writes:
            add(b.w)
            for t in b.r.values():
                add(t)
        return toks

    def _waits(self, e, toks):
        for sem, val in toks.items():
            if sem is e.sem and e is self.pe:
                continue
            if e.seen.get(sem, 0) >= val:
                continue
            e.h.wait_ge(sem.h, val)
            e.seen[sem] = val

    def _update(self, tok, reads, writes):
        for b in reads:
            b.r[tok[0]] = tok
        for b in writes:
            b.w = tok
            b.r = {}

    def op(self, e, fn, reads=(), writes=()):
        self._waits(e, self._deps(reads, writes))
        inst = fn()
        e.sem.cnt += 1
        inst.then_inc(e.sem.h, 1)
        tok = (e.sem, e.sem.cnt)
        self._update(tok, reads, writes)
        return tok

    def dma(self, q, out, in_, dsem, reads=(), writes=()):
        self._waits(q, self._deps(reads, writes))
        inst = q.h.dma_start(out=out, in_=in_)
        dsem.cnt += 16
        inst.then_inc(dsem.h, 16)
        tok = (dsem, dsem.cnt)
        self._update(tok, reads, writes)
        return tok

    def cond_snapshot(self):
        return ({s: s.cnt for s in self.all_sems}, {e.name: dict(e.seen) for e in self.engs})

    def cond_compensate(self, snap, owners):
        before, _ = snap
        for s in self.all_sems:
            delta = s.cnt - before[s]
            if delta == 0:
                continue
            e = None
            for en in self.engs:
                if en.sem is s:
                    e = en
            if e is None:
                e = owners[s]
            if before[s] > 0:
                e.h.wait_ge(s.h, before[s])
            e.h.sem_inc(s.h, delta)

    def cond_restore_seen(self, snap):
        _, seen = snap
        for e in self.engs:
            e.seen = dict(seen[e.name])

    def barrier(self):
        toks = {s: s.cnt for s in self.all_sems if s.cnt > 0}
        for e in self.engs:
            self._waits(e, toks)


class Arena:
    def __init__(self, t):
        self.t = t

    def view(self, off, shape, dt, parts=None):
        esz = 2 if dt == BF16 else 4
        n = 1
        for s in shape[1:]:
            n *= s
        nbytes = n * esz
        assert off % 4 == 0 and off + nbytes <= ARENA_BYTES, (off, shape)
        ap = self.t[:, off // 2:(off + nbytes) // 2]
        if dt != BF16:
            ap = ap.bitcast(dt)
        if len(shape) == 3:
            ap = ap.rearrange("p (a b) -> p a b", b=shape[2])
        elif len(shape) == 4:
            ap = ap.rearrange("p (a b c) -> p a b c", b=shape[2], c=shape[3])
        if shape[0] != 128:
            ap = ap[0:shape[0]]
        return ap


def build_program():
    nc = bass.Bass("TRN2", target_bir_lowering=False)

    def din(name, shape):
        return nc.dram_tensor(name, list(shape), F32, kind="ExternalInput").ap()

    xe = din("xe", [36 * 128, D])
    trig = din("trig", [2, 32, NK])
    ccol_d = din("ccol", [128, 8, 2])
    hmask_d = din("hmask", [128, 16])
    invc_d = din("invc", [128, 4, 16])
    w_mod = din("w_mod", [D, 6 * D])
    bmod2_d = din("bmod2", [2, 6 * D])
    n1col_d = din("n1col", [128, 8])
    n2col_d = din("n2col", [128, 8])
    w_in = din("w_in", [D, 928])
    qg_d = din("qgcol", [128, 2])
    kvg_d = din("kvgcol", [128, 1])
    w_uq = din("w_uq", [256, 768])
    w_ukv = din("w_ukv", [128, 1024])
    w_pool = din("w_pool", [4, 128, 128])
    pscol_d = din("pscol", [128, 4])
    w_out = din("w_out", [D, D])
    router_w = din("router_w", [D, NE])
    rb_d = din("rbrep", [128, NE])
    w_gu = din("w_gu", [NE, D, 2 * D])
    b_gu = din("b_gu", [NE, 2 * D])
    n2row_d = din("n2row", [1, D])
    iota_d = din("iota32", [128, NE])
    ustr_d = din("ustr", [128, 128])
    tokrow_d = nc.dram_tensor("tokrow", [128, 16, 2], I32, kind="ExternalInput").ap()
    h2d = nc.dram_tensor("h2d", [NQ, D], BF16).ap()
    xnew_d = nc.dram_tensor("xnew_d", [NQ, D], F32).ap()
    tokl = nc.dram_tensor("tokl", [NE * 2048, 2], I32).ap()
    Yd = nc.dram_tensor("Yd", [NE * 2048, D], F32).ap()
    nt_d = nc.dram_tensor("nt_d", [1, NE + 1], I32).ap()
    w_dn = din("w_dn", [NE, D, D])
    b_dn = din("b_dn", [NE, D])
    fg_d = din("fgrep", [128, D])
    ident_d = din("ident", [128, 128])
    y = nc.dram_tensor("y", [NQ, D], F32, kind="ExternalOutput").ap()
    with ExitStack() as es:
        T = Sched(nc, es)
        pe, act, dve, pool, sp = T.pe, T.act, T.dve, T.pool, T.sp

        def sb(name, shape, dt=F32):
            return es.enter_context(nc.sbuf_tensor("sb_" + name, list(shape), dt))

        arena_t = sb("arena", [128, ARENA_BYTES // 2], BF16)
        AR = Arena(arena_t)
        ps_t = es.enter_context(nc.psum_tensor("ps", [128, 4096], F32))
        PB = [Buf(f"psb{i}") for i in range(8)]

        def bank(i, n=512, parts=128, nb=1):
            return ps_t[0:parts, i * 512:i * 512 + (n if nb == 1 else nb * 512)]

        identF = sb("identF", [128, 128]); identB = sb("identB", [128, 128], BF16)
        onesF = sb("onesF", [128, 128])
        ccol = sb("ccolT", [128, 8, 2]); scT = sb("scT", [128, 8, 2])
        modcol = sb("modcol", [128, 32, 2])
        n1col = sb("n1col", [128, 8]); n2col = sb("n2col", [128, 8])
        G1L = sb("G1L", [128, 8]); G1C = sb("G1C", [128, 8]); G2c = sb("G2c", [128, 8])
        zb = sb("zb", [128, 16])
        qgc = sb("qgc", [128, 2]); kvgc = sb("kvgc", [128, 1]); pscol = sb("pscol", [128, 4])
        hmask = sb("hmask", [128, 16]); invc = sb("invc", [128, 4, 16])
        g2B = sb("g2B", [128, D])
        iota32 = sb("iota32s", [128, NE]); ustrF = sb("ustrF", [128, 128]); UstrB = sb("UstrB", [128, 128], BF16)
        onesB = sb("onesB", [128, 128], BF16); tokrow = sb("tokrows", [128, 16, 2], I32)
        rbB = sb("rbB", [128, NE]); rwt = sb("rwt", [128, 8, NE])
        bdS = sb("bdS", [32, D])
        ssv = sb("ssv", [128, 8]); rstdv = sb("rstdv", [128, 8])
        junk = sb("junk", [128, D], BF16)
        smallA = sb("smallA", [128, 64])
        b_const = Buf("const")

        ldA = T.mksem("ldA")
        for dst, src in ((identF, ident_d), (ccol, ccol_d), (n1col, n1col_d), (n2col, n2col_d), (qgc, qg_d),
                         (kvgc, kvg_d), (pscol, pscol_d), (hmask, hmask_d), (invc, invc_d), (iota32, iota_d),
                         (ustrF, ustr_d), (tokrow, tokrow_d), (rbB, rb_d)):
            T.dma(sp, dst[:], src, ldA, writes=[Buf()])
        T.dma(sp, rwt[:], router_w.rearrange("(k q) e -> q k e", q=128), ldA, writes=[Buf()])
        T.dma(sp, bdS[:], b_dn, ldA, writes=[Buf()])
        T.barrier()

        b_c2 = Buf("c2")
        epsT = sb("epsT", [128, 1])
        T.op(dve, lambda: nc.vector.memset(epsT[:], EPS), writes=[b_c2])
        T.op(dve, lambda: nc.vector.memset(onesF[:], 1.0), writes=[b_c2])
        T.op(dve, lambda: nc.vector.tensor_copy(out=identB[:], in_=identF[:]), reads=[b_const], writes=[b_c2])
        T.op(dve, lambda: nc.vector.memset(onesB[:], 1.0), writes=[b_c2])
        T.op(dve, lambda: nc.vector.tensor_copy(out=UstrB[:], in_=ustrF[:]), reads=[b_const], writes=[b_c2])

        modrow = AR.view(108 * KB, [2, 6 * D], F32)
        bmod2 = AR.view(132 * KB, [2, 6 * D], F32)
        wmod_ring = [AR.view(i * 16 * KB, [128, 8, 512], F32) for i in range(2)]
        win_f = AR.view(32 * KB, [128, 8, 928], F32)
        wuq_f = AR.view(61 * KB, [128, 2, 768], F32)
        wukv_f = AR.view(67 * KB, [128, 1024], F32)
        w_kn = AR.view(86 * KB, [128, 8, 64], BF16)
        w_v = AR.view(87 * KB, [128, 512], BF16)
        w_inL = AR.view(88 * KB, [128, 8, 416], BF16)
        w_inC = AR.view(95 * KB, [128, 8, 160], BF16)
        w_rotL = AR.view(98 * KB, [128, 8, 96], BF16)
        w_rotC = AR.view(100 * KB, [128, 8, 96], BF16)
        w_uqS = AR.view(102 * KB, [128, 2, 768], BF16)
        w_uqR = AR.view(105 * KB, [128, 2, 768], BF16)
        win_rotf = AR.view(156 * KB, [128, 8, 96], F32)

        b_ph0 = Buf("ph0")
        rows2 = AR.view(172 * KB, [1, 2, D], F32)
        n2row = AR.view(160 * KB, [1, D], F32)
        zt = AR.view(164 * KB, [128, 1024], I32)
        b_tokl0 = Buf("tokl0")
        zsem = T.mksem("zs")
        T.dma(sp, n2row, n2row_d, ldA, writes=[Buf()])
        T.op(pool, lambda: nc.gpsimd.memset(zt, 0), writes=[b_tokl0])
        T.dma(sp, tokl.rearrange("(p a) b -> p (a b)", p=128), zt, zsem, reads=[b_tokl0], writes=[b_tokl0])
        T.dma(sp, bmod2, bmod2_d, ldA, writes=[Buf()])
        T.dma(sp, win_f, w_in.rearrange("(k q) f -> q k f", q=128), ldA, writes=[Buf()])
        T.dma(sp, wuq_f, w_uq.rearrange("(k q) f -> q k f", q=128), ldA, writes=[Buf()])
        T.dma(sp, wukv_f, w_ukv, ldA, writes=[Buf()])
        T.barrier()

        T.op(act, lambda: nc.scalar.activation(out=scT[:], in_=ccol[:], func=AF.Silu), reads=[b_const], writes=[b_ph0])

        wm_sem = [T.mksem(f"wm{i}") for i in range(2)]
        wm_buf = [Buf(f"wm{i}") for i in range(2)]
        b_modrow = Buf("modrow")
        for ns in range(12):
            s = ns % 2
            T.dma(sp, wmod_ring[s], w_mod.rearrange("(k q) n -> q k n", q=128)[:, :, ns * 512:(ns + 1) * 512],
                  wm_sem[s], writes=[wm_buf[s]])
            pb = ns % 2

            def mm(s=s, pb=pb):
                last = None
                for k in range(8):
                    last = nc.tensor.matmul(bank(pb, parts=2), lhsT=scT[:, k, :], rhs=wmod_ring[s][:, k, :],
                                            start=(k == 0), stop=(k == 7))
                return last
            T.op(pe, mm, reads=[wm_buf[s], b_ph0], writes=[PB[pb]])
            T.op(dve, lambda pb=pb, ns=ns: nc.vector.tensor_tensor(
                out=modrow[:, ns * 512:(ns + 1) * 512], in0=bank(pb, parts=2), in1=bmod2[:, ns * 512:(ns + 1) * 512],
                op=ALU.add), reads=[PB[pb], b_const], writes=[b_modrow])

        chunk_src = [0 * 8 + i for i in range(8)] + [1 * 8 + i for i in range(8)] + \
                    [3 * 8 + i for i in range(8)] + [4 * 8 + i for i in range(8)]

        def tr_mod():
            last = None
            for i, ch in enumerate(chunk_src):
                last = nc.tensor.transpose(out=bank(2, 64)[:, 2 * i:2 * i + 2], in_=modrow[:, ch * 128:(ch + 1) * 128],
                                           identity=identF[0:2, 0:2])
            return last
        T.op(pe, tr_mod, reads=[b_modrow, b_const], writes=[PB[2]])
        T.op(dve, lambda: nc.vector.tensor_copy(out=modcol[:].rearrange("p a b -> p (a b)"), in_=bank(2, 64)),
             reads=[PB[2]], writes=[b_ph0])
        T.op(dve, lambda: nc.vector.scalar_tensor_tensor(out=G1L[:], in0=modcol[:, 8:16, 0], scalar=1.0, in1=n1col[:],
                                                         op0=ALU.add, op1=ALU.mult), reads=[b_ph0, b_const], writes=[b_ph0])
        T.op(dve, lambda: nc.vector.scalar_tensor_tensor(out=G1C[:], in0=modcol[:, 8:16, 1], scalar=1.0, in1=n1col[:],
                                                         op0=ALU.add, op1=ALU.mult), reads=[b_ph0, b_const], writes=[b_ph0])
        T.op(dve, lambda: nc.vector.scalar_tensor_tensor(out=G2c[:], in0=modcol[:, 24:32, 0], scalar=1.0, in1=n2col[:],
                                                         op0=ALU.add, op1=ALU.mult), reads=[b_ph0, b_const], writes=[b_ph0])
        for hf in range(2):
            T.op(pe, lambda hf=hf: nc.tensor.matmul(bank(3 + hf), lhsT=onesF[0:1, :],
                                                    rhs=modrow[0:1, 5 * D + hf * 512:5 * D + (hf + 1) * 512],
                                                    start=True, stop=True), reads=[b_modrow, b_const], writes=[PB[3 + hf]])
            T.op(dve, lambda hf=hf: nc.vector.tensor_copy(out=g2B[:, hf * 512:(hf + 1) * 512], in_=bank(3 + hf)),
                 reads=[PB[3 + hf]], writes=[b_ph0])
        g1row = sb("g1row", [1, D])
        T.op(act, lambda: nc.scalar.copy(out=g1row[:], in_=modrow[0:1, 2 * D:3 * D]), reads=[b_modrow], writes=[b_ph0])
        T.op(dve, lambda: nc.vector.scalar_tensor_tensor(out=rows2[0:1, 0, :], in0=modrow[0:1, 4 * D:5 * D], scalar=1.0, in1=n2row,
                                                         op0=ALU.add, op1=ALU.mult), reads=[b_modrow, b_const], writes=[b_ph0])
        T.op(dve, lambda: nc.vector.tensor_copy(out=rows2[0:1, 1, :], in_=modrow[0:1, 3 * D:4 * D]), reads=[b_modrow], writes=[b_ph0])
        T.op(dve, lambda: nc.vector.tensor_tensor(out=bdS[:], in0=bdS[:], in1=g2B[0:32, :], op=ALU.mult),
             reads=[b_const, b_ph0], writes=[b_ph0])

        T.op(dve, lambda: nc.vector.memset(win_rotf, 0.0), writes=[b_ph0])
        T.op(dve, lambda: nc.vector.tensor_scalar(out=win_rotf[:, :, 64:80], in0=win_f[:, :, 400:416], scalar1=-1.0,
                                                  scalar2=None, op0=ALU.mult), reads=[b_const], writes=[b_ph0])
        T.op(dve, lambda: nc.vector.tensor_copy(out=win_rotf[:, :, 80:96], in_=win_f[:, :, 384:400]),
             reads=[b_const], writes=[b_ph0])

        zb_specs = [(win_f, 0, 128, 0, 128), (win_f, 128, 256, 0, 128), (win_f, 256, 384, 0, 128),
                    (win_f, 320, 416, 0, 96), (win_rotf, 0, 96, 0, 96),
                    (win_f, 416, 544, 0, 128), (win_f, 544, 672, 0, 128), (win_f, 672, 800, 0, 128),
                    (win_f, 800, 928, 0, 128),
                    (win_f, 256, 384, 1, 128), (win_f, 320, 416, 1, 96), (win_rotf, 0, 96, 1, 96)]

        def zb_mm():
            last = None
            for j, (wt, c0, c1, which, m) in enumerate(zb_specs):
                for k in range(8):
                    last = nc.tensor.matmul(bank(5, 16)[0:m, j:j + 1], lhsT=wt[:, k, c0:c1],
                                            rhs=modcol[:, k, which:which + 1], start=(k == 0), stop=(k == 7))
            return last
        T.op(pe, zb_mm, reads=[b_const, b_ph0], writes=[PB[5]])
        T.op(dve, lambda: nc.vector.memset(zb[:], 0.0), writes=[b_ph0])
        for j, (wt, c0, c1, which, m) in enumerate(zb_specs):
            T.op(dve, lambda j=j, m=m: nc.vector.tensor_copy(out=zb[0:m, j:j + 1], in_=bank(5, 16)[0:m, j:j + 1]),
                 reads=[PB[5]], writes=[b_ph0])

        for k in range(8):
            T.op(dve, lambda k=k: nc.vector.tensor_scalar(out=w_inL[:, k, :], in0=win_f[:, k, 0:416], scalar1=G1L[:, k:k + 1],
                                                          scalar2=None, op0=ALU.mult), reads=[b_const, b_ph0], writes=[b_ph0])
            T.op(dve, lambda k=k: nc.vector.tensor_scalar(out=w_inC[:, k, :], in0=win_f[:, k, 256:416], scalar1=G1C[:, k:k + 1],
                                                          scalar2=None, op0=ALU.mult), reads=[b_const, b_ph0], writes=[b_ph0])
        for wr, ws, o in ((w_rotL, w_inL, 384), (w_rotC, w_inC, 128)):
            T.op(dve, lambda wr=wr: nc.vector.memset(wr, 0.0), writes=[b_ph0])
            T.op(dve, lambda wr=wr, ws=ws, o=o: nc.vector.tensor_scalar(out=wr[:, :, 64:80], in0=ws[:, :, o + 16:o + 32],
                                                                        scalar1=-1.0, scalar2=None, op0=ALU.mult),
                 reads=[b_ph0], writes=[b_ph0])
            T.op(dve, lambda wr=wr, ws=ws, o=o: nc.vector.tensor_copy(out=wr[:, :, 80:96], in_=ws[:, :, o:o + 16]),
                 reads=[b_ph0], writes=[b_ph0])
        T.op(dve, lambda: nc.vector.memset(w_uqR, 0.0), writes=[b_ph0])
        for c in range(2):
            T.op(dve, lambda c=c: nc.vector.tensor_scalar(out=w_uqS[:, c, :], in0=wuq_f[:, c, :], scalar1=qgc[:, c:c + 1],
                                                          scalar2=None, op0=ALU.mult), reads=[b_const], writes=[b_ph0])
            sv = w_uqS[:, c, :].rearrange("p (h e) -> p h e", e=96)
            rv = w_uqR[:, c, :].rearrange("p (h e) -> p h e", e=96)
            T.op(dve, lambda sv=sv, rv=rv: nc.vector.tensor_scalar(out=rv[:, :, 64:80], in0=sv[:, :, 80:96], scalar1=-1.0,
                                                                   scalar2=None, op0=ALU.mult), reads=[b_ph0], writes=[b_ph0])
            T.op(dve, lambda sv=sv, rv=rv: nc.vector.tensor_copy(out=rv[:, :, 80:96], in_=sv[:, :, 64:80]),
                 reads=[b_ph0], writes=[b_ph0])
        kvv = wukv_f.rearrange("p (h e) -> p h e", e=128)
        T.op(dve, lambda: nc.vector.tensor_scalar(out=w_kn, in0=kvv[:, :, 0:64], scalar1=kvgc[:, 0:1], scalar2=None,
                                                  op0=ALU.mult), reads=[b_const], writes=[b_ph0])
        T.op(dve, lambda: nc.vector.tensor_scalar(out=w_v.rearrange("p (h e) -> p h e", e=64), in0=kvv[:, :, 64:128],
                                                  scalar1=kvgc[:, 0:1], scalar2=None, op0=ALU.mult),
             reads=[b_const], writes=[b_ph0])
        T.barrier()

        x_sem = [T.mksem(f"xs{i}") for i in range(3)]
        x_buf = [Buf(f"xb{i}") for i in range(3)]
        xn_buf = [Buf(f"xn{i}") for i in range(2)]
        ss_buf = [Buf(f"ss{i}") for i in range(8)]
        state = {"xi": 0, "ni": 0, "si": 0, "tp": 0}

        def norm_tile_to_hT(x_ring, xn_ring, src_rows, hT_ap, hT_b, col0, tp_banks):
            xi = state["xi"] % len(x_ring); state["xi"] += 1
            ni = state["ni"] % 2; state["ni"] += 1
            si = state["si"] % 8; state["si"] += 1
            tb = tp_banks[state["tp"] % len(tp_banks)]; state["tp"] += 1
            xt = x_ring[xi]; xn = xn_ring[ni]
            T.dma(sp, xt, xe[src_rows * 128:(src_rows + 1) * 128, :], x_sem[xi], writes=[x_buf[xi]])
            T.op(act, lambda: nc.scalar.activation(out=junk[:], in_=xt, func=AF.Square, accum_out=ssv[:, si:si + 1]),
                 reads=[x_buf[xi]], writes=[ss_buf[si]])
            T.op(act, lambda: nc.scalar.activation(out=rstdv[:, si:si + 1], in_=ssv[:, si:si + 1], func=AF.Sqrt,
                                                   scale=1.0 / D, bias=epsT[:]), reads=[ss_buf[si]], writes=[ss_buf[si]])
            T.op(dve, lambda: nc.vector.reciprocal(out=rstdv[:, si:si + 1], in_=rstdv[:, si:si + 1]),
                 reads=[ss_buf[si]], writes=[ss_buf[si]])
            T.op(act, lambda: nc.scalar.activation(out=xn, in_=xt, func=AF.Copy, scale=rstdv[:, si:si + 1]),
                 reads=[x_buf[xi], ss_buf[si]], writes=[xn_buf[ni]])
            tpv = bank(tb).bitcast(BF16)

            def tr():
                last = None
                for c in range(8):
                    last = nc.tensor.transpose(out=tpv[:, c * 128:(c + 1) * 128], in_=xn[:, c * 128:(c + 1) * 128],
                                               identity=identB[:])
                return last
            T.op(pe, tr, reads=[xn_buf[ni], b_const], writes=[PB[tb]])
            T.op(dve, lambda: nc.vector.tensor_copy(out=hT_ap[:, :, col0:col0 + 128],
                                                    in_=tpv.rearrange("p (c t) -> p c t", t=128)),
                 reads=[PB[tb]], writes=[hT_b])

        def rstd_bcast(psb, n, out_ap, inv_n, bufs_r, bufs_w):
            T.op(act, lambda: nc.scalar.activation(out=out_ap, in_=bank(psb, n), func=AF.Sqrt, scale=inv_n, bias=epsT[:]),
                 reads=bufs_r, writes=bufs_w)
            T.op(dve, lambda: nc.vector.reciprocal(out=out_ap, in_=out_ap), reads=bufs_w, writes=bufs_w)

        QT_all = AR.view(0, [96, 8, NQ], BF16)
        V_ext = AR.view(32 * KB, [128, NKT, 8, 65], BF16)
        ckvnT = AR.view(68 * KB, [128, NK], BF16)
        krT = AR.view(77 * KB, [96, NK], BF16)
        x_ring = [AR.view((108 + 4 * i) * KB, [128, D], F32) for i in range(3)]
        xn_ring = [AR.view((120 + 2 * i) * KB, [128, D], BF16) for i in range(2)]
        hT_ring = [AR.view((124 + 8 * i) * KB, [128, 8, 512], BF16) for i in range(2)]
        cos_ring = [AR.view((140 + 4 * i) * KB, [128, 512], F32) for i in range(2)]
        sin_ring = [AR.view((142 + 4 * i) * KB, [128, 512], F32) for i in range(2)]
        ckv_sb = AR.view(148 * KB, [128, 512], F32)
        sq_kv = AR.view(150 * KB, [128, 512], F32)
        cq_sb = AR.view(152 * KB, [128, 2, 512], F32)
        sq_q = AR.view(156 * KB, [128, 2, 512], F32)
        rstd_kv = AR.view(160 * KB, [128, 512], F32)
        rstd_q = AR.view(162 * KB, [128, 512], F32)
        cqn = AR.view(164 * KB, [128, 2, 512], BF16)
        t1 = AR.view(168 * KB, [128, 512], F32)
        t2 = AR.view(170 * KB, [128, 512], F32)

        b_QT = Buf("QT"); b_V = Buf("V"); b_ckvn = Buf("ckvn"); b_kr = Buf("kr")
        hT_b = [Buf("hT0"), Buf("hT1")]
        tg_sem = [T.mksem(f"tg{i}") for i in range(2)]
        tg_b = [Buf("tg0"), Buf("tg1")]
        b_ckvsb = Buf("ckvsb"); b_sqkv = Buf("sqkv"); b_cqsb = Buf("cqsb"); b_sqq = Buf("sqq")
        b_rkv = Buf("rkv"); b_rq = Buf("rq"); b_cqn = Buf("cqn"); b_t1 = Buf("t1"); b_t2 = Buf("t2")

        T.op(dve, lambda: nc.vector.memset(V_ext, 1.0), writes=[b_V])

        mmb = {"i": 0}

        def nextbank():
            b = 2 + (mmb["i"] % 6)
            mmb["i"] += 1
            return b

        groups = []
        for g in range(4):
            groups.append(("own", [4 * g + i for i in range(4)], g * 512))
        for g in range(4):
            groups.append(("other", [18 + 4 * g + i for i in range(4)], 2048 + g * 512))
        groups.append(("ctx", [34, 35], 4096))

        def norm_group_1a(gi):
            kind, tiles, kc0 = groups[gi]
            n = 128 * len(tiles)
            hb = gi % 2
            T.dma(sp, cos_ring[hb][64:96, 0:n], trig[0, :, kc0:kc0 + n], tg_sem[hb], writes=[tg_b[hb]])
            T.dma(sp, sin_ring[hb][64:96, 0:n], trig[1, :, kc0:kc0 + n], tg_sem[hb], writes=[tg_b[hb]])
            for ti, t in enumerate(tiles):
                norm_tile_to_hT(x_ring, xn_ring, t, hT_ring[hb], hT_b[hb], ti * 128, (0, 1))

        for gi, (kind, tiles, kc0) in enumerate(groups):
            n = 128 * len(tiles)
            hb = gi % 2
            hT = hT_ring[hb]
            W = w_inC if kind == "ctx" else w_inL
            WR = w_rotC if kind == "ctx" else w_rotL
            off = -256 if kind == "ctx" else 0
            zj = (9, 10, 11) if kind == "ctx" else (2, 3, 4)
            if gi == 0:
                norm_group_1a(0)
            if gi + 1 < len(groups):
                norm_group_1a(gi + 1)
            pb = nextbank()

            def mm_ckv(pb=pb, W=W, off=off, hT=hT, n=n):
                last = None
                for k in range(8):
                    last = nc.tensor.matmul(bank(pb, n), lhsT=W[:, k, 256 + off:384 + off], rhs=hT[:, k, 0:n],
                                            start=(k == 0), stop=(k == 7))
                return last
            T.op(pe, mm_ckv, reads=[hT_b[hb], b_ph0], writes=[PB[pb]])
            T.op(act, lambda pb=pb, n=n, zj=zj: nc.scalar.activation(out=ckv_sb[:, 0:n], in_=bank(pb, n), func=AF.Identity,
                                                                    bias=zb[:, zj[0]:zj[0] + 1]),
                 reads=[PB[pb], b_ph0], writes=[b_ckvsb])
            T.op(act, lambda n=n: nc.scalar.activation(out=sq_kv[:, 0:n], in_=ckv_sb[:, 0:n], func=AF.Square),
                 reads=[b_ckvsb], writes=[b_sqkv])
            pb2 = nextbank()
            T.op(pe, lambda pb2=pb2, n=n: nc.tensor.matmul(bank(pb2, n), lhsT=onesF[:], rhs=sq_kv[:, 0:n], start=True, stop=True),
                 reads=[b_sqkv, b_const], writes=[PB[pb2]])
            rstd_bcast(pb2, n, rstd_kv[:, 0:n], 1.0 / 128, [PB[pb2]], [b_rkv])
            T.op(dve, lambda n=n, kc0=kc0: nc.vector.tensor_tensor(out=ckvnT[:, kc0:kc0 + n], in0=ckv_sb[:, 0:n],
                                                                   in1=rstd_kv[:, 0:n], op=ALU.mult),
                 reads=[b_ckvsb, b_rkv], writes=[b_ckvn])
            for ti in range(len(tiles)):
                kt = kc0 // 128 + ti
                pb3 = nextbank()
                T.op(pe, lambda pb3=pb3, kt=kt: nc.tensor.matmul(bank(pb3), lhsT=ckvnT[:, kt * 128:(kt + 1) * 128], rhs=w_v,
                                                                start=True, stop=True), reads=[b_ckvn, b_ph0], writes=[PB[pb3]])
                T.op(act, lambda pb3=pb3, kt=kt: nc.scalar.copy(out=V_ext[:, kt, :, 0:64],
                                                                in_=bank(pb3).rearrange("p (h e) -> p h e", e=64)),
                     reads=[PB[pb3]], writes=[b_V])
            pr = nextbank(); pq = nextbank()

            def mm_kr(pr=pr, pq=pq, W=W, WR=WR, off=off, hT=hT, n=n):
                last = None
                for k in range(8):
                    last = nc.tensor.matmul(bank(pr, n, 96), lhsT=W[:, k, 320 + off:416 + off], rhs=hT[:, k, 0:n],
                                            start=(k == 0), stop=(k == 7))
                for k in range(8):
                    last = nc.tensor.matmul(bank(pq, n, 96), lhsT=WR[:, k, :], rhs=hT[:, k, 0:n],
                                            start=(k == 0), stop=(k == 7))
                return last
            T.op(pe, mm_kr, reads=[hT_b[hb], b_ph0], writes=[PB[pr], PB[pq]])
            T.op(dve, lambda pr=pr, n=n, zj=zj, hb=hb: nc.vector.scalar_tensor_tensor(
                out=t1[64:96, 0:n], in0=bank(pr, n)[64:96], scalar=zb[64:96, zj[1]:zj[1] + 1], in1=cos_ring[hb][64:96, 0:n],
                op0=ALU.add, op1=ALU.mult), reads=[PB[pr], tg_b[hb], b_ph0], writes=[b_t1])
            T.op(dve, lambda pq=pq, n=n, zj=zj, hb=hb: nc.vector.scalar_tensor_tensor(
                out=t2[64:96, 0:n], in0=bank(pq, n)[64:96], scalar=zb[64:96, zj[2]:zj[2] + 1], in1=sin_ring[hb][64:96, 0:n],
                op0=ALU.add, op1=ALU.mult), reads=[PB[pq], tg_b[hb], b_ph0], writes=[b_t2])
            T.op(dve, lambda n=n, kc0=kc0: nc.vector.tensor_tensor(out=krT[64:96, kc0:kc0 + n], in0=t1[64:96, 0:n],
                                                                   in1=t2[64:96, 0:n], op=ALU.add),
                 reads=[b_t1, b_t2], writes=[b_kr])
            if kind != "own":
                continue
            for c in range(2):
                pc = nextbank()

                def mm_cq(pc=pc, c=c, hT=hT):
                    last = None
                    for k in range(8):
                        last = nc.tensor.matmul(bank(pc), lhsT=w_inL[:, k, c * 128:(c + 1) * 128], rhs=hT[:, k, :],
                                                start=(k == 0), stop=(k == 7))
                    return last
                T.op(pe, mm_cq, reads=[hT_b[hb], b_ph0], writes=[PB[pc]])
                T.op(act, lambda pc=pc, c=c: nc.scalar.activation(out=cq_sb[:, c, :], in_=bank(pc), func=AF.Identity,
                                                                  bias=zb[:, c:c + 1]), reads=[PB[pc], b_ph0], writes=[b_cqsb])
                T.op(act, lambda c=c: nc.scalar.activation(out=sq_q[:, c, :], in_=cq_sb[:, c, :], func=AF.Square),
                     reads=[b_cqsb], writes=[b_sqq])
            pss = nextbank()

            def mm_ssq(pss=pss):
                nc.tensor.matmul(bank(pss), lhsT=onesF[:], rhs=sq_q[:, 0, :], start=True, stop=False)
                return nc.tensor.matmul(bank(pss), lhsT=onesF[:], rhs=sq_q[:, 1, :], start=False, stop=True)
            T.op(pe, mm_ssq, reads=[b_sqq, b_const], writes=[PB[pss]])
            rstd_bcast(pss, 512, rstd_q, 1.0 / 256, [PB[pss]], [b_rq])
            for c in range(2):
                T.op(dve, lambda c=c: nc.vector.tensor_tensor(out=cqn[:, c, :], in0=cq_sb[:, c, :], in1=rstd_q,
                                                              op=ALU.mult), reads=[b_cqsb, b_rq], writes=[b_cqn])
            for h in range(H):
                pr = nextbank(); pq = nextbank()

                def mm_q(pr=pr, pq=pq, h=h):
                    last = None
                    for c in range(2):
                        last = nc.tensor.matmul(bank(pr, 512, 96), lhsT=w_uqS[:, c, h * 96:(h + 1) * 96], rhs=cqn[:, c, :],
                                                start=(c == 0), stop=(c == 1))
                    for c in range(2):
                        last = nc.tensor.matmul(bank(pq, 512, 96), lhsT=w_uqR[:, c, h * 96:(h + 1) * 96], rhs=cqn[:, c, :],
                                                start=(c == 0), stop=(c == 1))
                    return last
                T.op(pe, mm_q, reads=[b_cqn, b_ph0], writes=[PB[pr], PB[pq]])
                T.op(act, lambda pr=pr, h=h, kc0=kc0: nc.scalar.copy(out=QT_all[0:64, h, kc0:kc0 + 512], in_=bank(pr)[0:64]),
                     reads=[PB[pr]], writes=[b_QT])
                T.op(dve, lambda pr=pr, hb=hb: nc.vector.tensor_tensor(out=t1[64:96, :], in0=bank(pr)[64:96],
                                                                       in1=cos_ring[hb][64:96, :], op=ALU.mult),
                     reads=[PB[pr], tg_b[hb]], writes=[b_t1])
                T.op(dve, lambda pq=pq, hb=hb: nc.vector.tensor_tensor(out=t2[64:96, :], in0=bank(pq)[64:96],
                                                                       in1=sin_ring[hb][64:96, :], op=ALU.mult),
                     reads=[PB[pq], tg_b[hb]], writes=[b_t2])
                T.op(dve, lambda h=h, kc0=kc0: nc.vector.tensor_tensor(out=QT_all[64:96, h, kc0:kc0 + 512], in0=t1[64:96, :],
                                                                       in1=t2[64:96, :], op=ALU.add),
                     reads=[b_t1, b_t2], writes=[b_QT])
        T.barrier()

        KTs = [AR.view((88 + 9 * i) * KB, [96, NK], BF16) for i in range(2)]
        PT = [AR.view((106 + 2 * i) * KB, [128, 1024], BF16) for i in range(3)]
        att_tok = AR.view(112 * KB, [128, 16, 512], BF16)
        catT = AR.view(128 * KB, [128, 8, NQ], BF16)
        b_KT = [Buf("KT0"), Buf("KT1")]
        b_PT = [Buf(f"PT{i}") for i in range(3)]
        b_att = Buf("att"); b_cat = Buf("cat"); b_rc = Buf("rc")
        rc = smallA

        for i in range(2):
            T.op(pool, lambda i=i: nc.gpsimd.tensor_copy(out=KTs[i][64:96, :], in_=krT[64:96, :]), reads=[b_kr], writes=[b_KT[i]])

        kgroups = [(g * 512, 512) for g in range(8)] + [(4096, 256)]

        def build_KT(h):
            for gi2, (k0, n) in enumerate(kgroups):
                pb = 6 + (gi2 % 2)
                T.op(pe, lambda pb=pb, k0=k0, n=n: nc.tensor.matmul(bank(pb, n, 64), lhsT=w_kn[:, h, :], rhs=ckvnT[:, k0:k0 + n],
                                                                    start=True, stop=True), reads=[b_ckvn, b_ph0], writes=[PB[pb]])
                T.op(dve, lambda pb=pb, k0=k0, n=n: nc.vector.tensor_copy(out=KTs[h % 2][0:64, k0:k0 + n], in_=bank(pb, n, 64)),
                     reads=[PB[pb]], writes=[b_KT[h % 2]])

        build_KT(0)
        it = 0
        for h in range(H):
            if h + 1 < H:
                build_KT(h + 1)
            KT = KTs[h % 2]
            for qg in range(2):
                q0 = qg * 1024

                def S_mm(kt, sb_):
                    def f():
                        nc.tensor.matmul(bank(sb_), lhsT=KT[:, kt * 128:(kt + 1) * 128], rhs=QT_all[:, h, q0:q0 + 512],
                                         start=True, stop=True)
                        return nc.tensor.matmul(bank(sb_ + 1), lhsT=KT[:, kt * 128:(kt + 1) * 128],
                                                rhs=QT_all[:, h, q0 + 512:q0 + 1024], start=True, stop=True)
                    T.op(pe, f, reads=[b_KT[h % 2], b_QT], writes=[PB[sb_], PB[sb_ + 1]])

                S_mm(0, 0)
                for kt in range(NKT):
                    sb_ = 2 * (kt % 2)
                    if kt + 1 < NKT:
                        S_mm(kt + 1, 2 * ((kt + 1) % 2))
                    pi = it % 3; it += 1
                    T.op(act, lambda sb_=sb_, pi=pi: nc.scalar.activation(out=PT[pi], in_=bank(sb_, nb=2), func=AF.Exp, scale=SCALE),
                         reads=[PB[sb_], PB[sb_ + 1]], writes=[b_PT[pi]])

                    def PV(kt=kt, pi=pi):
                        last = None
                        for j in range(8):
                            ob = 4 + j // 4
                            last = nc.tensor.matmul(bank(ob)[:, (j % 4) * 65:(j % 4) * 65 + 65],
                                                    lhsT=PT[pi][:, j * 128:(j + 1) * 128], rhs=V_ext[:, kt, h, :],
                                                    start=(kt == 0 and j % 4 == 0), stop=(kt == NKT - 1),
                                                    skip_group_check=True)
                        return last
                    T.op(pe, PV, reads=[b_PT[pi], b_V], writes=[PB[4], PB[5]])
                for ob in (4, 5):
                    ov = bank(ob, 260).rearrange("p (s e) -> p s e", e=65)
                    T.op(dve, lambda ov=ov, ob=ob: nc.vector.reciprocal(out=rc[:, (ob - 4) * 4:(ob - 4) * 4 + 4], in_=ov[:, :, 64]),
                         reads=[PB[ob]], writes=[b_rc])
                    for s in range(4):
                        j = (ob - 4) * 4 + s
                        T.op(dve, lambda ov=ov, s=s, j=j: nc.vector.tensor_scalar(
                            out=att_tok[:, qg * 8 + j, h * 64:(h + 1) * 64], in0=ov[:, s, 0:64], scalar1=rc[:, j:j + 1],
                            scalar2=None, op0=ALU.mult), reads=[PB[ob], b_rc], writes=[b_att])
        for qt in range(16):
            tb = 6 + (qt % 2)
            tpv = bank(tb).bitcast(BF16)

            def tr(qt=qt, tpv=tpv):
                last = None
                for c in range(4):
                    last = nc.tensor.transpose(out=tpv[:, c * 128:(c + 1) * 128], in_=att_tok[:, qt, c * 128:(c + 1) * 128],
                                               identity=identB[:])
                return last
            T.op(pe, tr, reads=[b_att, b_const], writes=[PB[tb]])
            T.op(dve, lambda qt=qt, tpv=tpv: nc.vector.tensor_copy(out=catT[:, 0:4, qt * 128:(qt + 1) * 128],
                                                                   in_=tpv[:, 0:512].rearrange("p (c t) -> p c t", t=128)),
                 reads=[PB[tb]], writes=[b_cat])
        T.barrier()

        x_ring = [AR.view(4 * i * KB, [128, D], F32) for i in range(3)]
        xn_ring = [AR.view((12 + 2 * i) * KB, [128, D], BF16) for i in range(2)]
        hT_ring = [AR.view((16 + 8 * i) * KB, [128, 8, 512], BF16) for i in range(2)]
        w_inU = AR.view(32 * KB, [128, 8, 512], BF16)
        NU = NQ + 16
        uT = AR.view(40 * KB, [128, 4, NU], F32)
        tmpA = AR.view(73 * KB, [128, NU], F32)
        tmpB = AR.view(82 * KB, [128, NU], F32)
        mixB = [AR.view((91 + 4 * i) * KB, [128, NQ], BF16) for i in range(2)]
        w_poolB = AR.view(99 * KB, [128, 4, 128], BF16)
        winu_f = AR.view(100 * KB, [128, 8, 512], F32)
        b_wu = Buf("wu"); b_uT = Buf("uT"); b_tA = Buf("tA"); b_tB = Buf("tB"); b_mix = [Buf("mix0"), Buf("mix1")]
        ldP = T.mksem("ldP")
        b_ldP = Buf("ldP")
        T.dma(sp, winu_f, w_in.rearrange("(k q) f -> q k f", q=128)[:, :, 416:928], ldP, writes=[b_ldP])
        T.dma(pool, w_poolB, w_pool.rearrange("g c d -> c g d"), ldP, writes=[b_ldP])
        for k in range(8):
            T.op(dve, lambda k=k: nc.vector.tensor_scalar(out=w_inU[:, k, :], in0=winu_f[:, k, :], scalar1=G1L[:, k:k + 1],
                                                          scalar2=None, op0=ALU.mult), reads=[b_ldP, b_ph0], writes=[b_wu])
        pgroups = [("halo", [16, 17])] + [("own", [4 * g + i for i in range(4)]) for g in range(4)]
        og = 0
        def norm_group_p(gi):
            for ti, t in enumerate(pgroups[gi][1]):
                norm_tile_to_hT(x_ring, xn_ring, t, hT_ring[gi % 2], hT_b[gi % 2], ti * 128, (0, 1))

        norm_group_p(0)
        for gi, (kind, tiles) in enumerate(pgroups):
            n = 128 * len(tiles)
            hb = gi % 2
            hT = hT_ring[hb]
            if gi + 1 < len(pgroups):
                norm_group_p(gi + 1)
            for g in range(4):
                pb = nextbank()

                def mm_u(pb=pb, g=g, hT=hT, n=n):
                    last = None
                    for k in range(8):
                        last = nc.tensor.matmul(bank(pb, n), lhsT=w_inU[:, k, g * 128:(g + 1) * 128], rhs=hT[:, k, 0:n],
                                                start=(k == 0), stop=(k == 7))
                    return last
                T.op(pe, mm_u, reads=[hT_b[hb], b_wu], writes=[PB[pb]])
                if kind == "own":
                    c0 = 8 + og * 512
                    T.op(act, lambda pb=pb, g=g, c0=c0: nc.scalar.activation(out=uT[:, g, c0:c0 + 512], in_=bank(pb),
                                                                             func=AF.Identity, bias=zb[:, 5 + g:6 + g]),
                         reads=[PB[pb], b_ph0], writes=[b_uT])
                else:
                    T.op(dve, lambda pb=pb, g=g: nc.vector.scalar_tensor_tensor(
                        out=uT[:, g, 0:8], in0=bank(pb)[:, 120:128], scalar=zb[:, 5 + g:6 + g], in1=hmask[:, 0:8],
                        op0=ALU.add, op1=ALU.mult), reads=[PB[pb], b_ph0, b_const], writes=[b_uT])
                    T.op(dve, lambda pb=pb, g=g: nc.vector.scalar_tensor_tensor(
                        out=uT[:, g, NU - 8:NU], in0=bank(pb)[:, 128:136], scalar=zb[:, 5 + g:6 + g], in1=hmask[:, 8:16],
                        op0=ALU.add, op1=ALU.mult), reads=[PB[pb], b_ph0, b_const], writes=[b_uT])
            if kind == "own":
                og += 1

        def tt_add(e, o, a, b_, rb, wb):
            if e is pool:
                T.op(pool, lambda: nc.gpsimd.tensor_tensor(out=o, in0=a, in1=b_, op=ALU.add), reads=rb, writes=wb)
            else:
                T.op(dve, lambda: nc.vector.tensor_tensor(out=o, in0=a, in1=b_, op=ALU.add), reads=rb, writes=wb)

        LO, HI = 8, 8 + NQ
        for g in range(4):
            U = uT[:, g, :]
            wwin = (2, 4, 8, 16)[g]
            eng = dve
            if g == 0:
                tt_add(eng, tmpA[:, LO:HI], U[:, LO - 1:HI - 1], U[:, LO:HI], [b_uT], [b_tA]); S = tmpA; bS = b_tA
            else:
                tt_add(eng, tmpA[:, 1:NU], U[:, 0:NU - 1], U[:, 1:NU], [b_uT], [b_tA])
                if g == 1:
                    tt_add(eng, tmpB[:, LO:HI], tmpA[:, LO - 1:HI - 1], tmpA[:, LO + 1:HI + 1], [b_tA], [b_tB]); S = tmpB; bS = b_tB
                else:
                    tt_add(eng, tmpB[:, 2:NU - 1], tmpA[:, 1:NU - 2], tmpA[:, 3:NU], [b_tA], [b_tB])
                    if g == 2:
                        tt_add(eng, tmpA[:, LO:HI], tmpB[:, LO - 2:HI - 2], tmpB[:, LO + 2:HI + 2], [b_tB], [b_tA]); S = tmpA; bS = b_tA
                    else:
                        tt_add(eng, tmpA[:, 4:NU - 3], tmpB[:, 2:NU - 5], tmpB[:, 6:NU - 1], [b_tB], [b_tA])
                        tt_add(eng, tmpB[:, LO:HI], tmpA[:, LO - 4:HI - 4], tmpA[:, LO + 4:HI + 4], [b_tA], [b_tB]); S = tmpB; bS = b_tB
            mb = mixB[g % 2]
            T.op(dve, lambda S=S, U=U, mb=mb, wwin=wwin: nc.vector.scalar_tensor_tensor(
                out=mb, in0=S[:, LO:HI], scalar=1.0 / wwin, in1=U[:, LO:HI], op0=ALU.mult, op1=ALU.subtract),
                reads=[bS, b_uT], writes=[b_mix[g % 2]])
            for (a0, i0) in ((0, 0), (NQ - 8, 8)):
                T.op(dve, lambda S=S, a0=a0, i0=i0, g=g: nc.vector.tensor_tensor(
                    out=smallA[:, 16:24], in0=S[:, LO + a0:LO + a0 + 8], in1=invc[:, g, i0:i0 + 8], op=ALU.mult),
                    reads=[bS, b_const], writes=[b_rc])
                T.op(dve, lambda U=U, a0=a0, mb=mb: nc.vector.tensor_tensor(
                    out=mb[:, a0:a0 + 8], in0=smallA[:, 16:24], in1=U[:, LO + a0:LO + a0 + 8], op=ALU.subtract),
                    reads=[b_rc, b_uT], writes=[b_mix[g % 2]])
            for tg in range(4):
                pb = nextbank()
                T.op(pe, lambda pb=pb, g=g, mb=mb, tg=tg: nc.tensor.matmul(bank(pb), lhsT=w_poolB[:, g, :],
                                                                          rhs=mb[:, tg * 512:(tg + 1) * 512], start=True, stop=True),
                     reads=[b_mix[g % 2], b_ldP], writes=[PB[pb]])
                T.op(act, lambda pb=pb, g=g, tg=tg: nc.scalar.activation(out=catT[:, 4 + g, tg * 512:(tg + 1) * 512], in_=bank(pb),
                                                                         func=AF.Copy, scale=pscol[:, g:g + 1]),
                     reads=[PB[pb], b_const], writes=[b_cat])
        T.barrier()
        w_outS = AR.view(0, [128, 8, D], BF16)
        wo_stg = [AR.view((16 + 4 * i) * KB, [128, D], F32) for i in range(2)]
        x2_ring = [AR.view((24 + 4 * i) * KB, [128, D], F32) for i in range(2)]
        xnew = [AR.view((32 + 4 * i) * KB, [128, D], F32) for i in range(2)]
        xn2 = [AR.view((40 + 4 * i) * KB, [128, D], F32) for i in range(2)]
        h2Tf = [AR.view((48 + 4 * i) * KB, [128, 8, 128], F32) for i in range(2)]
        h2tok = [AR.view((56 + 2 * i) * KB, [128, D], BF16) for i in range(2)]
        G2B = AR.view(60 * KB, [128, D], F32)
        sh2B = AR.view(64 * KB, [128, D], F32)
        g1B = AR.view(68 * KB, [128, D], F32)
        tmpF = AR.view(72 * KB, [128, D], F32)
        b_wo = Buf("wo"); b_g1B = Buf("g1B"); b_GB = Buf("GB")
        wo_sem = [T.mksem(f"wo{i}") for i in range(2)]; b_wos = [Buf("wos0"), Buf("wos1")]
        x2_sem = [T.mksem(f"x2{i}") for i in range(2)]; b_x2 = [Buf("x20"), Buf("x21")]
        b_xnew = [Buf("xnew0"), Buf("xnew1")]
        b_xn2 = [Buf("xn20"), Buf("xn21")]; b_h2Tf = [Buf("h2Tf0"), Buf("h2Tf1")]
        b_h2tok = [Buf("h2tok0"), Buf("h2tok1")]; b_tmpF = Buf("tmpF")
        h2_sem = [T.mksem(f"h2s{i}") for i in range(2)]
        xw_sem = [T.mksem(f"xw{i}") for i in range(2)]
        sc_sem = T.mksem("scat")
        b_sm = Buf("sm"); b_rt = Buf("rt"); b_cum = Buf("cum")

        logit = sb("logit", [128, NE]); m8 = sb("m8", [128, 8]); idx8 = sb("idx8", [128, 8], U32)
        e4 = sb("e4", [128, 4]); gk4 = sb("gk4", [128, 4]); ekf = sb("ekf", [128, 4]); rkk = sb("rkk", [128, 4])
        slotf = sb("slotf", [128, 4]); gsum = sb("gsum", [128, 4]); gsumF = sb("gsumF", [128, 2])
        oh = sb("oh", [128, 4, NE]); Mb = sb("Mb", [128, NE], BF16); M2 = sb("M2", [128, 2, NE])
        gd = sb("gd", [128, NE]); rk = sb("rk", [128, NE]); cum = sb("cum", [128, NE]); junk32 = sb("junk32", [128, NE])
        gT = sb("gT", [32, 128])
        gd_all = sb("gd_all", [128, 16, NE]); gk_all = sb("gk_all", [128, 16, 4]); slot_all = sb("slot_all", [128, 16, 4], I32)
        cnt_i = sb("cnt_i", [128, NE + 1], I32); cmaxf = sb("cmaxf", [128, 1])

        T.op(dve, lambda: nc.vector.memset(cum[:], 0.0), writes=[b_cum])
        for hf in range(2):
            for (row, dst, bb) in ((g1row[0:1, :], g1B, b_g1B), (rows2[0:1, 0, :], G2B, b_GB), (rows2[0:1, 1, :], sh2B, b_GB)):
                pb = nextbank()
                T.op(pe, lambda pb=pb, hf=hf, row=row: nc.tensor.matmul(bank(pb), lhsT=onesF[0:1, :], rhs=row[:, hf * 512:(hf + 1) * 512],
                                                                        start=True, stop=True), reads=[b_ph0, b_const], writes=[PB[pb]])
                T.op(dve, lambda pb=pb, hf=hf, dst=dst: nc.vector.tensor_copy(out=dst[:, hf * 512:(hf + 1) * 512], in_=bank(pb)),
                     reads=[PB[pb]], writes=[bb])
        for k in range(8):
            s = k % 2
            T.dma(sp, wo_stg[s], w_out[k * 128:(k + 1) * 128, :], wo_sem[s], writes=[b_wos[s]])
            T.op(dve, lambda k=k, s=s: nc.vector.tensor_tensor(out=w_outS[:, k, :], in0=wo_stg[s], in1=g1B, op=ALU.mult),
                 reads=[b_wos[s], b_g1B], writes=[b_wo])

        b_smF = Buf("smF")

        def front_2a(qt):
            s = qt % 2
            T.dma(sp, x2_ring[s], xe[qt * 128:(qt + 1) * 128, :], x2_sem[s], writes=[b_x2[s]])
            for hf in range(2):
                pb = nextbank()

                def mm_o(pb=pb, hf=hf, qt=qt):
                    last = None
                    for k in range(8):
                        last = nc.tensor.matmul(bank(pb), lhsT=catT[:, k, qt * 128:(qt + 1) * 128],
                                                rhs=w_outS[:, k, hf * 512:(hf + 1) * 512], start=(k == 0), stop=(k == 7))
                    return last
                T.op(pe, mm_o, reads=[b_cat, b_wo], writes=[PB[pb]])
                T.op(dve, lambda pb=pb, hf=hf, s=s: nc.vector.tensor_tensor(
                    out=xnew[s][:, hf * 512:(hf + 1) * 512], in0=bank(pb), in1=x2_ring[s][:, hf * 512:(hf + 1) * 512], op=ALU.add),
                    reads=[PB[pb], b_x2[s]], writes=[b_xnew[s]])
            T.op(act, lambda s=s: nc.scalar.activation(out=junk[:], in_=xnew[s], func=AF.Square, accum_out=gsumF[:, 0:1]),
                 reads=[b_xnew[s]], writes=[b_smF])
            T.op(act, lambda: nc.scalar.activation(out=gsumF[:, 1:2], in_=gsumF[:, 0:1], func=AF.Sqrt, scale=1.0 / D,
                                                   bias=epsT[:]), reads=[b_smF], writes=[b_smF])
            T.op(dve, lambda: nc.vector.reciprocal(out=gsumF[:, 1:2], in_=gsumF[:, 1:2]), reads=[b_smF], writes=[b_smF])
            T.op(dve, lambda s=s: nc.vector.scalar_tensor_tensor(out=tmpF, in0=xnew[s], scalar=gsumF[:, 1:2], in1=G2B,
                                                                 op0=ALU.mult, op1=ALU.mult),
                 reads=[b_xnew[s], b_smF, b_GB], writes=[b_tmpF])
            T.op(dve, lambda s=s: nc.vector.tensor_tensor(out=xn2[s], in0=tmpF, in1=sh2B, op=ALU.add),
                 reads=[b_tmpF, b_GB], writes=[b_xn2[s]])
            T.op(act, lambda s=s: nc.scalar.copy(out=h2tok[s], in_=xn2[s]), reads=[b_xn2[s]], writes=[b_h2tok[s]])
            T.dma(act, h2d[qt * 128:(qt + 1) * 128, :], h2tok[s], h2_sem[s], reads=[b_h2tok[s]])

            def tr2(s=s):
                last = None
                for c in range(8):
                    last = nc.tensor.transpose(out=bank(c // 4)[:, (c % 4) * 128:(c % 4) * 128 + 128],
                                               in_=xn2[s][:, c * 128:(c + 1) * 128], identity=identF[:])
                return last
            T.op(pe, tr2, reads=[b_xn2[s], b_const], writes=[PB[0], PB[1]])
            T.op(act, lambda s=s: nc.scalar.copy(out=h2Tf[s], in_=bank(0, nb=2).rearrange("p (c t) -> p c t", t=128)),
                 reads=[PB[0], PB[1]], writes=[b_h2Tf[s]])

        logits_all = AR.view(76 * KB, [128, 16, NE], F32)
        m8_all = AR.view(78 * KB, [128, 16, 8], F32)
        idx8_all = AR.view(79 * KB, [128, 16, 8], U32)
        oh_all = AR.view(84 * KB, [128, 16, 4, NE], F32)
        ohw = AR.view(92 * KB, [128, 16, 4, NE], F32)
        rank_sb = AR.view(100 * KB, [128, 16, NE], F32)
        Mb_all = AR.view(102 * KB, [128, 16, NE], BF16)
        d4 = sb("d4", [128, 16, 4]); e4a = sb("e4a", [128, 16, 4]); gk4a = sb("gk4a", [128, 16, 4]); ekfa = sb("ekfa", [128, 16, 4])
        s4 = sb("s4", [128, 16]); rkka = sb("rkka", [128, 16, 4]); slotfa = sb("slotfa", [128, 16, 4])
        b_lg = Buf("lg")

        def router_2a(qt):
            s = qt % 2
            pbr = nextbank()

            def mm_r(pbr=pbr, s=s):
                last = None
                for c in range(8):
                    last = nc.tensor.matmul(bank(pbr, NE), lhsT=h2Tf[s][:, c, :], rhs=rwt[:, c, :], start=(c == 0), stop=(c == 7))
                return last
            T.op(pe, mm_r, reads=[b_h2Tf[s], b_const], writes=[PB[pbr]])
            T.op(dve, lambda pbr=pbr: nc.vector.tensor_tensor(out=logits_all[:, qt, :], in0=bank(pbr, NE), in1=rbB[:], op=ALU.add),
                 reads=[PB[pbr], b_const], writes=[b_lg])
            T.dma(act, xnew_d[qt * 128:(qt + 1) * 128, :], xnew[s], xw_sem[s], reads=[b_xnew[s]])

        X = mybir.AxisListType.X

        def route_batch(t0, t1):
            nt_ = t1 - t0
            sl = slice(t0, t1)
            for t in range(t0, t1):
                T.op(dve, lambda t=t: nc.vector.max(out=m8_all[:, t, :], in_=logits_all[:, t, :]), reads=[b_lg], writes=[b_rt])
            for t in range(t0, t1):
                T.op(dve, lambda t=t: nc.vector.max_index(out=idx8_all[:, t, :], in_max=m8_all[:, t, :], in_values=logits_all[:, t, :]),
                     reads=[b_lg, b_rt], writes=[b_rt])
            T.op(dve, lambda: nc.vector.tensor_tensor(out=d4[:, sl, :], in0=m8_all[:, sl, 0:4],
                                                      in1=m8_all[:, sl, 0:1].broadcast_to([128, nt_, 4]), op=ALU.subtract),
                 reads=[b_rt], writes=[b_rt])
            T.op(act, lambda: nc.scalar.activation(out=e4a[:, sl, :], in_=d4[:, sl, :], func=AF.Exp), reads=[b_rt], writes=[b_rt])
            T.op(dve, lambda: nc.vector.tensor_reduce(out=s4[:, sl], in_=e4a[:, sl, :], axis=X, op=ALU.add), reads=[b_rt], writes=[b_rt])
            T.op(dve, lambda: nc.vector.reciprocal(out=s4[:, sl], in_=s4[:, sl]), reads=[b_rt], writes=[b_rt])
            T.op(dve, lambda: nc.vector.tensor_tensor(out=gk4a[:, sl, :], in0=e4a[:, sl, :],
                                                      in1=s4[:, sl].unsqueeze(2).broadcast_to([128, nt_, 4]), op=ALU.mult),
                 reads=[b_rt], writes=[b_rt])
            T.op(dve, lambda: nc.vector.tensor_scalar(out=gk_all[:, sl, :], in0=gk4a[:, sl, :], scalar1=1.0 / 1.702, scalar2=None,
                                                      op0=ALU.mult), reads=[b_rt], writes=[b_rt])
            T.op(dve, lambda: nc.vector.tensor_copy(out=ekfa[:, sl, :], in_=idx8_all[:, sl, 0:4]), reads=[b_rt], writes=[b_rt])
            T.op(dve, lambda: nc.vector.tensor_tensor(
                out=oh_all[:, sl], in0=iota32[:].unsqueeze(1).unsqueeze(1).broadcast_to([128, nt_, 4, NE]),
                in1=ekfa[:, sl, :].unsqueeze(3).broadcast_to([128, nt_, 4, NE]), op=ALU.is_equal),
                reads=[b_rt, b_const], writes=[b_rt])
            T.op(dve, lambda: nc.vector.tensor_reduce(out=rank_sb[:, sl, :], in_=oh_all[:, sl].rearrange("p t k e -> p t e k"),
                                                      axis=X, op=ALU.add), reads=[b_rt], writes=[b_rt])
            T.op(dve, lambda: nc.vector.tensor_copy(out=Mb_all[:, sl, :], in_=rank_sb[:, sl, :]), reads=[b_rt], writes=[b_rt])
            T.op(dve, lambda: nc.vector.tensor_tensor(out=ohw[:, sl], in0=oh_all[:, sl],
                                                      in1=gk4a[:, sl, :].unsqueeze(3).broadcast_to([128, nt_, 4, NE]), op=ALU.mult),
                 reads=[b_rt], writes=[b_rt])
            T.op(dve, lambda: nc.vector.tensor_reduce(out=gd_all[:, sl, :], in_=ohw[:, sl].rearrange("p t k e -> p t e k"),
                                                      axis=X, op=ALU.add), reads=[b_rt], writes=[b_rt])
            rb = 2 + (t0 // 4) % 2

            def mm_rank():
                last = None
                for t in range(t0, t1):
                    c0 = (t - t0) * NE
                    last = nc.tensor.matmul(bank(rb)[:, c0:c0 + NE], lhsT=UstrB[:], rhs=Mb_all[:, t, :], start=True, stop=(t == 0),
                                            skip_group_check=True)
                    for t2 in range(t):
                        last = nc.tensor.matmul(bank(rb)[:, c0:c0 + NE], lhsT=onesB[:], rhs=Mb_all[:, t2, :], start=False,
                                                stop=(t2 == t - 1), skip_group_check=True)
                return last
            T.op(pe, mm_rank, reads=[b_rt, b_c2], writes=[PB[rb]])
            T.op(dve, lambda: nc.vector.tensor_copy(out=rank_sb[:, sl, :], in_=bank(rb, nt_ * NE).rearrange("p (t e) -> p t e", e=NE)),
                 reads=[PB[rb], b_rt], writes=[b_rt])
            T.op(dve, lambda: nc.vector.tensor_tensor(out=ohw[:, sl], in0=oh_all[:, sl],
                                                      in1=rank_sb[:, sl, :].unsqueeze(2).broadcast_to([128, nt_, 4, NE]), op=ALU.mult),
                 reads=[b_rt], writes=[b_rt])
            T.op(dve, lambda: nc.vector.tensor_reduce(out=rkka[:, sl, :], in_=ohw[:, sl], axis=X, op=ALU.add), reads=[b_rt], writes=[b_rt])
            T.op(dve, lambda: nc.vector.scalar_tensor_tensor(out=slotfa[:, sl, :], in0=ekfa[:, sl, :], scalar=2048.0, in1=rkka[:, sl, :],
                                                             op0=ALU.mult, op1=ALU.add), reads=[b_rt], writes=[b_rt])
            b_slot = Buf("slot")
            T.op(dve, lambda: nc.vector.tensor_copy(out=slot_all[:, sl, :], in_=slotfa[:, sl, :]), reads=[b_rt], writes=[b_slot])
            for qt in range(t0, t1):
                for k in range(4):
                    T._waits(pool, T._deps([b_slot, b_tokl0, b_const], []))
                    inst = nc.gpsimd.indirect_dma_start(out=tokl, out_offset=bass.IndirectOffsetOnAxis(ap=slot_all[:, qt, k:k + 1], axis=0),
                                                        in_=tokrow[:, qt, :], in_offset=None)
                    sc_sem.cnt += 16
                    inst.then_inc(sc_sem.h, 16)
                    T._update((sc_sem, sc_sem.cnt), [b_slot, b_tokl0, b_const], [])

        front_2a(0)
        for qt in range(16):
            if qt + 1 < 16:
                front_2a(qt + 1)
            router_2a(qt)
            if qt % 4 == 3:
                route_batch(qt - 3, qt + 1)

        def mm_cnt():
            last = None
            for t in range(16):
                last = nc.tensor.matmul(bank(4, NE), lhsT=onesB[:], rhs=Mb_all[:, t, :], start=(t == 0), stop=(t == 15))
            return last
        T.op(pe, mm_cnt, reads=[b_rt, b_c2], writes=[PB[4]])
        T.op(dve, lambda: nc.vector.tensor_copy(out=cum[:], in_=bank(4, NE)), reads=[PB[4], b_cum], writes=[b_cum])
        nt_sem = T.mksem("nts")
        b_nt = Buf("nt")
        cntf = sb("cntf", [128, NE + 1])
        T.op(dve, lambda: nc.vector.tensor_reduce(out=cmaxf[:], in_=cum[:], axis=mybir.AxisListType.X, op=ALU.max),
             reads=[b_cum], writes=[b_nt])
        T.op(dve, lambda: nc.vector.tensor_scalar(out=cntf[:, 0:NE], in0=cum[:], scalar1=-1.0, scalar2=4096.0,
                                                  op0=ALU.mult, op1=ALU.add), reads=[b_cum, b_nt], writes=[b_nt])
        T.op(dve, lambda: nc.vector.tensor_scalar(out=cntf[:, NE:NE + 1], in0=cmaxf[:], scalar1=-1.0, scalar2=4096.0,
                                                  op0=ALU.mult, op1=ALU.add), reads=[b_nt], writes=[b_nt])
        T.op(dve, lambda: nc.vector.tensor_copy(out=cnt_i[:], in_=cntf[:]), reads=[b_nt], writes=[b_nt])
        T.dma(sp, nt_d, cnt_i[0:1, :], nt_sem, reads=[b_nt], writes=[b_nt])
        T.barrier()

        NS = 3
        Wgu = [AR.view(32 * i * KB, [128, 8, 2 * D], BF16) for i in range(NS)]
        Wdn = [AR.view((96 + 16 * i) * KB, [128, 8, D], BF16) for i in range(NS)]
        bias_reg = AR.view(144 * KB, [128, 2 * D], BF16)
        bgur = [bias_reg[32 * i:32 * i + 1, :] for i in range(NS)]
        xb = [AR.view((148 + 2 * i) * KB, [128, D], BF16) for i in range(2)]
        xbT = [AR.view((152 + 2 * i) * KB, [128, 8, 128], BF16) for i in range(2)]
        Ag = AR.view(156 * KB, [128, D], F32)
        Qs = AR.view(160 * KB, [128, D], BF16)
        Cp = AR.view(162 * KB, [128, D], BF16)
        actS = [AR.view((164 + 2 * i) * KB, [128, D], BF16) for i in range(2)]
        actT = [AR.view((168 + 2 * i) * KB, [128, 8, 128], BF16) for i in range(2)]
        ysb = [AR.view((172 + 4 * i) * KB, [128, D], F32) for i in range(2)]
        idxT = [sb(f"idxT{i}", [128, 2], I32) for i in range(2)]
        w_sem = [T.mksem(f"wg{i}") for i in range(NS)]
        i_sem = [T.mksem(f"is{i}") for i in range(2)]; b_idx = [Buf("idx0"), Buf("idx1")]
        g_sem = [T.mksem(f"gs{i}") for i in range(2)]; b_xb = [Buf("xb0"), Buf("xb1")]
        y_sem = [T.mksem(f"ys{i}") for i in range(2)]; b_ysb = [Buf("ysb0"), Buf("ysb1")]
        b_xbT = [Buf("xbT0"), Buf("xbT1")]; b_Ag = Buf("Ag"); b_Qs = Buf("Qs"); b_Cp = Buf("Cp")
        b_actS = [Buf("actS0"), Buf("actS1")]; b_actT = [Buf("actT0"), Buf("actT1")]
        owners = {}
        for i in range(2):
            owners[i_sem[i]] = sp; owners[y_sem[i]] = act; owners[g_sem[i]] = pool
        for i in range(NS):
            owners[w_sem[i]] = pool

        b_Wc = [[Buf(f"Wc{i}_{c}") for c in range(13)] for i in range(NS)]

        def load_chunks(e, lo, hi):
            s = e % NS
            for ci in range(lo, hi):
                if ci < 8:
                    T.dma(pool, Wgu[s][:, ci, :], w_gu[e][ci * 128:(ci + 1) * 128, :], w_sem[s], writes=[b_Wc[s][ci]])
                else:
                    k0 = (ci - 8) * 2
                    T.dma(pool, Wdn[s][:, k0:k0 + 2, :], w_dn[e].rearrange("(k q) n -> q k n", q=128)[:, k0:k0 + 2, :],
                          w_sem[s], writes=[b_Wc[s][ci]])
                if ci == 0:
                    T.dma(pool, bgur[s], b_gu[e:e + 1, :], w_sem[s], writes=[b_Wc[s][12]])

        for eng in T.engs:
            T._waits(eng, {nt_sem: nt_sem.cnt})
        cnt_regs = []
        for e in range(NE + 1):
            rs = nc.alloc_registers(f"cnt{e}")
            for reg in rs:
                nc.reg_load(reg, nt_d[0:1, e:e + 1])
            cnt_regs.append(rs)

        def piece_A(e, j, s, u):
            r0 = e * 2048 + j * 128
            T.dma(sp, idxT[u][:], tokl[r0:r0 + 128, :], i_sem[u], writes=[b_idx[u]])
            T._waits(pool, T._deps([b_idx[u]], [b_xb[u]]))
            inst = nc.gpsimd.indirect_dma_start(out=xb[u], out_offset=None, in_=h2d,
                                                in_offset=bass.IndirectOffsetOnAxis(ap=idxT[u][:, 0:1], axis=0))
            g_sem[u].cnt += 16
            inst.then_inc(g_sem[u].h, 16)
            T._update((g_sem[u], g_sem[u].cnt), [b_idx[u]], [b_xb[u]])
            tp0 = bank(0).bitcast(BF16)

            def tr_in():
                last = None
                for c in range(8):
                    last = nc.tensor.transpose(out=tp0[:, c * 128:(c + 1) * 128], in_=xb[u][:, c * 128:(c + 1) * 128],
                                               identity=identB[:])
                return last
            T.op(pe, tr_in, reads=[b_xb[u], b_const], writes=[PB[0]])
            T.op(dve, lambda: nc.vector.tensor_copy(out=xbT[u], in_=tp0.rearrange("p (c t) -> p c t", t=128)),
                 reads=[PB[0]], writes=[b_xbT[u]])

            def mm_gu(c0):
                last = None
                for c in range(c0, c0 + 2):
                    nc.tensor.matmul(bank(1 + c), lhsT=onesB[32 * s:32 * s + 1, :], rhs=bgur[s][:, c * 512:(c + 1) * 512],
                                     start=True, stop=False)
                    for k in range(8):
                        last = nc.tensor.matmul(bank(1 + c), lhsT=xbT[u][:, k, :], rhs=Wgu[s][:, k, c * 512:(c + 1) * 512],
                                                start=False, stop=(k == 7))
                return last
            T.op(pe, lambda: mm_gu(0), reads=[b_xbT[u], b_c2] + b_Wc[s], writes=[PB[1], PB[2]])
            T.op(dve, lambda: nc.vector.tensor_scalar(out=Ag, in0=bank(1, nb=2), scalar1=7.0, scalar2=None, op0=ALU.min),
                 reads=[PB[1], PB[2]], writes=[b_Ag])
            T.op(pe, lambda: mm_gu(2), reads=[b_xbT[u], b_c2] + b_Wc[s], writes=[PB[3], PB[4]])
            T.op(act, lambda: nc.scalar.activation(out=Qs, in_=Ag, func=AF.Silu, scale=1.702), reads=[b_Ag], writes=[b_Qs])

        def piece_B(e, j, s, u):
            T.op(dve, lambda: nc.vector.tensor_scalar(out=Cp, in0=bank(3, nb=2), scalar1=1.0, scalar2=8.0,
                                                      op0=ALU.add, op1=ALU.min), reads=[PB[3], PB[4]], writes=[b_Cp])
            T.op(dve, lambda: nc.vector.scalar_tensor_tensor(out=actS[u], in0=Cp, scalar=-6.0, in1=Qs,
                                                             op0=ALU.max, op1=ALU.mult),
                 reads=[b_Cp, b_Qs], writes=[b_actS[u]])

        def piece_C(e, j, s, u):
            tp5 = bank(5).bitcast(BF16)

            def tr_act():
                last = None
                for c in range(8):
                    last = nc.tensor.transpose(out=tp5[:, c * 128:(c + 1) * 128], in_=actS[u][:, c * 128:(c + 1) * 128],
                                               identity=identB[:])
                return last
            T.op(pe, tr_act, reads=[b_actS[u], b_const], writes=[PB[5]])
            T.op(dve, lambda: nc.vector.tensor_copy(out=actT[u], in_=tp5.rearrange("p (c t) -> p c t", t=128)),
                 reads=[PB[5]], writes=[b_actT[u]])

        def piece_D(e, j, s, u):
            r0 = e * 2048 + j * 128

            def mm_dn():
                last = None
                for hf in range(2):
                    for k in range(8):
                        last = nc.tensor.matmul(bank(6 + hf), lhsT=actT[u][:, k, :], rhs=Wdn[s][:, k, hf * 512:(hf + 1) * 512],
                                                start=(k == 0), stop=(k == 7))
                return last
            T.op(pe, mm_dn, reads=[b_actT[u]] + b_Wc[s], writes=[PB[6], PB[7]])
            T.op(act, lambda: nc.scalar.copy(out=ysb[u], in_=bank(6, nb=2)), reads=[PB[6], PB[7]], writes=[b_ysb[u]])
            T.dma(act, Yd[r0:r0 + 128, :], ysb[u], y_sem[u], reads=[b_ysb[u]])

        def cond(fn, e, j, s, u):
            snap = T.cond_snapshot()
            with nc.If_lt(cnt_regs[e], 4096 - 128 * j):
                fn(e, j, s, u)
            with nc.Else():
                T.cond_compensate(snap, owners)
            T.cond_restore_seen(snap)

        def expert_tiles(e, s, j0, j1):
            for j in range(j0, j1 + 1):
                if j < j1:
                    cond(piece_A, e, j, s, j % 2)
                if j > j0:
                    cond(piece_C, e, j - 1, s, (j - 1) % 2)
                if j < j1:
                    cond(piece_B, e, j, s, j % 2)
                if j > j0:
                    cond(piece_D, e, j - 1, s, (j - 1) % 2)
                if j0 == 0 and 1 <= j <= 3 and e + 2 < NE and TPP == 4:
                    load_chunks(e + 2, 4 * (j - 1), 4 * j)

        load_chunks(0, 0, 12)
        load_chunks(1, 0, 12)
        for e in range(NE):
            expert_tiles(e, e % NS, 0, TPP)
            if TPP != 4 and e + 2 < NE:
                load_chunks(e + 2, 0, 12)
        for r in range(1, 16 // TPP):
            gsnap = T.cond_snapshot()
            with nc.If_lt(cnt_regs[NE], 4096 - 128 * TPP * r):
                for e in range(NE):
                    esnap = T.cond_snapshot()
                    with nc.If_lt(cnt_regs[e], 4096 - 128 * TPP * r):
                        load_chunks(e, 0, 12)
                        expert_tiles(e, e % NS, TPP * r, TPP * r + TPP)
                    with nc.Else():
                        T.cond_compensate(esnap, owners)
                    T.cond_restore_seen(esnap)
            with nc.Else():
                T.cond_compensate(gsnap, owners)
            T.cond_restore_seen(gsnap)
        T.barrier()

        fgB = AR.view(0, [128, D], F32)
        xa = [AR.view((4 + 4 * i) * KB, [128, D], F32) for i in range(2)]
        yk = [AR.view((12 + 4 * i) * KB, [128, D], F32) for i in range(4)]
        tacc = AR.view(28 * KB, [128, D], F32)
        ostg = [AR.view((32 + 4 * i) * KB, [128, D], F32) for i in range(2)]
        b_fg = Buf("fg"); b_os = [Buf("os0"), Buf("os1")]; b_xa = [Buf("xa0"), Buf("xa1")]
        b_yk = [Buf(f"yk{i}") for i in range(4)]; b_tacc = Buf("tacc")
        gTc = [AR.view((40 + i) * KB, [32, 128], F32) for i in range(2)]
        b_gTc = [Buf("gTc0"), Buf("gTc1")]
        o_sem = [T.mksem("o0"), T.mksem("o1")]
        xa_sem = [T.mksem("xa0"), T.mksem("xa1")]
        yk_sem = [T.mksem(f"yk{i}") for i in range(4)]
        T.dma(sp, fgB, fg_d, ldP, writes=[b_fg])
        for tt in range(16):
            s = tt % 2
            T.dma(sp, xa[s], xnew_d[tt * 128:(tt + 1) * 128, :], xa_sem[s], writes=[b_xa[s]])
            for k in range(4):
                T._waits(pool, T._deps([], [b_yk[k]]))
                inst = nc.gpsimd.indirect_dma_start(out=yk[k], out_offset=None, in_=Yd,
                                                    in_offset=bass.IndirectOffsetOnAxis(ap=slot_all[:, tt, k:k + 1], axis=0))
                yk_sem[k].cnt += 16
                inst.then_inc(yk_sem[k].h, 16)
                T._update((yk_sem[k], yk_sem[k].cnt), [], [b_yk[k]])
            T.op(dve, lambda tt=tt: nc.vector.tensor_scalar(out=tacc, in0=yk[0], scalar1=gk_all[:, tt, 0:1], scalar2=None,
                                                            op0=ALU.mult), reads=[b_yk[0]], writes=[b_tacc])
            for k in range(1, 4):
                T.op(dve, lambda tt=tt, k=k: nc.vector.scalar_tensor_tensor(out=tacc, in0=yk[k], scalar=gk_all[:, tt, k:k + 1],
                                                                            in1=tacc, op0=ALU.mult, op1=ALU.add),
                     reads=[b_yk[k], b_tacc], writes=[b_tacc])
            T.op(dve, lambda: nc.vector.tensor_tensor(out=tacc, in0=tacc, in1=g2B[:], op=ALU.mult), reads=[b_tacc], writes=[b_tacc])
            pbt = nextbank()
            T.op(pe, lambda pbt=pbt, tt=tt: nc.tensor.transpose(out=bank(pbt, 128, 32), in_=gd_all[:, tt, :], identity=identF[:]),
                 reads=[b_const], writes=[PB[pbt]])
            T.op(act, lambda pbt=pbt, s=s: nc.scalar.copy(out=gTc[s], in_=bank(pbt, 128, 32)), reads=[PB[pbt]], writes=[b_gTc[s]])
            pbd = 0

            def mm_bd(s=s):
                nc.tensor.matmul(bank(0), lhsT=gTc[s], rhs=bdS[:, 0:512], start=True, stop=True)
                return nc.tensor.matmul(bank(1), lhsT=gTc[s], rhs=bdS[:, 512:1024], start=True, stop=True)
            T.op(pe, mm_bd, reads=[b_gTc[s], b_ph0], writes=[PB[0], PB[1]])
            T.op(dve, lambda: nc.vector.tensor_tensor(out=tacc, in0=bank(0, nb=2), in1=tacc, op=ALU.add),
                 reads=[PB[0], PB[1], b_tacc], writes=[b_tacc])
            T.op(dve, lambda s=s: nc.vector.tensor_tensor(out=xa[s], in0=xa[s], in1=tacc, op=ALU.add),
                 reads=[b_tacc, b_xa[s]], writes=[b_xa[s]])
            T.op(act, lambda s=s: nc.scalar.activation(out=junk[:], in_=xa[s], func=AF.Square, accum_out=gsum[:, 0:1]),
                 reads=[b_xa[s]], writes=[b_sm])
            T.op(act, lambda: nc.scalar.activation(out=gsum[:, 1:2], in_=gsum[:, 0:1], func=AF.Sqrt, scale=1.0 / D,
                                                   bias=epsT[:]), reads=[b_sm], writes=[b_sm])
            T.op(dve, lambda: nc.vector.reciprocal(out=gsum[:, 1:2], in_=gsum[:, 1:2]), reads=[b_sm], writes=[b_sm])
            T.op(dve, lambda s=s: nc.vector.scalar_tensor_tensor(out=ostg[s], in0=xa[s], scalar=gsum[:, 1:2],
                                                                 in1=fgB, op0=ALU.mult, op1=ALU.mult),
                 reads=[b_xa[s], b_sm, b_fg], writes=[b_os[s]])
            T.dma(act, y[tt * 128:(tt + 1) * 128, :], ostg[s], o_sem[s], reads=[b_os[s]])
        T.barrier()
    return nc


def _rope_tables():
    rows = SEQ // 64
    row = np.repeat(np.arange(rows, dtype=np.float32), 64)
    col = np.tile(np.arange(64, dtype=np.float32), rows)
    freqs = (np.float32(10000.0) ** (-np.arange(8, dtype=np.float32) / np.float32(8))).astype(np.float32)
    ang = np.concatenate([row[:, None] * freqs, col[:, None] * freqs], axis=-1).astype(np.float32)
    return np.cos(ang).astype(np.float32), np.sin(ang).astype(np.float32)


def _core_inputs(core, x, c, ctx, c_ctx, shared, cos, sin):
    b, half = core // 2, core % 2
    q0 = half * NQ
    o0 = (1 - half) * NQ
    xe = np.zeros((36 * 128, D), np.float32)
    xe[0:NQ] = x[b, q0:q0 + NQ]
    hm = np.zeros((128, 16), np.float32)
    if half == 1:
        xe[16 * 128:17 * 128] = x[b, q0 - 128:q0]
        hm[:, 0:8] = 1.0
    else:
        xe[17 * 128:18 * 128] = x[b, q0 + NQ:q0 + NQ + 128]
        hm[:, 8:16] = 1.0
    xe[18 * 128:34 * 128] = x[b, o0:o0 + NQ]
    xe[34 * 128:36 * 128] = ctx[b]
    trig = np.zeros((2, 32, NK), np.float32)
    for (dst, src) in ((0, q0), (NQ, o0)):
        cs = cos[src:src + NQ].T
        sn = sin[src:src + NQ].T
        trig[0, 0:16, dst:dst + NQ] = cs
        trig[0, 16:32, dst:dst + NQ] = cs
        trig[1, 0:16, dst:dst + NQ] = sn
        trig[1, 16:32, dst:dst + NQ] = sn
    trig[0, :, 2 * NQ:] = 1.0
    ccol = np.stack([c[b].reshape(8, 128).T, c_ctx.reshape(8, 128).T], axis=-1).astype(np.float32)
    invc = np.zeros((128, 4, 16), np.float32)
    for g, w in enumerate((2, 4, 8, 16)):
        hw = w // 2
        for i in range(16):
            t = q0 + i if i < 8 else q0 + NQ - 16 + i
            lo = max(t - hw, 0)
            hi = min(t + hw, SEQ)
            invc[:, g, i] = np.float32(1.0) / np.float32(hi - lo)
    m = {"xe": xe, "trig": trig, "ccol": np.ascontiguousarray(ccol), "hmask": hm, "invc": invc}
    m.update(shared)
    return m


_NC_CACHE = {}


def kernel(x, c, ctx, c_ctx, w_mod, b_mod, norm1_g, w_in, q_norm_g, kv_norm_g, w_uq, w_ukv, w_pool, pool_scale,
           w_out, norm2_g, router_w, router_b, w_gate_up, b_gate_up, w_down, b_down, final_g):
    f = lambda a: np.ascontiguousarray(np.asarray(a, dtype=np.float32))
    x, c, ctx, c_ctx = f(x), f(c), f(ctx), f(c_ctx)
    col = lambda v: np.ascontiguousarray(f(v).reshape(-1, 128).T)
    shared = {
        "w_mod": f(w_mod)[0], "bmod2": np.ascontiguousarray(np.broadcast_to(f(b_mod)[0][None, :], (2, 6 * D))),
        "n1col": col(f(norm1_g)[0]), "n2col": col(f(norm2_g)[0]), "w_in": f(w_in)[0],
        "qgcol": col(f(q_norm_g)[0]), "kvgcol": col(f(kv_norm_g)[0]), "w_uq": f(w_uq)[0], "w_ukv": f(w_ukv)[0],
        "w_pool": f(w_pool)[0], "pscol": col(f(pool_scale)[0]), "w_out": f(w_out)[0], "router_w": f(router_w)[0],
        "rbrep": np.ascontiguousarray(np.broadcast_to(f(router_b)[0][None, :], (128, NE))),
        "w_gu": f(w_gate_up)[0],
        "b_gu": f(b_gate_up)[0], "n2row": np.ascontiguousarray(f(norm2_g)[0][None, :]),
        "iota32": np.ascontiguousarray(np.broadcast_to(np.arange(NE, dtype=np.float32)[None, :], (128, NE))),
        "ustr": np.triu(np.ones((128, 128), np.float32), 1),
        "tokrow": np.ascontiguousarray(np.broadcast_to(
            (np.arange(16, dtype=np.int32)[None, :, None] * 128 + np.arange(128, dtype=np.int32)[:, None, None]), (128, 16, 2))),
        "w_dn": f(w_down)[0], "b_dn": f(b_down)[0],
        "fgrep": np.ascontiguousarray(np.broadcast_to(f(final_g)[None, :], (128, D))),
        "ident": np.eye(128, dtype=np.float32),
    }
    cos, sin = _rope_tables()
    in_maps = [_core_inputs(core, x, c, ctx, c_ctx, shared, cos, sin) for core in range(8)]
    if "nc" not in _NC_CACHE:
        _NC_CACHE["nc"] = build_program()
    res = run_bass_kernel_spmd(_NC_CACHE["nc"], in_maps, core_ids=list(range(8)))
    out = np.zeros((4, SEQ, D), np.float32)
    for core in range(8):
        b, half = core // 2, core % 2
        out[b, half * NQ:(half + 1) * NQ] = res.results[core]["y"]
    return out
```
